# Optimizing a Trainium2 kernel written in Bass

```python
import math
import jax, jax.numpy as jnp
from jax import lax
import numpy as np

D_MODEL = 1024
BATCH = 2
SEQ = 8192
DEPTH = 4

N_MIXERS = 3
HEAD_DIM = 64
MIX_WIDTH = 768
N_MEM_HEADS = 4
MEM_WIDTH = N_MEM_HEADS * HEAD_DIM
N_MEM = 256
BRANCH = MIX_WIDTH + MEM_WIDTH
IN_WIDTH = 3 * MIX_WIDTH + MEM_WIDTH + BRANCH
RMS_EPS = 1e-6
SUBLN_EPS = 1e-5
NEG_INF = -1e30

DSW_GROUPS = ((128, 1), (512, 4), (2048, 16))
DSW_HEADS_PER_GROUP = 4
DSW_HEADS = DSW_HEADS_PER_GROUP * len(DSW_GROUPS)
DSW_GROUP_WIDTH = DSW_HEADS_PER_GROUP * HEAD_DIM
BAND_BLOCK = 128

MOBA_HEADS = MIX_WIDTH // HEAD_DIM
MOBA_BLOCK = 256
MOBA_TOPK = 3
MOBA_Q_CHUNK = 64

DIFF_HEADS = 6
DIFF_D = 64
DIFF_Q_BLOCK = 128
N_DIFF_LAYERS = len(range(2, DEPTH, N_MIXERS))

kernel_name = "hybrid_dilated_moba_diff_trunk"


def alibi_slopes(n):
    return jnp.asarray(2.0 ** (-8.0 * np.arange(1, n + 1) / n), dtype=jnp.float32)


def rmsnorm(x, g, eps=RMS_EPS):
    xf = x.astype(jnp.float32)
    y = xf * lax.rsqrt(jnp.mean(xf * xf, axis=-1, keepdims=True) + eps)
    return (y * g.astype(jnp.float32)).astype(x.dtype)


def split_heads(t, n):
    b, s, _ = t.shape
    return t.reshape(b, s, n, -1).transpose(0, 2, 1, 3)


def merge_heads(t):
    b, h, s, d = t.shape
    return t.transpose(0, 2, 1, 3).reshape(b, s, h * d)


def banded_causal_attention(q, k, v, span, slopes):
    b, h, L, hd = q.shape
    nb = -(-L // BAND_BLOCK)
    Lp = nb * BAND_BLOCK
    pad = ((0, 0), (0, 0), (0, Lp - L), (0, 0))
    qb = jnp.pad(q, pad).reshape(b, h, nb, BAND_BLOCK, hd)
    kb = jnp.pad(k, pad).reshape(b, h, nb, BAND_BLOCK, hd)
    vb = jnp.pad(v, pad).reshape(b, h, nb, BAND_BLOCK, hd)
    shift = ((0, 0), (0, 0), (1, 0), (0, 0), (0, 0))
    kk = jnp.concatenate([jnp.pad(kb, shift)[:, :, :-1], kb], axis=3)
    vv = jnp.concatenate([jnp.pad(vb, shift)[:, :, :-1], vb], axis=3)
    dist = (jnp.arange(BAND_BLOCK)[:, None] + BAND_BLOCK) - jnp.arange(2 * BAND_BLOCK)[None, :]
    kpos = jnp.arange(nb)[:, None] * BAND_BLOCK - BAND_BLOCK + jnp.arange(2 * BAND_BLOCK)[None, :]
    valid = ((dist >= 0) & (dist <= span))[None, :, :] & (kpos >= 0)[:, None, :]
    s = jnp.einsum('bhnqd,bhnkd->bhnqk', qb, kk).astype(jnp.float32) * (hd ** -0.5)
    s = s - slopes[None, :, None, None, None] * dist.astype(jnp.float32)
    s = jnp.where(valid, s, NEG_INF)
    lse = jax.nn.logsumexp(s, axis=-1)
    p = jnp.exp(s - lse[..., None])
    out = jnp.einsum('bhnqk,bhnkd->bhnqd', p, vv.astype(jnp.float32))
    return out.reshape(b, h, Lp, hd)[:, :, :L], lse.reshape(b, h, Lp)[:, :, :L]


def dilated_window_mixer(q, k, v):
    b, S, _ = q.shape
    hg = DSW_HEADS_PER_GROUP
    slopes_all = alibi_slopes(DSW_HEADS)
    outs, lses = [], []
    for g, (window, dil) in enumerate(DSW_GROUPS):
        cols = slice(g * DSW_GROUP_WIDTH, (g + 1) * DSW_GROUP_WIDTH)
        U = S // dil

        def to_residue(t):
            t = t.reshape(b, U, dil, hg, HEAD_DIM).transpose(0, 3, 2, 1, 4)
            return t.reshape(b, hg * dil, U, HEAD_DIM)

        slopes = jnp.repeat(slopes_all[g * hg:(g + 1) * hg] * dil, dil)
        o, lse = banded_causal_attention(to_residue(q[..., cols]), to_residue(k[..., cols]),
                                         to_residue(v[..., cols]), window // dil, slopes)
        o = o.reshape(b, hg, dil, U, HEAD_DIM).transpose(0, 3, 2, 1, 4).reshape(b, S, hg, HEAD_DIM)
        lse = lse.reshape(b, hg, dil, U).transpose(0, 3, 2, 1).reshape(b, S, hg)
        outs.append(o)
        lses.append(lse)
    alpha = jax.nn.softmax(jnp.stack(lses, axis=0), axis=0)
    mixed = [(outs[g] * alpha[g][..., None]).reshape(b, S, DSW_GROUP_WIDTH) for g in range(len(DSW_GROUPS))]
    return jnp.concatenate(mixed, axis=-1).astype(q.dtype)


def moba_mixer(q, k, v):
    b, S, _ = q.shape
    H = MOBA_HEADS
    slopes = alibi_slopes(H)
    nblk = -(-S // MOBA_BLOCK)
    Sp = nblk * MOBA_BLOCK
    n_gate = max(nblk, MOBA_TOPK)
    pad = ((0, 0), (0, 0), (0, Sp - S), (0, 0))
    qh = jnp.pad(split_heads(q, H), pad)
    kb = jnp.pad(split_heads(k, H), pad).reshape(b, H, nblk, MOBA_BLOCK, HEAD_DIM)
    vb = jnp.pad(split_heads(v, H), pad).reshape(b, H, nblk, MOBA_BLOCK, HEAD_DIM)
    kmean = jnp.mean(kb.astype(jnp.float32), axis=3)
    nchunk = Sp // MOBA_Q_CHUNK
    qc = qh.reshape(b, H, nchunk, MOBA_Q_CHUNK, HEAD_DIM).transpose(2, 0, 1, 3, 4)
    bi = jnp.arange(b)[:, None, None, None]
    hi = jnp.arange(H)[None, :, None, None]
    scale = HEAD_DIM ** -0.5

    def one_chunk(args):
        c, qx = args
        t = c * MOBA_Q_CHUNK + jnp.arange(MOBA_Q_CHUNK)
        n = (c * MOBA_Q_CHUNK) // MOBA_BLOCK
        gate = jnp.einsum('bhqd,bhnd->bhqn', qx.astype(jnp.float32), kmean)
        gate = jnp.pad(gate, ((0, 0), (0, 0), (0, 0), (0, n_gate - nblk)), constant_values=NEG_INF)
        gate = jnp.where(jnp.arange(n_gate) < n, gate, NEG_INF)
        _, idx = lax.top_k(gate, MOBA_TOPK)
        idx = jnp.minimum(idx, nblk - 1)
        sel_ok = jnp.arange(MOBA_TOPK) < n
        ks = kb[bi, hi, idx]
        vs = vb[bi, hi, idx]
        kpos_sel = idx[..., None] * MOBA_BLOCK + jnp.arange(MOBA_BLOCK)
        dist_sel = (t[None, None, :, None, None] - kpos_sel).astype(jnp.float32)
        s_sel = jnp.einsum('bhqd,bhqjkd->bhqjk', qx, ks).astype(jnp.float32) * scale
        s_sel = s_sel - slopes[None, :, None, None, None] * dist_sel
        s_sel = jnp.where(sel_ok[:, None], s_sel, NEG_INF)
        k_own = lax.dynamic_index_in_dim(kb, n, axis=2, keepdims=False)
        v_own = lax.dynamic_index_in_dim(vb, n, axis=2, keepdims=False)
        dist_own = t[:, None] - (n * MOBA_BLOCK + jnp.arange(MOBA_BLOCK))[None, :]
        s_own = jnp.einsum('bhqd,bhkd->bhqk', qx, k_own).astype(jnp.float32) * scale
        s_own = jnp.where(dist_own >= 0,
                          s_own - slopes[None, :, None, None] * dist_own.astype(jnp.float32), NEG_INF)
        s_all = jnp.concatenate([s_sel.reshape(b, H, MOBA_Q_CHUNK, MOBA_TOPK * MOBA_BLOCK), s_own], axis=-1)
        p = jax.nn.softmax(s_all, axis=-1)
        p_sel = p[..., :MOBA_TOPK * MOBA_BLOCK].reshape(b, H, MOBA_Q_CHUNK, MOBA_TOPK, MOBA_BLOCK)
        p_own = p[..., MOBA_TOPK * MOBA_BLOCK:]
        return (jnp.einsum('bhqjk,bhqjkd->bhqd', p_sel, vs.astype(jnp.float32))
                + jnp.einsum('bhqk,bhkd->bhqd', p_own, v_own.astype(jnp.float32)))

    o = lax.map(one_chunk, (jnp.arange(nchunk), qc))
    o = o.transpose(1, 2, 0, 3, 4).reshape(b, H, Sp, HEAD_DIM)[:, :, :S]
    return merge_heads(o).astype(q.dtype)


def diff_mixer(q, k, v, lq1, lk1, lq2, lk2, subln_g, lambda_init):
    b, S, _ = q.shape
    H = DIFF_HEADS
    slopes = alibi_slopes(H)
    qh = q.reshape(b, S, H, 2, DIFF_D).transpose(0, 2, 3, 1, 4)
    kh = k.reshape(b, S, H, 2, DIFF_D).transpose(0, 2, 3, 1, 4)
    vh = split_heads(v, H).astype(jnp.float32)
    lam = (jnp.exp(jnp.sum(lq1.astype(jnp.float32) * lk1.astype(jnp.float32)))
           - jnp.exp(jnp.sum(lq2.astype(jnp.float32) * lk2.astype(jnp.float32))) + lambda_init)
    nqb = S // DIFF_Q_BLOCK
    qblocks = qh.reshape(b, H, 2, nqb, DIFF_Q_BLOCK, DIFF_D).transpose(3, 0, 1, 2, 4, 5)
    kpos = jnp.arange(S)

    def one_block(args):
        j, qx = args
        t = j * DIFF_Q_BLOCK + jnp.arange(DIFF_Q_BLOCK)
        dist = t[:, None] - kpos[None, :]
        s = jnp.einsum('bhmqd,bhmkd->bhmqk', qx, kh).astype(jnp.float32) * (DIFF_D ** -0.5)
        s = jnp.where(dist >= 0, s - slopes[None, :, None, None, None] * dist.astype(jnp.float32), NEG_INF)
        p = jax.nn.softmax(s, axis=-1)
        w = p[:, :, 0] - lam * p[:, :, 1]
        return jnp.einsum('bhqk,bhkd->bhqd', w, vh)

    o = lax.map(one_block, (jnp.arange(nqb), qblocks))
    o = o.transpose(1, 2, 0, 3, 4).reshape(b, H, S, 2 * DIFF_D)
    o = rmsnorm(o, subln_g, SUBLN_EPS) * (1.0 - lambda_init)
    return merge_heads(o).astype(q.dtype)


def memory_attention(qm, mem_n, w_mem_kv):
    kv = mem_n @ w_mem_kv
    km = split_heads(kv[..., :MEM_WIDTH], N_MEM_HEADS)
    vm = split_heads(kv[..., MEM_WIDTH:], N_MEM_HEADS)
    qh = split_heads(qm, N_MEM_HEADS)
    s = jnp.einsum('bhqd,bhkd->bhqk', qh, km).astype(jnp.float32) * (HEAD_DIM ** -0.5)
    p = jax.nn.softmax(s, axis=-1)
    return merge_heads(jnp.einsum('bhqk,bhkd->bhqd', p, vm.astype(jnp.float32))).astype(qm.dtype)


def setup_inputs(seed: int = 0) -> dict:
    key = jax.random.key(seed)
    ks = jax.random.split(key, 14)
    f32 = jnp.float32
    nd = max(N_DIFF_LAYERS, 0)
    return {
        "x": jax.random.normal(ks[0], (BATCH, SEQ, D_MODEL), f32),
        "mem": jax.random.normal(ks[1], (BATCH, N_MEM, D_MODEL), f32),
        "norm_g": 1.0 + 0.02 * jax.random.normal(ks[2], (DEPTH, D_MODEL), f32),
        "w_in": jax.random.normal(ks[3], (DEPTH, D_MODEL, IN_WIDTH), f32) * D_MODEL ** -0.5,
        "w_out": jax.random.normal(ks[4], (DEPTH, BRANCH, D_MODEL), f32) * BRANCH ** -0.5,
        "mem_norm_g": 1.0 + 0.02 * jax.random.normal(ks[5], (DEPTH, D_MODEL), f32),
        "w_mem_kv": jax.random.normal(ks[6], (DEPTH, D_MODEL, 2 * MEM_WIDTH), f32) * D_MODEL ** -0.5,
        "diff_lambda_q1": 0.1 * jax.random.normal(ks[7], (nd, DIFF_D), f32),
        "diff_lambda_k1": 0.1 * jax.random.normal(ks[8], (nd, DIFF_D), f32),
        "diff_lambda_q2": 0.1 * jax.random.normal(ks[9], (nd, DIFF_D), f32),
        "diff_lambda_k2": 0.1 * jax.random.normal(ks[10], (nd, DIFF_D), f32),
        "diff_subln_g": 1.0 + 0.02 * jax.random.normal(ks[11], (nd, 2 * DIFF_D), f32),
        "final_norm_g": 1.0 + 0.02 * jax.random.normal(ks[12], (D_MODEL,), f32),
    }


def reference(x, mem, norm_g, w_in, w_out, mem_norm_g, w_mem_kv, diff_lambda_q1, diff_lambda_k1,
              diff_lambda_q2, diff_lambda_k2, diff_subln_g, final_norm_g):
    c1, c2, c3, c4 = MIX_WIDTH, 2 * MIX_WIDTH, 3 * MIX_WIDTH, 3 * MIX_WIDTH + MEM_WIDTH
    for i in range(DEPTH):
        h = rmsnorm(x, norm_g[i])
        proj = h @ w_in[i]
        q, k, v = proj[..., :c1], proj[..., c1:c2], proj[..., c2:c3]
        qm, gate = proj[..., c3:c4], proj[..., c4:]
        kind = i % N_MIXERS
        if kind == 0:
            mix = dilated_window_mixer(q, k, v)
        elif kind == 1:
            mix = moba_mixer(q, k, v)
        else:
            c = i // N_MIXERS
            lambda_init = 0.8 - 0.6 * math.exp(-0.3 * i)
            mix = diff_mixer(q, k, v, diff_lambda_q1[c], diff_lambda_k1[c], diff_lambda_q2[c],
                             diff_lambda_k2[c], diff_subln_g[c], lambda_init)
        mo = memory_attention(qm, rmsnorm(mem, mem_norm_g[i]), w_mem_kv[i])
        y = jnp.concatenate([mix, mo], axis=-1) * jax.nn.silu(gate)
        x = x + y @ w_out[i]
    return rmsnorm(x, final_norm_g)
```

```python
import numpy as np
import ml_dtypes
import concourse.bass as bass
import concourse.mybir as mybir
from concourse.bass_utils import run_bass_kernel_spmd

F32 = mybir.dt.float32
BF16 = mybir.dt.bfloat16
AF = mybir.ActivationFunctionType
ALU = mybir.AluOpType
AX = mybir.AxisListType
NPBF = ml_dtypes.bfloat16

D = 1024
S = 8192
B = 2
DEPTH = 4
TL = 2048
NCORE = 8
MIXW = 768
INW = 3584
NEG = -30000.0
RMS_EPS = 1e-6
SUBLN_EPS = 1e-5


class Res:
    __slots__ = ("name", "lw", "readers")

    def __init__(self, name=""):
        self.name = name
        self.lw = None
        self.readers = []


class Op:
    __slots__ = ("eng", "fn", "dma", "sem", "semval", "needs_inc", "idx", "waits", "gen")


class Prog:
    ENGS = ("pe", "act", "dve", "pool", "sp")

    def __init__(self, nc, same_sync=("act", "dve", "pool"), ndma=12):
        self.nc = nc
        self.h = dict(pe=nc.tensor, act=nc.scalar, dve=nc.vector, pool=nc.gpsimd, sp=nc.sync)
        self.ops = {e: [] for e in self.ENGS}
        self.obs = {e: {} for e in self.ENGS}
        self.same_sync = set(same_sync)
        self.esems = {e: [nc.alloc_semaphore(name=f"es_{e}_0")] for e in ("pe", "act", "dve", "pool")}
        self.gen = {e: 0 for e in ("pe", "act", "dve", "pool", "sp")}
        self.gcount = {e: 0 for e in ("pe", "act", "dve", "pool")}
        self.pending = {e: [] for e in self.ENGS}
        self.ndma = ndma
        self.dsem = {q: [nc.alloc_semaphore(name=f"ds_{q}_{i}") for i in range(ndma)] for q in ("sp", "pool", "act")}
        self.dlast = {q: [None] * ndma for q in ("sp", "pool", "act")}
        self.dcnt = {q: 0 for q in ("sp", "pool", "act")}
        self.all_dma = []

    def add(self, eng, fn, reads=(), writes=(), dma=False):
        op = Op()
        op.eng = eng
        op.fn = fn
        op.dma = dma
        op.needs_inc = False
        op.sem = None
        op.semval = None
        op.idx = len(self.ops[eng])
        op.gen = self.gen[eng]
        deps = []
        if self.pending[eng]:
            deps.extend(self.pending[eng])
            self.pending[eng] = []
        for r in reads:
            if r.lw is not None:
                deps.append(r.lw)
        for w in writes:
            if w.lw is not None:
                deps.append(w.lw)
            deps.extend(w.readers)
        if dma:
            n = self.dcnt[eng]
            slot = n % self.ndma
            prev = self.dlast[eng][slot]
            if prev is not None:
                deps.append(prev)
            self.dlast[eng][slot] = op
            op.sem = self.dsem[eng][slot]
            op.semval = 16 * (n // self.ndma + 1)
            self.dcnt[eng] = n + 1
            self.all_dma.append(op)
        waits = []
        obs = self.obs[eng]
        for p in deps:
            if p is op:
                continue
            if p.dma:
                key = ("d", id(p.sem))
                if obs.get(key, 0) >= p.semval:
                    continue
                obs[key] = p.semval
                waits.append(p)
            else:
                if p.eng == eng and eng not in self.same_sync:
                    continue
                key = ("e", p.eng)
                if obs.get(key, -1) >= p.idx:
                    continue
                obs[key] = p.idx
                if not p.needs_inc:
                    p.needs_inc = True
                    self.gcount[p.eng] += 1
                waits.append(p)
        op.waits = waits
        for r in reads:
            r.readers.append(op)
        for w in writes:
            w.lw = op
            w.readers = []
        self.ops[eng].append(op)
        return op

    def barrier(self, rotate_at=12000):
        lasts = []
        for e in ("pe", "act", "dve", "pool"):
            for op in reversed(self.ops[e]):
                if not op.dma:
                    if not op.needs_inc:
                        op.needs_inc = True
                        self.gcount[e] += 1
                    lasts.append(op)
                    break
        for q in self.dlast:
            for op in self.dlast[q]:
                if op is not None:
                    lasts.append(op)
        for e in self.ENGS:
            self.pending[e] = list(lasts)
        for e in ("pe", "act", "dve", "pool"):
            if self.gcount[e] > rotate_at:
                self.gen[e] += 1
                self.gcount[e] = 0
                self.esems[e].append(self.nc.alloc_semaphore(name=f"es_{e}_{self.gen[e]}"))

    def dma(self, q, out, in_, reads=(), writes=()):
        return self.add(q, lambda h: h.dma_start(out=out, in_=in_), reads, writes, dma=True)

    def emit(self):
        nc = self.nc
        final_waits = {}
        for op in self.all_dma:
            k = id(op.sem)
            if k not in final_waits or final_waits[k][1] < op.semval:
                final_waits[k] = (op.sem, op.semval)
        for e in ("pe", "act", "dve", "pool"):
            c = {}
            for op in self.ops[e]:
                if op.needs_inc and not op.dma:
                    c[op.gen] = c.get(op.gen, 0) + 1
                    op.sem = self.esems[e][op.gen]
                    op.semval = c[op.gen]
        self.max_semval = {e: sum(1 for o in self.ops[e] if o.needs_inc) for e in ("pe", "act", "dve", "pool")}

        def run(e):
            h = self.h[e]
            for op in self.ops[e]:
                for p in op.waits:
                    h.wait_ge(p.sem, p.semval)
                ins = op.fn(h)
                if op.dma:
                    ins.then_inc(op.sem, 16)
                elif op.needs_inc:
                    ins.then_inc(op.sem, 1)
            if e == "sp":
                for p in self.pending[e]:
                    h.wait_ge(p.sem, p.semval)
                for sem, val in final_waits.values():
                    h.wait_ge(sem, val)

        with nc.Block() as block:
            @block.tensor
            def _(t):
                run("pe")

            @block.scalar
            def _(t):
                run("act")

            @block.vector
            def _(t):
                run("dve")

            @block.gpsimd
            def _(t):
                run("pool")

            @block.sync
            def _(t):
                run("sp")


_LET = "abcdefgh"


def _view(ap2d, shape):
    dims = list(shape[1:])
    if len(dims) > 1:
        names = " ".join(_LET[:len(dims)])
        kw = {_LET[i]: dims[i] for i in range(len(dims))}
        ap2d = ap2d.rearrange(f"p ({names}) -> p {names}", **kw)
    if shape[0] != 128:
        ap2d = ap2d[0:shape[0]]
    return ap2d


class SB:
    def __init__(self, nc, arena_words=None):
        self.nc = nc
        self.n = 0
        self.arena = None
        if arena_words is not None:
            self.arena = nc.alloc_sbuf_tensor("arena", [128, arena_words], F32)
            self.words = arena_words
            self.off = 0
            self.banks = [nc.alloc_psum_tensor(f"bank{i}", [128, 512], F32) for i in range(8)]
            self.pi = 0

    def reset(self, keep=0):
        self.off = keep
        self.pi = 0

    def sb(self, shape, dt, name=None):
        self.n += 1
        if self.arena is None:
            return self.nc.alloc_sbuf_tensor(name or f"sb{self.n}", shape, dt)
        n = int(np.prod(shape[1:]))
        esz = 4 if dt == F32 else 2
        words = (n * esz + 3) // 4
        words = (words + 7) // 8 * 8
        assert self.off + words <= self.words, f"SBUF arena overflow: {self.off}+{words} > {self.words}"
        v = self.arena[:, self.off:self.off + words]
        self.off += words
        if dt != F32:
            v = v.bitcast(dt)
        v = v[:, 0:n]
        return _view(v, shape)

    def ps(self, shape, dt, name=None):
        self.n += 1
        if self.arena is None:
            return self.nc.alloc_psum_tensor(name or f"ps{self.n}", shape, dt)
        assert self.pi < 8, "out of PSUM banks"
        b = self.banks[self.pi]
        self.pi += 1
        n = int(np.prod(shape[1:]))
        return _view(b[:, 0:n], shape)


def unit_cols(kind, g, u):
    if kind in (0, 1):
        hh = 4 * u + g
        return hh * 64, MIXW + hh * 64, 2 * MIXW + hh * 64, 64
    mm = 3 * g + u
    head, m = mm // 2, mm % 2
    return head * 128 + m * 64, MIXW + head * 128 + m * 64, 2 * MIXW + head * 128, 128


def alibi_slopes(n):
    return (2.0 ** (-8.0 * np.arange(1, n + 1) / n)).astype(np.float64)


def phase_a(nc, P, A, kind, xT_res, xT_sb, wA, gcol_d, send_qk, send_v, gateT_d, qmT_d, load_x_from=None):
    vd = 128 if kind == 2 else 64
    NG = TL // 512
    ones_bf = A.sb([128, 128], BF16)
    r_ones = Res()
    P.add("pool", lambda h: h.memset(ones_bf[:], 1.0), writes=[r_ones])
    gcol = A.sb([128, 8], F32)
    r_g = Res()
    P.dma("sp", gcol[:], gcol_d, writes=[r_g])
    if load_x_from is not None:
        xv = load_x_from.rearrange("(c p) t -> p c t", p=128)
        for G in range(NG):
            P.dma("sp" if G % 2 == 0 else "pool", xT_sb[:, :, G * 512:(G + 1) * 512], xv[:, :, G * 512:(G + 1) * 512],
                  writes=[xT_res[G]])

    hT = A.sb([128, 8, TL], BF16)
    r_h = [Res() for _ in range(NG)]
    sq = [A.sb([128, 512], BF16) for _ in range(2)]
    r_sq = [Res() for _ in range(2)]
    ss_ps = A.ps([128, 512], F32)
    r_ss = Res()
    rstd = A.sb([128, 512], F32)
    r_rstd = Res()
    i = 0
    for G in range(NG):
        gs = slice(G * 512, (G + 1) * 512)
        for c in range(8):
            b = i % 2
            i += 1
            P.add("act", lambda h, b=b, c=c, gs=gs: h.activation(out=sq[b][:], in_=xT_sb[:, c, gs], func=AF.Square),
                  reads=[xT_res[G]], writes=[r_sq[b]])
            P.add("pe", lambda h, b=b, c=c: h.matmul(ss_ps[:], ones_bf[:], sq[b][:], start=(c == 0), stop=(c == 7)),
                  reads=[r_sq[b], r_ones], writes=[r_ss])
        P.add("act", lambda h: h.activation(out=rstd[:], in_=ss_ps[:], func=AF.Sqrt, scale=1.0 / D, bias=RMS_EPS),
              reads=[r_ss], writes=[r_rstd])
        P.add("dve", lambda h: h.reciprocal(out=rstd[:], in_=rstd[:]), reads=[r_rstd], writes=[r_rstd])
        for c in range(8):
            eng = "dve" if c % 2 == 0 else "pool"
            P.add(eng, lambda h, c=c, gs=gs: h.tensor_tensor(out=hT[:, c, gs], in0=xT_sb[:, c, gs], in1=rstd[:], op=ALU.mult),
                  reads=[xT_res[G], r_rstd], writes=[r_h[G]])

    CG = 256
    ncg = INW // CG
    wst = [A.sb([128, 8, CG], F32) for _ in range(2)]
    r_wst = [Res() for _ in range(2)]
    wb = [A.sb([128, 8, CG], BF16) for _ in range(2)]
    r_wb = [Res() for _ in range(2)]
    wv = wA.rearrange("(c p) n -> p c n", p=128)
    ev = [A.sb([128, TL], BF16) for _ in range(2)]
    r_ev = [Res() for _ in range(2)]
    vsb = [A.sb([128, 16, CG], BF16) for _ in range(2)]
    r_vsb = [Res() for _ in range(2)]
    pacc = [A.ps([128, 512], F32) for _ in range(4)]
    r_pacc = [Res() for _ in range(4)]
    pi = 0
    evi = 0
    vi = 0
    dest = {}
    for g in range(4):
        for u in range(3):
            qc, kc, vc, _ = unit_cols(kind, g, u)
            dest[qc] = (g, 0, u)
            dest[kc] = (g, 1, u)
    vdest = {}
    for g in range(4):
        for u in range(3):
            qc, kc, vc, _ = unit_cols(kind, g, u)
            vdest.setdefault(vc, []).append((g, u))
    for cg in range(ncg):
        b = cg % 2
        c0 = cg * CG
        P.dma("sp" if cg % 2 == 0 else "pool", wst[b][:], wv[:, :, c0:c0 + CG], writes=[r_wst[b]])
        for c in range(8):
            eng = "dve" if c % 2 == 0 else "pool"
            P.add(eng, lambda h, b=b, c=c: h.tensor_scalar(out=wb[b][:, c, :], in0=wst[b][:, c, :], scalar1=gcol[:, c:c + 1],
                                                            scalar2=None, op0=ALU.mult),
                  reads=[r_wst[b], r_g], writes=[r_wb[b]])
        if 2 * MIXW <= c0 < 3 * MIXW:
            vb = vi % 2
            vi += 1
            for tt in range(16):
                pb = pi % 4
                pi += 1
                for c in range(8):
                    P.add("pe", lambda h, pb=pb, c=c, tt=tt, b=b: h.matmul(pacc[pb][:, 0:CG], hT[:, c, tt * 128:(tt + 1) * 128],
                                                                          wb[b][:, c, :], start=(c == 0), stop=(c == 7)),
                          reads=[r_h[tt // 4], r_wb[b]], writes=[r_pacc[pb]])
                eng = "act" if tt % 2 == 0 else "dve"
                if eng == "act":
                    P.add("act", lambda h, pb=pb, vb=vb, tt=tt: h.activation(out=vsb[vb][:, tt, :], in_=pacc[pb][:, 0:CG], func=AF.Copy),
                          reads=[r_pacc[pb]], writes=[r_vsb[vb]])
                else:
                    P.add("dve", lambda h, pb=pb, vb=vb, tt=tt: h.tensor_copy(out=vsb[vb][:, tt, :], in_=pacc[pb][:, 0:CG]),
                          reads=[r_pacc[pb]], writes=[r_vsb[vb]])
            for off in range(0, CG, vd):
                col = c0 + off
                for (g, u) in vdest.get(col, []):
                    dst = send_v[g, u].rearrange("(t p) v -> p t v", p=128)
                    P.dma("sp", dst, vsb[vb][:, :, off:off + vd], reads=[r_vsb[vb]])
            continue
        for ch in range(CG // 128):
            col = c0 + ch * 128
            isq = col < MIXW or (3 * MIXW <= col < 3 * MIXW + 256)
            eb = evi % 2
            evi += 1
            for G in range(NG):
                gs = slice(G * 512, (G + 1) * 512)
                pb = pi % 4
                pi += 1
                for c in range(8):
                    P.add("pe", lambda h, pb=pb, c=c, gs=gs, b=b, ch=ch: h.matmul(pacc[pb][:], wb[b][:, c, ch * 128:(ch + 1) * 128],
                                                                                  hT[:, c, gs], start=(c == 0), stop=(c == 7)),
                          reads=[r_h[G], r_wb[b]], writes=[r_pacc[pb]])
                sc = 0.125 if isq else 1.0
                if G % 2 == 0:
                    P.add("act", lambda h, pb=pb, eb=eb, gs=gs, sc=sc: h.activation(out=ev[eb][:, gs], in_=pacc[pb][:], func=AF.Copy, scale=sc),
                          reads=[r_pacc[pb]], writes=[r_ev[eb]])
                else:
                    P.add("dve", lambda h, pb=pb, eb=eb, gs=gs, sc=sc: h.tensor_scalar(out=ev[eb][:, gs], in0=pacc[pb][:], scalar1=sc, scalar2=None, op0=ALU.mult),
                          reads=[r_pacc[pb]], writes=[r_ev[eb]])
            if col < 2 * MIXW:
                for half in range(2):
                    g, qk, u = dest[col + half * 64]
                    P.dma("sp", send_qk[g, qk, u], ev[eb][half * 64:(half + 1) * 64, :], reads=[r_ev[eb]])
            elif col < 3 * MIXW + 256:
                r0 = col - 3 * MIXW
                P.dma("sp", qmT_d[r0:r0 + 128, :], ev[eb][:], reads=[r_ev[eb]])
            else:
                r0 = col - (3 * MIXW + 256)
                P.dma("sp", gateT_d[r0:r0 + 128, :], ev[eb][:], reads=[r_ev[eb]])


def build_phase_a_prog(kind):
    nc = bass.Bass("TRN2", target_bir_lowering=False)
    vd = 128 if kind == 2 else 64
    xT_d = nc.dram_tensor("xT", [D, TL], F32, kind="ExternalInput").ap()
    w_d = nc.dram_tensor("w_in", [D, INW], F32, kind="ExternalInput").ap()
    g_d = nc.dram_tensor("gcol", [128, 8], F32, kind="ExternalInput").ap()
    send_qk = nc.dram_tensor("send_qk", [4, 2, 3, 64, TL], BF16, kind="ExternalOutput").ap()
    send_v = nc.dram_tensor("send_v", [4, 3, TL, vd], BF16, kind="ExternalOutput").ap()
    gateT = nc.dram_tensor("gateT", [D, TL], BF16, kind="ExternalOutput").ap()
    qmT = nc.dram_tensor("qmT", [256, TL], BF16, kind="ExternalOutput").ap()
    P = Prog(nc)
    A = SB(nc)
    xT_sb = A.sb([128, 8, TL], F32)
    xres = [Res() for _ in range(TL // 512)]
    phase_a(nc, P, A, kind, xres, xT_sb, w_d, g_d, send_qk, send_v, gateT, qmT, load_x_from=xT_d)
    P.emit()
    return nc, P


DSW = ((128, 1), (512, 4), (2048, 16))


def unit_slope(kind, g, u):
    if kind in (0, 1):
        return alibi_slopes(12)[4 * u + g]
    return alibi_slopes(6)[(3 * g + u) // 2]


def split3(x):
    hi = x.astype(NPBF)
    r = x - hi.astype(np.float64)
    lo = r.astype(NPBF)
    r2 = r - lo.astype(np.float64)
    lo2 = r2.astype(NPBF)
    return hi, lo, lo2


def phase_b_consts(kind, g):
    c = {}
    c["ident"] = np.eye(128, dtype=np.float32).astype(NPBF)
    sel = np.zeros((128, 64), np.float32)
    sel[64, :] = 1.0
    c["sel"] = sel
    t = np.arange(S, dtype=np.float64)
    qrow = np.zeros((3, 3, S), dtype=NPBF)
    biast = np.zeros((128, 3, 64), dtype=np.float32)
    p = np.arange(128, dtype=np.float64)
    for u in range(3):
        sl = unit_slope(kind, g, u)
        x = -sl * (t - (t // 512) * 512)
        hi, lo, lo2 = split3(x)
        qrow[u, 0], qrow[u, 1], qrow[u, 2] = hi, lo, lo2
        for j in range(64):
            jp = 3 - j
            biast[:, u, j] = (sl * (128.0 * jp + p)).astype(np.float32)
    c["qrow"] = qrow
    c["biast"] = biast
    kones = np.zeros((35, S), dtype=np.float32)
    for b in range(32):
        kones[b, b * 256:(b + 1) * 256] = 1.0
    kones[32:35] = 1.0
    c["kones"] = kones.astype(NPBF)
    q = np.arange(512)[None, :]
    if kind == 0:
        tiles = []
        for (w, d) in DSW:
            for jp in range(-w // 128, 4):
                tk = 128 * jp + np.arange(128)[:, None]
                dist = q - tk
                valid = (dist >= 0) & (dist <= w) & (dist % d == 0)
                tiles.append(np.where(valid, 0.0, NEG))
        m = np.stack(tiles, axis=1)
    else:
        tiles = []
        for i in range(4):
            tk = 128 * i + np.arange(128)[:, None]
            tiles.append(np.where(tk > q, NEG, 0.0))
        m = np.stack(tiles, axis=1)
    c["masks"] = m.astype(np.float32).astype(NPBF)
    return c


def phase_b(nc, P, A, kind, recv_qk, recv_v, cd, oT_d):
    vd = 128 if kind == 2 else 64
    VW = vd + 1
    arow = 96 if kind == 1 else 64
    KA = arow + 3
    NGq = S // 512
    nmask = 33 if kind == 0 else 4
    ident = A.sb([128, 128], BF16)
    r_id = Res()
    P.dma("sp", ident[:], cd["ident"], writes=[r_id])
    biast = A.sb([128, 3, 64], F32)
    r_bt = Res()
    P.dma("sp", biast[:], cd["biast"], writes=[r_bt])
    masks = A.sb([128, nmask, 512], BF16)
    r_mk = Res()
    P.dma("pool", masks[:], cd["masks"], writes=[r_mk])
    ones_f = A.sb([128, 64], F32)
    r_of = Res()
    P.dma("sp", ones_f[:], cd["sel"], writes=[r_of])

    nunit_res = 3 if kind == 0 else 2
    ka = [A.sb([128, S], BF16) for _ in range(nunit_res)]
    r_ka = [Res() for _ in range(nunit_res)]
    va = [A.sb([128, 64, VW], BF16) for _ in range(nunit_res)]
    r_va = [Res() for _ in range(nunit_res)]
    for i in range(nunit_res):
        P.add("pool", lambda h, i=i: h.memset(va[i][:, :, 64:65], 1.0), writes=[r_va[i]])
    NQB = 4
    qa = [A.sb([128, 512], BF16) for _ in range(NQB)]
    r_qa = [Res() for _ in range(NQB)]
    NS = 3
    s_ps = [A.ps([128, 512], F32) for _ in range(NS)]
    r_s = [Res() for _ in range(NS)]
    NPT = 4
    pt = [A.sb([128, 512], BF16) for _ in range(NPT)]
    r_pt = [Res() for _ in range(NPT)]
    if kind == 2:
        oa_ps = [A.ps([128, 512], F32) for _ in range(2)]
        ob_ps = [A.ps([128, 512], F32) for _ in range(2)]
        r_oa = [Res() for _ in range(2)]
        r_ob = [Res() for _ in range(2)]
    elif kind == 0:
        oa_ps = [A.ps([128, 512], F32) for _ in range(3)]
        r_oa = [Res() for _ in range(3)]
    else:
        oa_ps = [A.ps([128, 512], F32) for _ in range(2)]
        r_oa = [Res() for _ in range(2)]
    bc_ps = A.ps([128, 512], F32)
    r_bc = Res()
    rd = A.sb([128, 512], F32)
    r_rd = Res()
    P.add("pool", lambda h: h.memset(rd[:], 0.0), writes=[r_rd])
    osb = [A.sb([64, 512], F32) for _ in range(2)]
    r_osb = [Res() for _ in range(2)]
    outb = [A.sb([64, 512], BF16) for _ in range(4)]
    r_outb = [Res() for _ in range(4)]
    if kind == 1:
        gate_ps = A.ps([128, 4, 32], F32)
        r_gate = Res()
        tr_ps = A.ps([128, 512], F32)
        r_tr = Res()
        gm = A.sb([128, 4, 32], F32)
        r_gm = Res()
        P.add("pool", lambda h: h.memset(gm[:], -1e30), writes=[r_gm])
        mx8 = A.sb([128, 4, 8], F32)
        r_mx = Res()
        negm = A.sb([128, 4, 96], BF16)
        r_negm = Res()
        P.add("pool", lambda h: h.memset(negm[:], 0.0), writes=[r_negm])
        kms = A.sb([64, 32], F32)
        kmb = A.sb([64, 32], BF16)
        r_km = Res()

    cnt = dict(q=0, s=0, pt=0, o=0, osb=0, outb=0)

    def load_unit(u, slot):
        for src in range(4):
            P.dma("sp", ka[slot][0:64, src * TL:(src + 1) * TL], recv_qk[src, 1, u], writes=[r_ka[slot]])
        if kind == 1:
            P.dma("pool", ka[slot][64:99, :], cd["kones"], writes=[r_ka[slot]])
        else:
            P.dma("pool", ka[slot][64:67, :], cd["kones"][32:35, :], writes=[r_ka[slot]])
        for src in range(4):
            sv = recv_v[src, u].rearrange("(t p) v -> p t v", p=128)
            if vd == 64:
                P.dma("pool", va[slot][:, src * 16:(src + 1) * 16, 0:64], sv, writes=[r_va[slot]])
            else:
                P.dma("pool", va[slot][:, src * 16:(src + 1) * 16, 0:64], sv[:, :, 0:64], writes=[r_va[slot]])
                P.dma("pool", va[slot][:, src * 16:(src + 1) * 16, 65:129], sv[:, :, 64:128], writes=[r_va[slot]])

    def load_q(u, G):
        qb = cnt["q"] % NQB
        cnt["q"] += 1
        src, lg = G // 4, G % 4
        P.dma("sp", qa[qb][0:64, :], recv_qk[src, 0, u, :, lg * 512:(lg + 1) * 512], writes=[r_qa[qb]])
        P.dma("sp", qa[qb][arow:arow + 3, :], cd["qrow"][u, :, G * 512:(G + 1) * 512], writes=[r_qa[qb]])
        return qb

    def moba_prep(slot, qb, G):
        for i in range(4):
            P.add("pe", lambda h, i=i: h.matmul(gate_ps[:, i, :], qa[qb][0:64, i * 128:(i + 1) * 128], kmb[:, :], start=True, stop=True),
                  reads=[r_qa[qb], r_km], writes=[r_gate])
        for i in range(4):
            n = (4 * G + i) // 2
            if n > 0:
                P.add("dve", lambda h, i=i, n=n: h.tensor_copy(out=gm[:, i, 0:n], in_=gate_ps[:, i, 0:n]), reads=[r_gate], writes=[r_gm])
            P.add("dve", lambda h, i=i: h.max(out=mx8[:, i, :], in_=gm[:, i, :]), reads=[r_gm], writes=[r_mx])
            P.add("dve", lambda h, i=i: h.tensor_scalar(out=negm[:, i, 64:96], in0=gm[:, i, :], scalar1=mx8[:, i, 2:3], scalar2=1.0,
                                                        op0=ALU.is_ge, op1=ALU.subtract), reads=[r_gm, r_mx], writes=[r_negm])
            P.add("dve", lambda h, i=i, n=n: h.memset(negm[:, i, 64 + n:65 + n], 0.0), writes=[r_negm])
        for i in range(4):
            P.add("pe", lambda h, i=i: h.matmul(tr_ps[0:96, i * 128:(i + 1) * 128], negm[:, i, :], ident[:], start=True, stop=True),
                  reads=[r_negm, r_id], writes=[r_tr])
        P.add("act", lambda h: h.activation(out=qa[qb][64:96, :], in_=tr_ps[64:96, :], func=AF.Copy, scale=-NEG),
              reads=[r_tr], writes=[r_qa[qb]])

    def steps_for(u, G):
        out = []
        if kind == 0:
            w, d = DSW[u]
            nb = w // 128
            moff = [0, 5, 13][u]
            for jp in range(-nb, 4):
                kt = 4 * G + jp
                if kt < 0:
                    continue
                out.append((kt, moff + jp + nb, 3 - jp))
        else:
            for kt in range(0, 4 * G + 4):
                jp = kt - 4 * G
                out.append((kt, jp if jp >= 0 else None, 3 - jp))
        return out

    def attend(u, slot, G, qb, ob):
        steps = steps_for(u, G)
        n = len(steps)
        sb_of = {}
        pt_of = {}

        def qk(i):
            kt, mi, bj = steps[i]
            sbk = cnt["s"] % NS
            cnt["s"] += 1
            sb_of[i] = sbk
            P.add("pe", lambda h: h.matmul(s_ps[sbk][:], ka[slot][0:KA, kt * 128:(kt + 1) * 128], qa[qb][0:KA, :], start=True, stop=(mi is None)),
                  reads=[r_ka[slot], r_qa[qb]], writes=[r_s[sbk]])
            if mi is not None:
                P.add("pe", lambda h: h.matmul(s_ps[sbk][:], ident[:], masks[:, mi, :], start=False, stop=True),
                      reads=[r_id, r_mk], writes=[r_s[sbk]])

        def ex(i):
            kt, mi, bj = steps[i]
            sbk = sb_of[i]
            pb = cnt["pt"] % NPT
            cnt["pt"] += 1
            pt_of[i] = pb
            P.add("act", lambda h: h.activation(out=pt[pb][:], in_=s_ps[sbk][:], func=AF.Exp, bias=biast[:, u, bj:bj + 1], scale=1.0),
                  reads=[r_s[sbk], r_bt], writes=[r_pt[pb]])

        def pv(i):
            kt, mi, bj = steps[i]
            pb = pt_of[i]
            P.add("pe", lambda h: h.matmul(oa_ps[ob][0:65, :], va[slot][:, kt, 0:65], pt[pb][:], start=(i == 0), stop=(i == n - 1)),
                  reads=[r_va[slot], r_pt[pb]], writes=[r_oa[ob]])
            if kind == 2:
                P.add("pe", lambda h: h.matmul(ob_ps[ob][0:64, :], va[slot][:, kt, 65:129], pt[pb][:], start=(i == 0), stop=(i == n - 1)),
                      reads=[r_va[slot], r_pt[pb]], writes=[r_ob[ob]])

        LA = 2
        for i in range(min(LA, n)):
            qk(i)
            ex(i)
        for i in range(n):
            if i + LA < n:
                qk(i + LA)
                ex(i + LA)
            pv(i)

    def finish(u, G, obs):
        gs = slice(G * 512, (G + 1) * 512)
        first = True
        for (uu, ob) in obs:
            if first:
                P.add("dve", lambda h, ob=ob: h.tensor_copy(out=rd[64:65, :], in_=oa_ps[ob][64:65, :]), reads=[r_oa[ob]], writes=[r_rd])
                first = False
            else:
                P.add("dve", lambda h, ob=ob: h.tensor_tensor(out=rd[64:65, :], in0=rd[64:65, :], in1=oa_ps[ob][64:65, :], op=ALU.add),
                      reads=[r_oa[ob], r_rd], writes=[r_rd])
        P.add("dve", lambda h: h.reciprocal(out=rd[64:65, :], in_=rd[64:65, :]), reads=[r_rd], writes=[r_rd])
        P.add("pe", lambda h: h.matmul(bc_ps[0:64, :], ones_f[:, :], rd[:, :], start=True, stop=True),
              reads=[r_of, r_rd], writes=[r_bc])
        for (uu, ob) in obs:
            parts = [(oa_ps, r_oa, 0)] + ([(ob_ps, r_ob, 64)] if kind == 2 else [])
            for (pst, rr, row0) in parts:
                sb = cnt["osb"] % 2
                cnt["osb"] += 1
                bb = cnt["outb"] % 4
                cnt["outb"] += 1
                P.add("act", lambda h, pst=pst, ob=ob, sb=sb: h.activation(out=osb[sb][:], in_=pst[ob][0:64, :], func=AF.Copy),
                      reads=[rr[ob]], writes=[r_osb[sb]])
                P.add("dve", lambda h, sb=sb, bb=bb: h.tensor_tensor(out=outb[bb][:], in0=osb[sb][:], in1=bc_ps[0:64, :], op=ALU.mult),
                      reads=[r_osb[sb], r_bc], writes=[r_outb[bb]])
                P.dma("sp", oT_d[uu, row0:row0 + 64, gs], outb[bb][:], reads=[r_outb[bb]])

    if kind == 0:
        for u in range(3):
            load_unit(u, u)
        for G in range(NGq):
            obs = []
            for u in range(3):
                qb = load_q(u, G)
                attend(u, u, G, qb, u)
                obs.append((u, u))
            finish(0, G, obs)
    else:
        load_unit(0, 0)
        for u in range(3):
            slot = u % 2
            if u + 1 < 3:
                load_unit(u + 1, (u + 1) % 2)
            if kind == 1:
                P.add("dve", lambda h, slot=slot: h.tensor_reduce(out=kms[:, :], in_=ka[slot][0:64, :].rearrange("p (b k) -> p b k", k=256),
                                                                   axis=AX.X, op=ALU.add), reads=[r_ka[slot]], writes=[r_km])
                P.add("dve", lambda h: h.tensor_copy(out=kmb[:, :], in_=kms[:, :]), reads=[r_km], writes=[r_km])
                if u > 0:
                    P.add("pool", lambda h: h.memset(gm[:], -1e30), writes=[r_gm])
            for G in range(NGq):
                qb = load_q(u, G)
                if kind == 1:
                    moba_prep(slot, qb, G)
                ob = cnt["o"] % 2
                cnt["o"] += 1
                attend(u, slot, G, qb, ob)
                finish(u, G, [(u, ob)])


def build_phase_b_prog(kind):
    nc = bass.Bass("TRN2", target_bir_lowering=False)
    vd = 128 if kind == 2 else 64
    recv_qk = nc.dram_tensor("recv_qk", [4, 2, 3, 64, TL], BF16, kind="ExternalInput").ap()
    recv_v = nc.dram_tensor("recv_v", [4, 3, TL, vd], BF16, kind="ExternalInput").ap()
    nmask = 33 if kind == 0 else 4
    cd = dict(
        ident=nc.dram_tensor("ident", [128, 128], BF16, kind="ExternalInput").ap(),
        sel=nc.dram_tensor("sel", [128, 64], F32, kind="ExternalInput").ap(),
        qrow=nc.dram_tensor("qrow", [3, 3, S], BF16, kind="ExternalInput").ap(),
        biast=nc.dram_tensor("biast", [128, 3, 64], F32, kind="ExternalInput").ap(),
        kones=nc.dram_tensor("kones", [35, S], BF16, kind="ExternalInput").ap(),
        masks=nc.dram_tensor("masks", [128, nmask, 512], BF16, kind="ExternalInput").ap(),
    )
    oT = nc.dram_tensor("oT", [3, vd, S], BF16, kind="ExternalOutput").ap()
    P = Prog(nc)
    A = SB(nc)
    phase_b(nc, P, A, kind, recv_qk, recv_v, cd, oT)
    P.emit()
    return nc, P


def phase_c(nc, P, A, kind, layer, xT_res, xT_sb, recv_o, gateT_d, qmT_d, memT_d, mgcol_d, wkv_d, wout_d, sel_d,
            diffp=None, final=None, xT_out=None, load_x_from=None):
    vd = 128 if kind == 2 else 64
    NG = TL // 512
    lam_init = 0.8 - 0.6 * float(np.exp(-0.3 * layer))
    if load_x_from is not None:
        xv = load_x_from.rearrange("(c p) t -> p c t", p=128)
        for G in range(NG):
            P.dma("pool", xT_sb[:, :, G * 512:(G + 1) * 512], xv[:, :, G * 512:(G + 1) * 512], writes=[xT_res[G]])
    ones_bf = A.sb([128, 128], BF16)
    r_ones = Res()
    P.add("pool", lambda h: h.memset(ones_bf[:], 1.0), writes=[r_ones])
    sel = A.sb([128, 64], F32)
    r_sel = Res()
    P.dma("sp", sel[:], sel_d, writes=[r_sel])
    mgcol = A.sb([128, 8], F32)
    r_mg = Res()
    P.dma("sp", mgcol[:], mgcol_d, writes=[r_mg])

    NPS = 6
    ps = [A.ps([128, 512], F32) for _ in range(NPS)]
    r_ps = [Res() for _ in range(NPS)]
    cnt = dict(ps=0, pt=0, st=0)

    def nps():
        i = cnt["ps"] % NPS
        cnt["ps"] += 1
        return i

    memT = A.sb([128, 8, 256], F32)
    r_mem = Res()
    P.dma("sp", memT[:], memT_d.rearrange("(c p) t -> p c t", p=128), writes=[r_mem])
    sqm = [A.sb([128, 512], BF16) for _ in range(2)]
    r_sqm = [Res() for _ in range(2)]
    rstd = A.sb([128, 512], F32)
    r_rstd = Res()
    mnT = A.sb([128, 8, 256], BF16)
    r_mn = Res()
    pb = nps()
    for c in range(8):
        b = c % 2
        P.add("act", lambda h, b=b, c=c: h.activation(out=sqm[b][:, 0:256], in_=memT[:, c, :], func=AF.Square), reads=[r_mem], writes=[r_sqm[b]])
        P.add("pe", lambda h, b=b, c=c: h.matmul(ps[pb][:, 0:256], ones_bf[:], sqm[b][:, 0:256], start=(c == 0), stop=(c == 7)),
              reads=[r_sqm[b], r_ones], writes=[r_ps[pb]])
    P.add("act", lambda h: h.activation(out=rstd[:, 0:256], in_=ps[pb][:, 0:256], func=AF.Sqrt, scale=1.0 / D, bias=RMS_EPS), reads=[r_ps[pb]], writes=[r_rstd])
    P.add("dve", lambda h: h.reciprocal(out=rstd[:, 0:256], in_=rstd[:, 0:256]), reads=[r_rstd], writes=[r_rstd])
    for c in range(8):
        P.add("dve", lambda h, c=c: h.tensor_tensor(out=mnT[:, c, :], in0=memT[:, c, :], in1=rstd[:, 0:256], op=ALU.mult), reads=[r_mem, r_rstd], writes=[r_mn])
    WS = 128
    wst = [A.sb([128, 8, WS], F32) for _ in range(2)]
    r_wst = [Res() for _ in range(2)]
    wkv = A.sb([128, 8, 512], BF16)
    r_wkv = Res()
    wkv_v = wkv_d.rearrange("(c p) n -> p c n", p=128)
    for j in range(512 // WS):
        b = cnt["st"] % 2
        cnt["st"] += 1
        P.dma("sp", wst[b][:], wkv_v[:, :, j * WS:(j + 1) * WS], writes=[r_wst[b]])
        for c in range(8):
            eng = "dve" if c % 2 == 0 else "pool"
            P.add(eng, lambda h, b=b, c=c, j=j: h.tensor_scalar(out=wkv[:, c, j * WS:(j + 1) * WS], in0=wst[b][:, c, :], scalar1=mgcol[:, c:c + 1],
                                                                 scalar2=None, op0=ALU.mult), reads=[r_wst[b], r_mg], writes=[r_wkv])
    kmT = A.sb([128, 2, 256], BF16)
    r_kmT = Res()
    for ch in range(2):
        pb = nps()
        for c in range(8):
            P.add("pe", lambda h, pb=pb, c=c, ch=ch: h.matmul(ps[pb][:, 0:256], wkv[:, c, ch * 128:(ch + 1) * 128], mnT[:, c, :], start=(c == 0), stop=(c == 7)),
                  reads=[r_wkv, r_mn], writes=[r_ps[pb]])
        P.add("act", lambda h, pb=pb, ch=ch: h.activation(out=kmT[:, ch, :], in_=ps[pb][:, 0:256], func=AF.Copy), reads=[r_ps[pb]], writes=[r_kmT])
    vma = A.sb([128, 2, 4, 65], BF16)
    r_vma = Res()
    P.add("pool", lambda h: h.memset(vma[:], 1.0), writes=[r_vma])
    for mt in range(2):
        pb = nps()
        for c in range(8):
            P.add("pe", lambda h, pb=pb, c=c, mt=mt: h.matmul(ps[pb][:, 0:256], mnT[:, c, mt * 128:(mt + 1) * 128], wkv[:, c, 256:512], start=(c == 0), stop=(c == 7)),
                  reads=[r_wkv, r_mn], writes=[r_ps[pb]])
        P.add("dve", lambda h, pb=pb, mt=mt: h.tensor_copy(out=vma[:, mt, :, 0:64], in_=ps[pb][:, 0:256].rearrange("p (h d) -> p h d", d=64)),
              reads=[r_ps[pb]], writes=[r_vma])

    qm = A.sb([128, 2, TL], BF16)
    r_qm = Res()
    P.dma("sp", qm[:], qmT_d.rearrange("(c p) t -> p c t", p=128), writes=[r_qm])
    yT = A.sb([128, 8, TL], BF16)
    r_y = [[Res() for _ in range(NG)] for _ in range(8)]
    pt = [A.sb([128, 512], BF16) for _ in range(3)]
    r_pt = [Res() for _ in range(3)]
    rd = A.sb([128, 512], F32)
    r_rd = Res()
    P.add("pool", lambda h: h.memset(rd[:], 0.0), writes=[r_rd])
    osb = [A.sb([64, 512], F32) for _ in range(2)]
    r_osb = [Res() for _ in range(2)]
    mo = [A.sb([64, 512], BF16) for _ in range(2)]
    r_mo = [Res() for _ in range(2)]
    k = 0
    for G in range(NG):
        gs = slice(G * 512, (G + 1) * 512)
        for hm in range(4):
            ch, r0 = hm // 2, (hm % 2) * 64
            po = nps()
            for mt in range(2):
                pb = nps()
                P.add("pe", lambda h, pb=pb, ch=ch, r0=r0, mt=mt, gs=gs: h.matmul(ps[pb][:], kmT[r0:r0 + 64, ch, mt * 128:(mt + 1) * 128], qm[r0:r0 + 64, ch, gs],
                                                                                 start=True, stop=True), reads=[r_kmT, r_qm], writes=[r_ps[pb]])
                pi = cnt["pt"] % 3
                cnt["pt"] += 1
                P.add("act", lambda h, pb=pb, pi=pi: h.activation(out=pt[pi][:], in_=ps[pb][:], func=AF.Exp), reads=[r_ps[pb]], writes=[r_pt[pi]])
                P.add("pe", lambda h, po=po, pi=pi, mt=mt, hm=hm: h.matmul(ps[po][0:65, :], vma[:, mt, hm, :], pt[pi][:], start=(mt == 0), stop=(mt == 1)),
                      reads=[r_vma, r_pt[pi]], writes=[r_ps[po]])
            P.add("dve", lambda h, po=po: h.reciprocal(out=rd[64:65, :], in_=ps[po][64:65, :]), reads=[r_ps[po], r_rd], writes=[r_rd])
            pbc = nps()
            P.add("pe", lambda h, pbc=pbc: h.matmul(ps[pbc][0:64, :], sel[:, :], rd[:, :], start=True, stop=True), reads=[r_sel, r_rd], writes=[r_ps[pbc]])
            sb = k % 2
            mb = k % 2
            k += 1
            P.add("act", lambda h, po=po, sb=sb: h.activation(out=osb[sb][:], in_=ps[po][0:64, :], func=AF.Copy), reads=[r_ps[po]], writes=[r_osb[sb]])
            P.add("dve", lambda h, sb=sb, mb=mb, pbc=pbc: h.tensor_tensor(out=mo[mb][:], in0=osb[sb][:], in1=ps[pbc][0:64, :], op=ALU.mult),
                  reads=[r_osb[sb], r_ps[pbc]], writes=[r_mo[mb]])
            P.dma("pool", yT[r0:r0 + 64, 6 + ch, gs], mo[mb][:], reads=[r_mo[mb]], writes=[r_y[6 + ch][G]])

    if kind in (0, 1):
        for c in range(6):
            for half in range(2):
                hh = 2 * c + half
                g, u = hh % 4, hh // 4
                P.dma("sp", yT[half * 64:(half + 1) * 64, c, :], recv_o[g, u], writes=[r_y[c][G] for G in range(NG)])
    else:
        lp = A.sb([128, 4, 64], F32)
        r_lp = Res()
        for i, nm in enumerate(("lq1", "lk1", "lq2", "lk2")):
            P.dma("sp", lp[:, i, :], diffp[nm][0].partition_broadcast(128), writes=[r_lp])
        sgc = A.sb([128, 1], F32)
        r_sg = Res()
        P.dma("sp", sgc[:], diffp["sg"], writes=[r_sg])
        lpr = A.sb([128, 2, 64], F32)
        lsum = A.sb([128, 2], F32)
        nlam = A.sb([128, 1], F32)
        r_lam = Res()
        P.add("dve", lambda h: h.tensor_tensor(out=lpr[:, 0, :], in0=lp[:, 0, :], in1=lp[:, 1, :], op=ALU.mult), reads=[r_lp], writes=[r_lam])
        P.add("dve", lambda h: h.tensor_tensor(out=lpr[:, 1, :], in0=lp[:, 2, :], in1=lp[:, 3, :], op=ALU.mult), reads=[r_lp, r_lam], writes=[r_lam])
        P.add("dve", lambda h: h.tensor_reduce(out=lsum[:, :], in_=lpr[:, :, :], axis=AX.X, op=ALU.add), reads=[r_lam], writes=[r_lam])
        P.add("act", lambda h: h.activation(out=lsum[:, :], in_=lsum[:, :], func=AF.Exp), reads=[r_lam], writes=[r_lam])
        P.add("dve", lambda h: h.tensor_tensor(out=nlam[:, :], in0=lsum[:, 1:2], in1=lsum[:, 0:1], op=ALU.subtract), reads=[r_lam], writes=[r_lam])
        P.add("dve", lambda h: h.tensor_scalar(out=nlam[:, :], in0=nlam[:, :], scalar1=-lam_init, scalar2=None, op0=ALU.add), reads=[r_lam], writes=[r_lam])
        P.add("dve", lambda h: h.tensor_scalar(out=sgc[:, :], in0=sgc[:, :], scalar1=(1.0 - lam_init), scalar2=None, op0=ALU.mult), reads=[r_sg], writes=[r_sg])
        m12 = [A.sb([128, 2, 512], BF16) for _ in range(2)]
        r_m12 = [Res() for _ in range(2)]
        od = [A.sb([128, 512], F32) for _ in range(2)]
        r_od = [Res() for _ in range(2)]
        sq2 = [A.sb([128, 512], BF16) for _ in range(2)]
        r_sq2 = [Res() for _ in range(2)]
        rs2 = A.sb([128, 512], F32)
        r_rs2 = Res()
        k = 0
        for G in range(NG):
            gs = slice(G * 512, (G + 1) * 512)
            for c in range(6):
                b = k % 2
                k += 1
                for m_ in range(2):
                    mm = 2 * c + m_
                    g, u = mm // 3, mm % 3
                    P.dma("sp", m12[b][:, m_, :], recv_o[g, u, :, gs], writes=[r_m12[b]])
                P.add("dve", lambda h, b=b: h.scalar_tensor_tensor(out=od[b][:], in0=m12[b][:, 1, :], scalar=nlam[:, 0:1], in1=m12[b][:, 0, :],
                                                                     op0=ALU.mult, op1=ALU.add), reads=[r_m12[b], r_lam], writes=[r_od[b]])
                P.add("act", lambda h, b=b: h.activation(out=sq2[b][:], in_=od[b][:], func=AF.Square), reads=[r_od[b]], writes=[r_sq2[b]])
                pb = nps()
                P.add("pe", lambda h, pb=pb, b=b: h.matmul(ps[pb][:], ones_bf[:], sq2[b][:], start=True, stop=True), reads=[r_sq2[b], r_ones], writes=[r_ps[pb]])
                P.add("act", lambda h, pb=pb: h.activation(out=rs2[:], in_=ps[pb][:], func=AF.Sqrt, scale=1.0 / 128.0, bias=SUBLN_EPS), reads=[r_ps[pb]], writes=[r_rs2])
                P.add("dve", lambda h: h.reciprocal(out=rs2[:], in_=rs2[:]), reads=[r_rs2], writes=[r_rs2])
                P.add("dve", lambda h, b=b, c=c, gs=gs: h.scalar_tensor_tensor(out=yT[:, c, gs], in0=od[b][:], scalar=sgc[:, 0:1], in1=rs2[:],
                                                                                 op0=ALU.mult, op1=ALU.mult), reads=[r_od[b], r_sg, r_rs2], writes=[r_y[c][G]])

    gt = [A.sb([128, 512], BF16) for _ in range(3)]
    r_gt = [Res() for _ in range(3)]
    gview = gateT_d.rearrange("(c p) t -> p c t", p=128)
    kk = 0
    for c in range(8):
        for G in range(NG):
            b = kk % 3
            kk += 1
            gs = slice(G * 512, (G + 1) * 512)
            P.dma("sp", gt[b][:], gview[:, c, gs], writes=[r_gt[b]])
            P.add("act", lambda h, b=b: h.activation(out=gt[b][:], in_=gt[b][:], func=AF.Silu), reads=[r_gt[b]], writes=[r_gt[b]])
            eng = "dve" if G % 2 == 0 else "pool"
            P.add(eng, lambda h, b=b, c=c, gs=gs: h.tensor_tensor(out=yT[:, c, gs], in0=yT[:, c, gs], in1=gt[b][:], op=ALU.mult),
                  reads=[r_gt[b], r_y[c][G]], writes=[r_y[c][G]])

    wo = A.sb([128, 8, D], BF16)
    r_wo = [Res() for _ in range(8)]
    wo_v = wout_d.rearrange("(c p) n -> p c n", p=128)
    for j in range(8):
        b = cnt["st"] % 2
        cnt["st"] += 1
        P.dma("sp", wst[b][:], wo_v[:, :, j * WS:(j + 1) * WS], writes=[r_wst[b]])
        for c in range(8):
            eng = "dve" if c % 2 == 0 else "pool"
            P.add(eng, lambda h, b=b, c=c, j=j: h.tensor_copy(out=wo[:, c, j * WS:(j + 1) * WS], in_=wst[b][:, c, :]), reads=[r_wst[b]], writes=[r_wo[j]])
    for G in range(NG):
        gs = slice(G * 512, (G + 1) * 512)
        for co in range(8):
            pb = nps()
            for c in range(8):
                P.add("pe", lambda h, pb=pb, c=c, co=co, gs=gs: h.matmul(ps[pb][:], wo[:, c, co * 128:(co + 1) * 128], yT[:, c, gs], start=(c == 0), stop=(c == 7)),
                      reads=[r_wo[co], r_y[c][G]], writes=[r_ps[pb]])
            P.add("dve", lambda h, pb=pb, co=co, gs=gs: h.tensor_tensor(out=xT_sb[:, co, gs], in0=xT_sb[:, co, gs], in1=ps[pb][:], op=ALU.add),
                  reads=[r_ps[pb], xT_res[G]], writes=[xT_res[G]])
    if xT_out is not None:
        xo = xT_out.rearrange("(c p) t -> p c t", p=128)
        for G in range(NG):
            gs = slice(G * 512, (G + 1) * 512)
            P.dma("sp", xo[:, :, gs], xT_sb[:, :, gs], reads=[xT_res[G]])
    if final is not None:
        fg = A.sb([128, 8], F32)
        r_fg = Res()
        P.dma("sp", fg[:], final["gcol"], writes=[r_fg])
        ob = [A.sb([128, 512], F32) for _ in range(2)]
        r_ob = [Res() for _ in range(2)]
        fo = final["out"].rearrange("(c p) t -> p c t", p=128)
        k = 0
        for G in range(NG):
            gs = slice(G * 512, (G + 1) * 512)
            pb = nps()
            for c in range(8):
                b = c % 2
                P.add("act", lambda h, b=b, c=c, gs=gs: h.activation(out=sqm[b][:], in_=xT_sb[:, c, gs], func=AF.Square), reads=[xT_res[G]], writes=[r_sqm[b]])
                P.add("pe", lambda h, b=b, c=c, pb=pb: h.matmul(ps[pb][:], ones_bf[:], sqm[b][:], start=(c == 0), stop=(c == 7)), reads=[r_sqm[b], r_ones], writes=[r_ps[pb]])
            P.add("act", lambda h, pb=pb: h.activation(out=rstd[:], in_=ps[pb][:], func=AF.Sqrt, scale=1.0 / D, bias=RMS_EPS), reads=[r_ps[pb]], writes=[r_rstd])
            P.add("dve", lambda h: h.reciprocal(out=rstd[:], in_=rstd[:]), reads=[r_rstd], writes=[r_rstd])
            for c in range(8):
                b = k % 2
                k += 1
                P.add("dve", lambda h, b=b, c=c, gs=gs: h.scalar_tensor_tensor(out=ob[b][:], in0=xT_sb[:, c, gs], scalar=fg[:, c:c + 1], in1=rstd[:],
                                                                                 op0=ALU.mult, op1=ALU.mult), reads=[xT_res[G], r_fg, r_rstd], writes=[r_ob[b]])
                P.dma("sp", fo[:, c, gs], ob[b][:], reads=[r_ob[b]])


def build_phase_c_prog(kind, layer, last):
    nc = bass.Bass("TRN2", target_bir_lowering=False)
    vd = 128 if kind == 2 else 64
    xT_d = nc.dram_tensor("xT", [D, TL], F32, kind="ExternalInput").ap()
    recv_o = nc.dram_tensor("recv_o", [4, 3, vd, TL], BF16, kind="ExternalInput").ap()
    gateT = nc.dram_tensor("gateT", [D, TL], BF16, kind="ExternalInput").ap()
    qmT = nc.dram_tensor("qmT", [256, TL], BF16, kind="ExternalInput").ap()
    memT = nc.dram_tensor("memT", [D, 256], F32, kind="ExternalInput").ap()
    mgcol = nc.dram_tensor("mgcol", [128, 8], F32, kind="ExternalInput").ap()
    wkv = nc.dram_tensor("wkv", [D, 512], F32, kind="ExternalInput").ap()
    wout = nc.dram_tensor("wout", [D, D], F32, kind="ExternalInput").ap()
    sel = nc.dram_tensor("sel", [128, 64], F32, kind="ExternalInput").ap()
    diffp = None
    if kind == 2:
        diffp = {nm: nc.dram_tensor(nm, [1, 64], F32, kind="ExternalInput").ap() for nm in ("lq1", "lk1", "lq2", "lk2")}
        diffp["sg"] = nc.dram_tensor("sg", [128, 1], F32, kind="ExternalInput").ap()
    final = None
    xT_out = None
    if last:
        final = dict(gcol=nc.dram_tensor("fgcol", [128, 8], F32, kind="ExternalInput").ap(),
                     out=nc.dram_tensor("outT", [D, TL], F32, kind="ExternalOutput").ap())
    else:
        xT_out = nc.dram_tensor("xT_out", [D, TL], F32, kind="ExternalOutput").ap()
    P = Prog(nc)
    A = SB(nc)
    xT_sb = A.sb([128, 8, TL], F32)
    xres = [Res() for _ in range(TL // 512)]
    phase_c(nc, P, A, kind, layer, xres, xT_sb, recv_o, gateT, qmT, memT, mgcol, wkv, wout, sel, diffp=diffp, final=final,
            xT_out=xT_out, load_x_from=xT_d)
    P.emit()
    return nc, P


_PROGS = {}
DEBUG = {}


def _prog(key, builder):
    if key not in _PROGS:
        _PROGS[key] = builder()[0]
    return _PROGS[key]


def _col8(v):
    return np.ascontiguousarray(np.asarray(v, np.float32).reshape(8, 128).T)


def kernel_unfused(x, mem, norm_g, w_in, w_out, mem_norm_g, w_mem_kv, diff_lambda_q1, diff_lambda_k1,
                   diff_lambda_q2, diff_lambda_k2, diff_subln_g, final_norm_g):
    x = np.asarray(x, np.float32)
    mem = np.asarray(mem, np.float32)
    cores = list(range(NCORE))
    xT = [np.ascontiguousarray(x[c // 4, (c % 4) * TL:(c % 4 + 1) * TL, :].T) for c in cores]
    memT = [np.ascontiguousarray(mem[b].T) for b in range(B)]
    sel = np.zeros((128, 64), np.float32)
    sel[64, :] = 1.0
    out = None
    for layer in range(DEPTH):
        kind = layer % 3
        vd = 128 if kind == 2 else 64
        last = layer == DEPTH - 1
        ncA = _prog(("A", kind), lambda: build_phase_a_prog(kind))
        wl = np.ascontiguousarray(np.asarray(w_in[layer], np.float32))
        gcol = _col8(norm_g[layer])
        resA = run_bass_kernel_spmd(ncA, [{"xT": xT[c], "w_in": wl, "gcol": gcol} for c in cores], core_ids=cores).results
        in_b = []
        for c in cores:
            b, g = c // 4, c % 4
            rq = np.stack([resA[b * 4 + r]["send_qk"][g] for r in range(4)], axis=0)
            rv = np.stack([resA[b * 4 + r]["send_v"][g] for r in range(4)], axis=0)
            m = {"recv_qk": np.ascontiguousarray(rq), "recv_v": np.ascontiguousarray(rv)}
            m.update(phase_b_consts(kind, g))
            in_b.append(m)
        ncB = _prog(("B", kind), lambda: build_phase_b_prog(kind))
        resB = run_bass_kernel_spmd(ncB, in_b, core_ids=cores).results
        in_c = []
        for c in cores:
            b, r = c // 4, c % 4
            ro = np.stack([resB[b * 4 + g]["oT"][:, :, r * TL:(r + 1) * TL] for g in range(4)], axis=0)
            m = {"xT": xT[c], "recv_o": np.ascontiguousarray(ro), "gateT": resA[c]["gateT"], "qmT": resA[c]["qmT"],
                 "memT": memT[b], "mgcol": _col8(mem_norm_g[layer]),
                 "wkv": np.ascontiguousarray(np.asarray(w_mem_kv[layer], np.float32)),
                 "wout": np.ascontiguousarray(np.asarray(w_out[layer], np.float32)), "sel": sel}
            if kind == 2:
                ci = layer // 3
                m["lq1"] = np.asarray(diff_lambda_q1[ci], np.float32).reshape(1, 64)
                m["lk1"] = np.asarray(diff_lambda_k1[ci], np.float32).reshape(1, 64)
                m["lq2"] = np.asarray(diff_lambda_q2[ci], np.float32).reshape(1, 64)
                m["lk2"] = np.asarray(diff_lambda_k2[ci], np.float32).reshape(1, 64)
                m["sg"] = np.asarray(diff_subln_g[ci], np.float32).reshape(128, 1)
            if last:
                m["fgcol"] = _col8(final_norm_g)
            in_c.append(m)
        ncC = _prog(("C", kind, layer, last), lambda: build_phase_c_prog(kind, layer, last))
        resC = run_bass_kernel_spmd(ncC, in_c, core_ids=cores).results
        if last:
            out = np.empty((B, S, D), np.float32)
            for c in cores:
                out[c // 4, (c % 4) * TL:(c % 4 + 1) * TL, :] = resC[c]["outT"].T
        else:
            xT = [resC[c]["xT_out"] for c in cores]
            if "dump" in DEBUG:
                xs = np.empty((B, S, D), np.float32)
                for c in cores:
                    xs[c // 4, (c % 4) * TL:(c % 4 + 1) * TL, :] = xT[c].T
                DEBUG["dump"].append(xs)
    return out


KINDS = [l % 3 for l in range(DEPTH)]


def build_fused():
    nc = bass.Bass("TRN2", target_bir_lowering=False)

    def din(name, shape, dt=F32):
        return nc.dram_tensor(name, shape, dt, kind="ExternalInput").ap()

    def dint(name, shape, dt):
        return nc.dram_tensor(name, shape, dt, kind="Internal").ap()

    xT_in = din("xT_in", [4, D, TL])
    w_in = din("w_in", [DEPTH, D, INW])
    w_out = din("w_out", [DEPTH, D, D])
    wkv = din("wkv", [DEPTH, D, 512])
    gcols = din("gcols", [DEPTH, 128, 8])
    mgcols = din("mgcols", [DEPTH, 128, 8])
    fgcol = din("fgcol", [128, 8])
    memT = din("memT", [D, 256])
    sel = din("sel", [128, 64])
    diffp = {nm: din(nm, [1, 64]) for nm in ("lq1", "lk1", "lq2", "lk2")}
    diffp["sg"] = din("sg", [128, 1])
    ident = din("ident", [128, 128], BF16)
    kones = din("kones", [35, S], BF16)
    masks = {0: din("masks0", [128, 33, 512], BF16), 1: din("masks1", [128, 4, 512], BF16)}
    masks[2] = masks[1]
    qrow = {k: din(f"qrow{k}", [4, 3, 3, S], BF16) for k in (0, 1, 2)}
    biast = {k: din(f"biast{k}", [4, 128, 3, 64]) for k in (0, 1, 2)}
    outT = nc.dram_tensor("outT", [4, D, TL], F32, kind="ExternalOutput").ap()

    xbuf = dint("xbuf", [2, 4, D, TL], F32)
    qkbuf = dint("qkbuf", [4, 4, 2, 3, 64, TL], BF16)
    vbuf = {64: dint("vbuf64", [4, 4, 3, TL, 64], BF16), 128: dint("vbuf128", [4, 4, 3, TL, 128], BF16)}
    gbuf = dint("gbuf", [4, D, TL], BF16)
    qmbuf = dint("qmbuf", [4, 256, TL], BF16)
    obuf = {64: dint("obuf64", [4, 3, 64, S], BF16), 128: dint("obuf128", [4, 3, 128, S], BF16)}

    P = Prog(nc)
    A = SB(nc, arena_words=48 * 1024 - 64)
    for layer in range(DEPTH):
        kind = KINDS[layer]
        vd = 128 if kind == 2 else 64
        last = layer == DEPTH - 1
        for s_ in range(4):
            A.reset()
            xT_sb = A.sb([128, 8, TL], F32)
            xres = [Res() for _ in range(TL // 512)]
            src = xT_in[s_] if layer == 0 else xbuf[layer % 2, s_]
            phase_a(nc, P, A, kind, xres, xT_sb, w_in[layer], gcols[layer], qkbuf[s_], vbuf[vd][s_], gbuf[s_], qmbuf[s_], load_x_from=src)
            P.barrier()
        for g in range(4):
            A.reset()
            cd = dict(ident=ident, sel=sel, kones=kones, masks=masks[kind], qrow=qrow[kind][g], biast=biast[kind][g])
            phase_b(nc, P, A, kind, qkbuf[:, g], vbuf[vd][:, g], cd, obuf[vd][g])
            P.barrier()
        for s_ in range(4):
            A.reset()
            xT_sb = A.sb([128, 8, TL], F32)
            xres = [Res() for _ in range(TL // 512)]
            src = xT_in[s_] if layer == 0 else xbuf[layer % 2, s_]
            phase_c(nc, P, A, kind, layer, xres, xT_sb, obuf[vd][:, :, :, s_ * TL:(s_ + 1) * TL], gbuf[s_], qmbuf[s_], memT, mgcols[layer],
                    wkv[layer], w_out[layer], sel, diffp=(diffp if kind == 2 else None),
                    final=(dict(gcol=fgcol, out=outT[s_]) if last else None),
                    xT_out=(None if last else xbuf[(layer + 1) % 2, s_]), load_x_from=src)
            P.barrier()
    P.emit()
    return nc, P


_FUSED = {}


def kernel(x, mem, norm_g, w_in, w_out, mem_norm_g, w_mem_kv, diff_lambda_q1, diff_lambda_k1,
           diff_lambda_q2, diff_lambda_k2, diff_subln_g, final_norm_g):
    x = np.asarray(x, np.float32)
    mem = np.asarray(mem, np.float32)
    cores = list(range(NCORE))
    if "nc" not in _FUSED:
        _FUSED["nc"] = build_fused()[0]
    nc = _FUSED["nc"]
    f32 = lambda a: np.ascontiguousarray(np.asarray(a, np.float32))
    sel = np.zeros((128, 64), np.float32)
    sel[64, :] = 1.0
    shared = {
        "w_in": f32(w_in), "w_out": f32(w_out), "wkv": f32(w_mem_kv),
        "gcols": np.stack([_col8(norm_g[l]) for l in range(DEPTH)]),
        "mgcols": np.stack([_col8(mem_norm_g[l]) for l in range(DEPTH)]),
        "fgcol": _col8(final_norm_g), "sel": sel,
        "lq1": f32(diff_lambda_q1[0]).reshape(1, 64), "lk1": f32(diff_lambda_k1[0]).reshape(1, 64),
        "lq2": f32(diff_lambda_q2[0]).reshape(1, 64), "lk2": f32(diff_lambda_k2[0]).reshape(1, 64),
        "sg": f32(diff_subln_g[0]).reshape(128, 1),
    }
    for k in (0, 1, 2):
        cs = [phase_b_consts(k, g) for g in range(4)]
        shared[f"qrow{k}"] = np.stack([c["qrow"] for c in cs])
        shared[f"biast{k}"] = np.stack([c["biast"] for c in cs])
        if k < 2:
            shared[f"masks{k}"] = cs[0]["masks"]
        shared["ident"] = cs[0]["ident"]
        shared["kones"] = cs[0]["kones"]
    in_maps = []
    for c in cores:
        b = c // 4
        m = dict(shared)
        m["xT_in"] = np.ascontiguousarray(x[b].reshape(4, TL, D).transpose(0, 2, 1))
        m["memT"] = np.ascontiguousarray(mem[b].T)
        in_maps.append(m)
    res = run_bass_kernel_spmd(nc, in_maps, core_ids=cores).results
    out = np.empty((B, S, D), np.float32)
    for c in cores:
        b, r = c // 4, c % 4
        out[b, r * TL:(r + 1) * TL, :] = res[c]["outT"][r].T
    return out
```

```python
import numpy as np
import ml_dtypes
import concourse.bass as bass
import concourse.mybir as mybir
from concourse.bass_utils import run_bass_kernel_spmd

F32 = mybir.dt.float32
BF16 = mybir.dt.bfloat16
AF = mybir.ActivationFunctionType
ALU = mybir.AluOpType
AX = mybir.AxisListType
NPBF = ml_dtypes.bfloat16

D = 1024
S = 8192
B = 2
DEPTH = 4
TL = 2048
NCORE = 8
MIXW = 768
INW = 3584
NEG = -30000.0
RMS_EPS = 1e-6
SUBLN_EPS = 1e-5


class Res:
    __slots__ = ("name", "lw", "readers")

    def __init__(self, name=""):
        self.name = name
        self.lw = None
        self.readers = []


class Op:
    __slots__ = ("eng", "fn", "dma", "sem", "semval", "needs_inc", "idx", "waits", "gen")


class Prog:
    ENGS = ("pe", "act", "dve", "pool", "sp")

    def __init__(self, nc, same_sync=("act", "dve", "pool"), ndma=12):
        self.nc = nc
        self.h = dict(pe=nc.tensor, act=nc.scalar, dve=nc.vector, pool=nc.gpsimd, sp=nc.sync)
        self.ops = {e: [] for e in self.ENGS}
        self.obs = {e: {} for e in self.ENGS}
        self.same_sync = set(same_sync)
        self.same_dist = 4
        self.esems = {e: [nc.alloc_semaphore(name=f"es_{e}_0")] for e in ("pe", "act", "dve", "pool")}
        self.gen = {e: 0 for e in ("pe", "act", "dve", "pool", "sp")}
        self.gcount = {e: 0 for e in ("pe", "act", "dve", "pool")}
        self.pending = {e: [] for e in self.ENGS}
        self.ndma = ndma
        self.dsem = {q: [nc.alloc_semaphore(name=f"ds_{q}_{i}") for i in range(ndma)] for q in ("sp", "pool", "act")}
        self.dlast = {q: [None] * ndma for q in ("sp", "pool", "act")}
        self.dcnt = {q: 0 for q in ("sp", "pool", "act")}
        self.all_dma = []

    def add(self, eng, fn, reads=(), writes=(), dma=False):
        op = Op()
        op.eng = eng
        op.fn = fn
        op.dma = dma
        op.needs_inc = False
        op.sem = None
        op.semval = None
        op.idx = len(self.ops[eng])
        op.gen = self.gen[eng]
        deps = []
        if self.pending[eng]:
            deps.extend(self.pending[eng])
            self.pending[eng] = []
        for r in reads:
            if r.lw is not None:
                deps.append(r.lw)
        for w in writes:
            if w.lw is not None:
                deps.append(w.lw)
            deps.extend(w.readers)
        if dma:
            n = self.dcnt[eng]
            slot = n % self.ndma
            prev = self.dlast[eng][slot]
            if prev is not None:
                deps.append(prev)
            self.dlast[eng][slot] = op
            op.sem = self.dsem[eng][slot]
            op.semval = 16 * (n // self.ndma + 1)
            self.dcnt[eng] = n + 1
            self.all_dma.append(op)
        waits = []
        obs = self.obs[eng]
        for p in deps:
            if p is op:
                continue
            if p.dma:
                key = ("d", id(p.sem))
                if obs.get(key, 0) >= p.semval:
                    continue
                obs[key] = p.semval
                waits.append(p)
            else:
                if p.eng == eng and (eng not in self.same_sync or op.idx - p.idx >= self.same_dist):
                    continue
                key = ("e", p.eng)
                if obs.get(key, -1) >= p.idx:
                    continue
                obs[key] = p.idx
                if not p.needs_inc:
                    p.needs_inc = True
                    self.gcount[p.eng] += 1
                waits.append(p)
        op.waits = waits
        for r in reads:
            r.readers.append(op)
        for w in writes:
            w.lw = op
            w.readers = []
        self.ops[eng].append(op)
        return op

    def barrier(self, rotate_at=12000):
        lasts = []
        for e in ("pe", "act", "dve", "pool"):
            for op in reversed(self.ops[e]):
                if not op.dma:
                    if not op.needs_inc:
                        op.needs_inc = True
                        self.gcount[e] += 1
                    lasts.append(op)
                    break
        for q in self.dlast:
            for op in self.dlast[q]:
                if op is not None:
                    lasts.append(op)
        for e in self.ENGS:
            self.pending[e] = list(lasts)
        for e in ("pe", "act", "dve", "pool"):
            if self.gcount[e] > rotate_at:
                self.gen[e] += 1
                self.gcount[e] = 0
                self.esems[e].append(self.nc.alloc_semaphore(name=f"es_{e}_{self.gen[e]}"))

    def dma(self, q, out, in_, reads=(), writes=()):
        return self.add(q, lambda h: h.dma_start(out=out, in_=in_), reads, writes, dma=True)

    def emit(self):
        nc = self.nc
        final_waits = {}
        for op in self.all_dma:
            k = id(op.sem)
            if k not in final_waits or final_waits[k][1] < op.semval:
                final_waits[k] = (op.sem, op.semval)
        for e in ("pe", "act", "dve", "pool"):
            c = {}
            for op in self.ops[e]:
                if op.needs_inc and not op.dma:
                    c[op.gen] = c.get(op.gen, 0) + 1
                    op.sem = self.esems[e][op.gen]
                    op.semval = c[op.gen]
        self.max_semval = {e: sum(1 for o in self.ops[e] if o.needs_inc) for e in ("pe", "act", "dve", "pool")}

        def run(e):
            h = self.h[e]
            for op in self.ops[e]:
                for p in op.waits:
                    h.wait_ge(p.sem, p.semval)
                ins = op.fn(h)
                if op.dma:
                    ins.then_inc(op.sem, 16)
                elif op.needs_inc:
                    ins.then_inc(op.sem, 1)
            if e == "sp":
                for p in self.pending[e]:
                    h.wait_ge(p.sem, p.semval)
                for sem, val in final_waits.values():
                    h.wait_ge(sem, val)

        with nc.Block() as block:
            @block.tensor
            def _(t):
                run("pe")

            @block.scalar
            def _(t):
                run("act")

            @block.vector
            def _(t):
                run("dve")

            @block.gpsimd
            def _(t):
                run("pool")

            @block.sync
            def _(t):
                run("sp")


_LET = "abcdefgh"


def _view(ap2d, shape):
    dims = list(shape[1:])
    if len(dims) > 1:
        names = " ".join(_LET[:len(dims)])
        kw = {_LET[i]: dims[i] for i in range(len(dims))}
        ap2d = ap2d.rearrange(f"p ({names}) -> p {names}", **kw)
    if shape[0] != 128:
        ap2d = ap2d[0:shape[0]]
    return ap2d


class SB:
    def __init__(self, nc, arena_words=None):
        self.nc = nc
        self.n = 0
        self.arena = None
        if arena_words is not None:
            self.arena = nc.alloc_sbuf_tensor("arena", [128, arena_words], F32)
            self.words = arena_words
            self.off = 0
            self.banks = [nc.alloc_psum_tensor(f"bank{i}", [128, 512], F32) for i in range(8)]
            self.pi = 0

    def reset(self, keep=0):
        self.off = keep
        self.pi = 0

    def sb(self, shape, dt, name=None):
        self.n += 1
        if self.arena is None:
            return self.nc.alloc_sbuf_tensor(name or f"sb{self.n}", shape, dt)
        n = int(np.prod(shape[1:]))
        esz = 4 if dt == F32 else 2
        words = (n * esz + 3) // 4
        words = (words + 7) // 8 * 8
        assert self.off + words <= self.words, f"SBUF arena overflow: {self.off}+{words} > {self.words}"
        v = self.arena[:, self.off:self.off + words]
        self.off += words
        if dt != F32:
            v = v.bitcast(dt)
        v = v[:, 0:n]
        return _view(v, shape)

    def ps(self, shape, dt, name=None):
        self.n += 1
        if self.arena is None:
            return self.nc.alloc_psum_tensor(name or f"ps{self.n}", shape, dt)
        assert self.pi < 8, "out of PSUM banks"
        b = self.banks[self.pi]
        self.pi += 1
        n = int(np.prod(shape[1:]))
        return _view(b[:, 0:n], shape)


def unit_cols(kind, g, u):
    if kind in (0, 1):
        hh = 4 * u + g
        return hh * 64, MIXW + hh * 64, 2 * MIXW + hh * 64, 64
    mm = 3 * g + u
    head, m = mm // 2, mm % 2
    return head * 128 + m * 64, MIXW + head * 128 + m * 64, 2 * MIXW + head * 128, 128


def alibi_slopes(n):
    return (2.0 ** (-8.0 * np.arange(1, n + 1) / n)).astype(np.float64)


def phase_a(nc, P, A, kind, xT_res, xT_sb, wA, gcol_d, send_qk, send_v, gateT_d, qmT_d, load_x_from=None):
    vd = 128 if kind == 2 else 64
    NG = TL // 512
    ones_bf = A.sb([128, 128], BF16)
    r_ones = Res()
    P.add("pool", lambda h: h.memset(ones_bf[:], 1.0), writes=[r_ones])
    gcol = A.sb([128, 8], F32)
    r_g = Res()
    P.dma("sp", gcol[:], gcol_d, writes=[r_g])
    if load_x_from is not None:
        xv = load_x_from.rearrange("(c p) t -> p c t", p=128)
        for G in range(NG):
            P.dma("sp" if G % 2 == 0 else "pool", xT_sb[:, :, G * 512:(G + 1) * 512], xv[:, :, G * 512:(G + 1) * 512],
                  writes=[xT_res[G]])

    hT = A.sb([128, 8, TL], BF16)
    r_h = [Res() for _ in range(NG)]
    sq = [A.sb([128, 512], BF16) for _ in range(2)]
    r_sq = [Res() for _ in range(2)]
    ss_ps = A.ps([128, 512], F32)
    r_ss = Res()
    rstd = A.sb([128, 512], F32)
    r_rstd = Res()
    i = 0
    for G in range(NG):
        gs = slice(G * 512, (G + 1) * 512)
        for c in range(8):
            b = i % 2
            i += 1
            P.add("act", lambda h, b=b, c=c, gs=gs: h.activation(out=sq[b][:], in_=xT_sb[:, c, gs], func=AF.Square),
                  reads=[xT_res[G]], writes=[r_sq[b]])
            P.add("pe", lambda h, b=b, c=c: h.matmul(ss_ps[:], ones_bf[:], sq[b][:], start=(c == 0), stop=(c == 7)),
                  reads=[r_sq[b], r_ones], writes=[r_ss])
        P.add("act", lambda h: h.activation(out=rstd[:], in_=ss_ps[:], func=AF.Sqrt, scale=1.0 / D, bias=RMS_EPS),
              reads=[r_ss], writes=[r_rstd])
        P.add("dve", lambda h: h.reciprocal(out=rstd[:], in_=rstd[:]), reads=[r_rstd], writes=[r_rstd])
        for c in range(8):
            P.add("dve", lambda h, c=c, gs=gs: h.tensor_tensor(out=hT[:, c, gs], in0=xT_sb[:, c, gs], in1=rstd[:], op=ALU.mult),
                  reads=[xT_res[G], r_rstd], writes=[r_h[G]])

    CG = 256
    ncg = INW // CG
    wst = [A.sb([128, 8, CG], F32) for _ in range(2)]
    r_wst = [Res() for _ in range(2)]
    wb = [A.sb([128, 8, CG], BF16) for _ in range(2)]
    r_wb = [Res() for _ in range(2)]
    wv = wA.rearrange("(c p) n -> p c n", p=128)
    ev = [A.sb([128, TL], BF16) for _ in range(2)]
    r_ev = [Res() for _ in range(2)]
    vsb = [A.sb([128, 16, CG], BF16) for _ in range(2)]
    r_vsb = [Res() for _ in range(2)]
    pacc = [A.ps([128, 512], F32) for _ in range(4)]
    r_pacc = [Res() for _ in range(4)]
    pi = 0
    evi = 0
    vi = 0
    dest = {}
    for g in range(4):
        for u in range(3):
            qc, kc, vc, _ = unit_cols(kind, g, u)
            dest[qc] = (g, 0, u)
            dest[kc] = (g, 1, u)
    vdest = {}
    for g in range(4):
        for u in range(3):
            qc, kc, vc, _ = unit_cols(kind, g, u)
            vdest.setdefault(vc, []).append((g, u))
    for cg in range(ncg):
        b = cg % 2
        c0 = cg * CG
        P.dma("sp" if cg % 2 == 0 else "pool", wst[b][:], wv[:, :, c0:c0 + CG], writes=[r_wst[b]])
        for c in range(8):
            if c % 2 == 0:
                P.add("dve", lambda h, b=b, c=c: h.tensor_scalar(out=wb[b][:, c, :], in0=wst[b][:, c, :], scalar1=gcol[:, c:c + 1],
                                                                  scalar2=None, op0=ALU.mult),
                      reads=[r_wst[b], r_g], writes=[r_wb[b]])
            else:
                P.add("act", lambda h, b=b, c=c: h.activation(out=wb[b][:, c, :], in_=wst[b][:, c, :], func=AF.Copy, scale=gcol[:, c:c + 1]),
                      reads=[r_wst[b], r_g], writes=[r_wb[b]])
        if 2 * MIXW <= c0 < 3 * MIXW:
            vb = vi % 2
            vi += 1
            for tt in range(16):
                pb = pi % 4
                pi += 1
                for c in range(8):
                    P.add("pe", lambda h, pb=pb, c=c, tt=tt, b=b: h.matmul(pacc[pb][:, 0:CG], hT[:, c, tt * 128:(tt + 1) * 128],
                                                                          wb[b][:, c, :], start=(c == 0), stop=(c == 7)),
                          reads=[r_h[tt // 4], r_wb[b]], writes=[r_pacc[pb]])
                eng = "act" if tt % 2 == 0 else "dve"
                if eng == "act":
                    P.add("act", lambda h, pb=pb, vb=vb, tt=tt: h.activation(out=vsb[vb][:, tt, :], in_=pacc[pb][:, 0:CG], func=AF.Copy),
                          reads=[r_pacc[pb]], writes=[r_vsb[vb]])
                else:
                    P.add("dve", lambda h, pb=pb, vb=vb, tt=tt: h.tensor_copy(out=vsb[vb][:, tt, :], in_=pacc[pb][:, 0:CG]),
                          reads=[r_pacc[pb]], writes=[r_vsb[vb]])
            for off in range(0, CG, vd):
                col = c0 + off
                for (g, u) in vdest.get(col, []):
                    dst = send_v[g, u].rearrange("(t p) v -> p t v", p=128)
                    P.dma("sp", dst, vsb[vb][:, :, off:off + vd], reads=[r_vsb[vb]])
            continue
        for ch in range(CG // 128):
            col = c0 + ch * 128
            isq = col < MIXW or (3 * MIXW <= col < 3 * MIXW + 256)
            eb = evi % 2
            evi += 1
            for G in range(NG):
                gs = slice(G * 512, (G + 1) * 512)
                pb = pi % 4
                pi += 1
                for c in range(8):
                    P.add("pe", lambda h, pb=pb, c=c, gs=gs, b=b, ch=ch: h.matmul(pacc[pb][:], wb[b][:, c, ch * 128:(ch + 1) * 128],
                                                                                  hT[:, c, gs], start=(c == 0), stop=(c == 7)),
                          reads=[r_h[G], r_wb[b]], writes=[r_pacc[pb]])
                sc = 0.125 if isq else 1.0
                if G % 2 == 0:
                    P.add("act", lambda h, pb=pb, eb=eb, gs=gs, sc=sc: h.activation(out=ev[eb][:, gs], in_=pacc[pb][:], func=AF.Copy, scale=sc),
                          reads=[r_pacc[pb]], writes=[r_ev[eb]])
                else:
                    P.add("dve", lambda h, pb=pb, eb=eb, gs=gs, sc=sc: h.tensor_scalar(out=ev[eb][:, gs], in0=pacc[pb][:], scalar1=sc, scalar2=None, op0=ALU.mult),
                          reads=[r_pacc[pb]], writes=[r_ev[eb]])
            if col < 2 * MIXW:
                for half in range(2):
                    g, qk, u = dest[col + half * 64]
                    P.dma("sp", send_qk[g, qk, u], ev[eb][half * 64:(half + 1) * 64, :], reads=[r_ev[eb]])
            elif col < 3 * MIXW + 256:
                r0 = col - 3 * MIXW
                P.dma("sp", qmT_d[r0:r0 + 128, :], ev[eb][:], reads=[r_ev[eb]])
            else:
                r0 = col - (3 * MIXW + 256)
                P.dma("sp", gateT_d[r0:r0 + 128, :], ev[eb][:], reads=[r_ev[eb]])


def build_phase_a_prog(kind):
    nc = bass.Bass("TRN2", target_bir_lowering=False)
    vd = 128 if kind == 2 else 64
    xT_d = nc.dram_tensor("xT", [D, TL], F32, kind="ExternalInput").ap()
    w_d = nc.dram_tensor("w_in", [D, INW], F32, kind="ExternalInput").ap()
    g_d = nc.dram_tensor("gcol", [128, 8], F32, kind="ExternalInput").ap()
    send_qk = nc.dram_tensor("send_qk", [4, 2, 3, 64, TL], BF16, kind="ExternalOutput").ap()
    send_v = nc.dram_tensor("send_v", [4, 3, TL, vd], BF16, kind="ExternalOutput").ap()
    gateT = nc.dram_tensor("gateT", [D, TL], BF16, kind="ExternalOutput").ap()
    qmT = nc.dram_tensor("qmT", [256, TL], BF16, kind="ExternalOutput").ap()
    P = Prog(nc)
    A = SB(nc)
    xT_sb = A.sb([128, 8, TL], F32)
    xres = [Res() for _ in range(TL // 512)]
    phase_a(nc, P, A, kind, xres, xT_sb, w_d, g_d, send_qk, send_v, gateT, qmT, load_x_from=xT_d)
    P.emit()
    return nc, P


DSW = ((128, 1), (512, 4), (2048, 16))


def unit_slope(kind, g, u):
    if kind in (0, 1):
        return alibi_slopes(12)[4 * u + g]
    return alibi_slopes(6)[(3 * g + u) // 2]


def split3(x):
    hi = x.astype(NPBF)
    r = x - hi.astype(np.float64)
    lo = r.astype(NPBF)
    r2 = r - lo.astype(np.float64)
    lo2 = r2.astype(NPBF)
    return hi, lo, lo2


def phase_b_consts(kind, g):
    c = {}
    c["ident"] = np.eye(128, dtype=np.float32).astype(NPBF)
    sel = np.zeros((128, 64), np.float32)
    sel[64, :] = 1.0
    c["sel"] = sel
    t = np.arange(S, dtype=np.float64)
    qrow = np.zeros((3, 3, S), dtype=NPBF)
    biast = np.zeros((128, 3, 64), dtype=np.float32)
    p = np.arange(128, dtype=np.float64)
    for u in range(3):
        sl = unit_slope(kind, g, u)
        x = -sl * (t - (t // 512) * 512)
        hi, lo, lo2 = split3(x)
        qrow[u, 0], qrow[u, 1], qrow[u, 2] = hi, lo, lo2
        for j in range(64):
            jp = 3 - j
            biast[:, u, j] = (sl * (128.0 * jp + p)).astype(np.float32)
    c["qrow"] = qrow
    c["biast"] = biast
    kones = np.zeros((35, S), dtype=np.float32)
    for b in range(32):
        kones[b, b * 256:(b + 1) * 256] = 1.0
    kones[32:35] = 1.0
    c["kones"] = kones.astype(NPBF)
    q = np.arange(512)[None, :]
    if kind == 0:
        tiles = []
        for (w, d) in DSW:
            for jp in range(-w // 128, 4):
                tk = 128 * jp + np.arange(128)[:, None]
                dist = q - tk
                valid = (dist >= 0) & (dist <= w) & (dist % d == 0)
                tiles.append(np.where(valid, 0.0, NEG))
        m = np.stack(tiles, axis=1)
    else:
        tiles = []
        for i in range(4):
            tk = 128 * i + np.arange(128)[:, None]
            tiles.append(np.where(tk > q, NEG, 0.0))
        m = np.stack(tiles, axis=1)
    c["masks"] = m.astype(np.float32).astype(NPBF)
    return c


def phase_b(nc, P, A, kind, recv_qk, recv_v, cd, oT_d):
    vd = 128 if kind == 2 else 64
    VW = vd + 1
    arow = 96 if kind == 1 else 64
    KA = arow + 3
    NGq = S // 512
    nmask = 33 if kind == 0 else 4
    ident = A.sb([128, 128], BF16)
    r_id = Res()
    P.dma("sp", ident[:], cd["ident"], writes=[r_id])
    biast = A.sb([128, 3, 64], F32)
    r_bt = Res()
    P.dma("sp", biast[:], cd["biast"], writes=[r_bt])
    masks = A.sb([128, nmask, 512], BF16)
    r_mk = Res()
    P.dma("pool", masks[:], cd["masks"], writes=[r_mk])
    ones_f = A.sb([128, 64], F32)
    r_of = Res()
    P.dma("sp", ones_f[:], cd["sel"], writes=[r_of])

    nunit_res = 3 if kind == 0 else 2
    ka = [A.sb([128, S], BF16) for _ in range(nunit_res)]
    r_ka = [Res() for _ in range(nunit_res)]
    va = [A.sb([128, 64, VW], BF16) for _ in range(nunit_res)]
    r_va = [Res() for _ in range(nunit_res)]
    for i in range(nunit_res):
        P.add("pool", lambda h, i=i: h.memset(va[i][:, :, 64:65], 1.0), writes=[r_va[i]])
    NQB = 4
    qa = [A.sb([128, 512], BF16) for _ in range(NQB)]
    r_qa = [Res() for _ in range(NQB)]
    NS = 3
    s_ps = [A.ps([128, 512], F32) for _ in range(NS)]
    r_s = [Res() for _ in range(NS)]
    NPT = 4
    pt = [A.sb([128, 512], BF16) for _ in range(NPT)]
    r_pt = [Res() for _ in range(NPT)]
    if kind == 2:
        oa_ps = [A.ps([128, 512], F32) for _ in range(2)]
        ob_ps = [A.ps([128, 512], F32) for _ in range(2)]
        r_oa = [Res() for _ in range(2)]
        r_ob = [Res() for _ in range(2)]
    elif kind == 0:
        oa_ps = [A.ps([128, 512], F32) for _ in range(3)]
        r_oa = [Res() for _ in range(3)]
    else:
        oa_ps = [A.ps([128, 512], F32) for _ in range(2)]
        r_oa = [Res() for _ in range(2)]
    bc_ps = A.ps([128, 512], F32)
    r_bc = Res()
    rd = A.sb([128, 512], F32)
    r_rd = Res()
    P.add("pool", lambda h: h.memset(rd[:], 0.0), writes=[r_rd])
    osb = [A.sb([64, 512], F32) for _ in range(2)]
    r_osb = [Res() for _ in range(2)]
    outb = [A.sb([64, 512], BF16) for _ in range(4)]
    r_outb = [Res() for _ in range(4)]
    if kind == 1:
        gate_ps = A.ps([128, 4, 32], F32)
        r_gate = Res()
        tr_ps = A.ps([128, 512], F32)
        r_tr = Res()
        gm = A.sb([128, 4, 32], F32)
        r_gm = Res()
        P.add("pool", lambda h: h.memset(gm[:], -1e30), writes=[r_gm])
        mx8 = A.sb([128, 4, 8], F32)
        r_mx = Res()
        negm = A.sb([128, 4, 96], BF16)
        r_negm = Res()
        P.add("pool", lambda h: h.memset(negm[:], 0.0), writes=[r_negm])
        kms = A.sb([64, 32], F32)
        kmb = A.sb([64, 32], BF16)
        r_km = Res()

    cnt = dict(q=0, s=0, pt=0, o=0, osb=0, outb=0)

    def load_unit(u, slot):
        for src in range(4):
            P.dma("sp", ka[slot][0:64, src * TL:(src + 1) * TL], recv_qk[src, 1, u], writes=[r_ka[slot]])
        if kind == 1:
            P.dma("pool", ka[slot][64:99, :], cd["kones"], writes=[r_ka[slot]])
        else:
            P.dma("pool", ka[slot][64:67, :], cd["kones"][32:35, :], writes=[r_ka[slot]])
        for src in range(4):
            sv = recv_v[src, u].rearrange("(t p) v -> p t v", p=128)
            if vd == 64:
                P.dma("pool", va[slot][:, src * 16:(src + 1) * 16, 0:64], sv, writes=[r_va[slot]])
            else:
                P.dma("pool", va[slot][:, src * 16:(src + 1) * 16, 0:64], sv[:, :, 0:64], writes=[r_va[slot]])
                P.dma("pool", va[slot][:, src * 16:(src + 1) * 16, 65:129], sv[:, :, 64:128], writes=[r_va[slot]])

    def load_q(u, G):
        qb = cnt["q"] % NQB
        cnt["q"] += 1
        src, lg = G // 4, G % 4
        P.dma("sp", qa[qb][0:64, :], recv_qk[src, 0, u, :, lg * 512:(lg + 1) * 512], writes=[r_qa[qb]])
        P.dma("sp", qa[qb][arow:arow + 3, :], cd["qrow"][u, :, G * 512:(G + 1) * 512], writes=[r_qa[qb]])
        return qb

    def moba_prep(slot, qb, G):
        for i in range(4):
            P.add("pe", lambda h, i=i: h.matmul(gate_ps[:, i, :], qa[qb][0:64, i * 128:(i + 1) * 128], kmb[:, :], start=True, stop=True),
                  reads=[r_qa[qb], r_km], writes=[r_gate])
        for i in range(4):
            n = (4 * G + i) // 2
            if n > 0:
                P.add("dve", lambda h, i=i, n=n: h.tensor_copy(out=gm[:, i, 0:n], in_=gate_ps[:, i, 0:n]), reads=[r_gate], writes=[r_gm])
            P.add("dve", lambda h, i=i: h.max(out=mx8[:, i, :], in_=gm[:, i, :]), reads=[r_gm], writes=[r_mx])
            P.add("dve", lambda h, i=i: h.tensor_scalar(out=negm[:, i, 64:96], in0=gm[:, i, :], scalar1=mx8[:, i, 2:3], scalar2=1.0,
                                                        op0=ALU.is_ge, op1=ALU.subtract), reads=[r_gm, r_mx], writes=[r_negm])
            P.add("dve", lambda h, i=i, n=n: h.memset(negm[:, i, 64 + n:65 + n], 0.0), writes=[r_negm])

    def moba_prep2(slot, qb, G):
        for i in range(4):
            P.add("pe", lambda h, i=i: h.matmul(tr_ps[0:96, i * 128:(i + 1) * 128], negm[:, i, :], ident[:], start=True, stop=True),
                  reads=[r_negm, r_id], writes=[r_tr])
        P.add("act", lambda h: h.activation(out=qa[qb][64:96, :], in_=tr_ps[64:96, :], func=AF.Copy, scale=-NEG),
              reads=[r_tr], writes=[r_qa[qb]])

    def steps_for(u, G):
        out = []
        if kind == 0:
            w, d = DSW[u]
            nb = w // 128
            moff = [0, 5, 13][u]
            for jp in range(-nb, 4):
                kt = 4 * G + jp
                if kt < 0:
                    continue
                out.append((kt, moff + jp + nb, 3 - jp))
        else:
            for kt in range(0, 4 * G + 4):
                jp = kt - 4 * G
                out.append((kt, jp if jp >= 0 else None, 3 - jp))
        return out

    def attend(u, slot, G, qb, ob):
        steps = steps_for(u, G)
        n = len(steps)
        sb_of = {}
        pt_of = {}

        def qk(i):
            kt, mi, bj = steps[i]
            sbk = cnt["s"] % NS
            cnt["s"] += 1
            sb_of[i] = sbk
            P.add("pe", lambda h: h.matmul(s_ps[sbk][:], ka[slot][0:KA, kt * 128:(kt + 1) * 128], qa[qb][0:KA, :], start=True, stop=(mi is None)),
                  reads=[r_ka[slot], r_qa[qb]], writes=[r_s[sbk]])
            if mi is not None:
                P.add("pe", lambda h: h.matmul(s_ps[sbk][:], ident[:], masks[:, mi, :], start=False, stop=True),
                      reads=[r_id, r_mk], writes=[r_s[sbk]])

        def ex(i):
            kt, mi, bj = steps[i]
            sbk = sb_of[i]
            pb = cnt["pt"] % NPT
            cnt["pt"] += 1
            pt_of[i] = pb
            P.add("act", lambda h: h.activation(out=pt[pb][:], in_=s_ps[sbk][:], func=AF.Exp, bias=biast[:, u, bj:bj + 1], scale=1.0),
                  reads=[r_s[sbk], r_bt], writes=[r_pt[pb]])

        def pv(i):
            kt, mi, bj = steps[i]
            pb = pt_of[i]
            P.add("pe", lambda h: h.matmul(oa_ps[ob][0:65, :], va[slot][:, kt, 0:65], pt[pb][:], start=(i == 0), stop=(i == n - 1)),
                  reads=[r_va[slot], r_pt[pb]], writes=[r_oa[ob]])
            if kind == 2:
                P.add("pe", lambda h: h.matmul(ob_ps[ob][0:64, :], va[slot][:, kt, 65:129], pt[pb][:], start=(i == 0), stop=(i == n - 1)),
                      reads=[r_va[slot], r_pt[pb]], writes=[r_ob[ob]])

        LA = 2
        for i in range(min(LA, n)):
            qk(i)
            ex(i)
        for i in range(n):
            if i + LA < n:
                qk(i + LA)
                ex(i + LA)
            pv(i)

    def finish(u, G, obs):
        gs = slice(G * 512, (G + 1) * 512)
        first = True
        for (uu, ob) in obs:
            if first:
                P.add("dve", lambda h, ob=ob: h.tensor_copy(out=rd[64:65, :], in_=oa_ps[ob][64:65, :]), reads=[r_oa[ob]], writes=[r_rd])
                first = False
            else:
                P.add("dve", lambda h, ob=ob: h.tensor_tensor(out=rd[64:65, :], in0=rd[64:65, :], in1=oa_ps[ob][64:65, :], op=ALU.add),
                      reads=[r_oa[ob], r_rd], writes=[r_rd])
        P.add("dve", lambda h: h.reciprocal(out=rd[64:65, :], in_=rd[64:65, :]), reads=[r_rd], writes=[r_rd])

    def finish2(u, G, obs):
        gs = slice(G * 512, (G + 1) * 512)
        P.add("pe", lambda h: h.matmul(bc_ps[0:64, :], ones_f[:, :], rd[:, :], start=True, stop=True),
              reads=[r_of, r_rd], writes=[r_bc])
        for (uu, ob) in obs:
            parts = [(oa_ps, r_oa, 0)] + ([(ob_ps, r_ob, 64)] if kind == 2 else [])
            for (pst, rr, row0) in parts:
                sb = cnt["osb"] % 2
                cnt["osb"] += 1
                bb = cnt["outb"] % 4
                cnt["outb"] += 1
                P.add("act", lambda h, pst=pst, ob=ob, sb=sb: h.activation(out=osb[sb][:], in_=pst[ob][0:64, :], func=AF.Copy),
                      reads=[rr[ob]], writes=[r_osb[sb]])
                P.add("dve", lambda h, sb=sb, bb=bb: h.tensor_tensor(out=outb[bb][:], in0=osb[sb][:], in1=bc_ps[0:64, :], op=ALU.mult),
                      reads=[r_osb[sb], r_bc], writes=[r_outb[bb]])
                P.dma("sp", oT_d[uu, row0:row0 + 64, gs], outb[bb][:], reads=[r_outb[bb]])

    if kind == 0:
        for u in range(3):
            load_unit(u, u)
        for G in range(NGq):
            obs = []
            for u in range(3):
                qb = load_q(u, G)
                attend(u, u, G, qb, u)
                obs.append((u, u))
            finish(0, G, obs)
            finish2(0, G, obs)
    else:
        load_unit(0, 0)
        for u in range(3):
            slot = u % 2
            if u + 1 < 3:
                load_unit(u + 1, (u + 1) % 2)
            if kind == 1:
                P.add("dve", lambda h, slot=slot: h.tensor_reduce(out=kms[:, :], in_=ka[slot][0:64, :].rearrange("p (b k) -> p b k", k=256),
                                                                   axis=AX.X, op=ALU.add), reads=[r_ka[slot]], writes=[r_km])
                P.add("dve", lambda h: h.tensor_copy(out=kmb[:, :], in_=kms[:, :]), reads=[r_km], writes=[r_km])
                if u > 0:
                    P.add("pool", lambda h: h.memset(gm[:], -1e30), writes=[r_gm])
            qbs = {0: load_q(u, 0)}
            if kind == 1:
                moba_prep(slot, qbs[0], 0)
                moba_prep2(slot, qbs[0], 0)
            pend = None
            for G in range(NGq):
                if G + 1 < NGq:
                    qbs[G + 1] = load_q(u, G + 1)
                    if kind == 1:
                        moba_prep(slot, qbs[G + 1], G + 1)
                ob = cnt["o"] % 2
                cnt["o"] += 1
                attend(u, slot, G, qbs[G], ob)
                if pend is not None:
                    finish2(*pend)
                if kind == 1 and G + 1 < NGq:
                    moba_prep2(slot, qbs[G + 1], G + 1)
                finish(u, G, [(u, ob)])
                pend = (u, G, [(u, ob)])
            finish2(*pend)


def build_phase_b_prog(kind):
    nc = bass.Bass("TRN2", target_bir_lowering=False)
    vd = 128 if kind == 2 else 64
    recv_qk = nc.dram_tensor("recv_qk", [4, 2, 3, 64, TL], BF16, kind="ExternalInput").ap()
    recv_v = nc.dram_tensor("recv_v", [4, 3, TL, vd], BF16, kind="ExternalInput").ap()
    nmask = 33 if kind == 0 else 4
    cd = dict(
        ident=nc.dram_tensor("ident", [128, 128], BF16, kind="ExternalInput").ap(),
        sel=nc.dram_tensor("sel", [128, 64], F32, kind="ExternalInput").ap(),
        qrow=nc.dram_tensor("qrow", [3, 3, S], BF16, kind="ExternalInput").ap(),
        biast=nc.dram_tensor("biast", [128, 3, 64], F32, kind="ExternalInput").ap(),
        kones=nc.dram_tensor("kones", [35, S], BF16, kind="ExternalInput").ap(),
        masks=nc.dram_tensor("masks", [128, nmask, 512], BF16, kind="ExternalInput").ap(),
    )
    oT = nc.dram_tensor("oT", [3, vd, S], BF16, kind="ExternalOutput").ap()
    P = Prog(nc)
    A = SB(nc)
    phase_b(nc, P, A, kind, recv_qk, recv_v, cd, oT)
    P.emit()
    return nc, P


def phase_c(nc, P, A, kind, layer, xT_res, xT_sb, recv_o, gateT_d, qmT_d, memT_d, mgcol_d, wkv_d, wout_d, sel_d,
            diffp=None, final=None, xT_out=None, load_x_from=None):
    vd = 128 if kind == 2 else 64
    NG = TL // 512
    lam_init = 0.8 - 0.6 * float(np.exp(-0.3 * layer))
    if load_x_from is not None:
        xv = load_x_from.rearrange("(c p) t -> p c t", p=128)
        for G in range(NG):
            P.dma("pool", xT_sb[:, :, G * 512:(G + 1) * 512], xv[:, :, G * 512:(G + 1) * 512], writes=[xT_res[G]])
    ones_bf = A.sb([128, 128], BF16)
    r_ones = Res()
    P.add("pool", lambda h: h.memset(ones_bf[:], 1.0), writes=[r_ones])
    sel = A.sb([128, 64], F32)
    r_sel = Res()
    P.dma("sp", sel[:], sel_d, writes=[r_sel])
    mgcol = A.sb([128, 8], F32)
    r_mg = Res()
    P.dma("sp", mgcol[:], mgcol_d, writes=[r_mg])

    NPS = 6
    ps = [A.ps([128, 512], F32) for _ in range(NPS)]
    r_ps = [Res() for _ in range(NPS)]
    cnt = dict(ps=0, pt=0, st=0)

    def nps():
        i = cnt["ps"] % NPS
        cnt["ps"] += 1
        return i

    memT = A.sb([128, 8, 256], F32)
    r_mem = Res()
    P.dma("sp", memT[:], memT_d.rearrange("(c p) t -> p c t", p=128), writes=[r_mem])
    sqm = [A.sb([128, 512], BF16) for _ in range(2)]
    r_sqm = [Res() for _ in range(2)]
    rstd = A.sb([128, 512], F32)
    r_rstd = Res()
    mnT = A.sb([128, 8, 256], BF16)
    r_mn = Res()
    pb = nps()
    for c in range(8):
        b = c % 2
        P.add("act", lambda h, b=b, c=c: h.activation(out=sqm[b][:, 0:256], in_=memT[:, c, :], func=AF.Square), reads=[r_mem], writes=[r_sqm[b]])
        P.add("pe", lambda h, b=b, c=c: h.matmul(ps[pb][:, 0:256], ones_bf[:], sqm[b][:, 0:256], start=(c == 0), stop=(c == 7)),
              reads=[r_sqm[b], r_ones], writes=[r_ps[pb]])
    P.add("act", lambda h: h.activation(out=rstd[:, 0:256], in_=ps[pb][:, 0:256], func=AF.Sqrt, scale=1.0 / D, bias=RMS_EPS), reads=[r_ps[pb]], writes=[r_rstd])
    P.add("dve", lambda h: h.reciprocal(out=rstd[:, 0:256], in_=rstd[:, 0:256]), reads=[r_rstd], writes=[r_rstd])
    for c in range(8):
        P.add("dve", lambda h, c=c: h.tensor_tensor(out=mnT[:, c, :], in0=memT[:, c, :], in1=rstd[:, 0:256], op=ALU.mult), reads=[r_mem, r_rstd], writes=[r_mn])
    WS = 128
    wst = [A.sb([128, 8, WS], F32) for _ in range(2)]
    r_wst = [Res() for _ in range(2)]
    wkv = A.sb([128, 8, 512], BF16)
    r_wkv = Res()
    wkv_v = wkv_d.rearrange("(c p) n -> p c n", p=128)
    for j in range(512 // WS):
        b = cnt["st"] % 2
        cnt["st"] += 1
        P.dma("sp", wst[b][:], wkv_v[:, :, j * WS:(j + 1) * WS], writes=[r_wst[b]])
        for c in range(8):
            if c % 2 == 0:
                P.add("dve", lambda h, b=b, c=c, j=j: h.tensor_scalar(out=wkv[:, c, j * WS:(j + 1) * WS], in0=wst[b][:, c, :], scalar1=mgcol[:, c:c + 1],
                                                                       scalar2=None, op0=ALU.mult), reads=[r_wst[b], r_mg], writes=[r_wkv])
            else:
                P.add("act", lambda h, b=b, c=c, j=j: h.activation(out=wkv[:, c, j * WS:(j + 1) * WS], in_=wst[b][:, c, :], func=AF.Copy, scale=mgcol[:, c:c + 1]),
                      reads=[r_wst[b], r_mg], writes=[r_wkv])
    kmT = A.sb([128, 2, 256], BF16)
    r_kmT = Res()
    for ch in range(2):
        pb = nps()
        for c in range(8):
            P.add("pe", lambda h, pb=pb, c=c, ch=ch: h.matmul(ps[pb][:, 0:256], wkv[:, c, ch * 128:(ch + 1) * 128], mnT[:, c, :], start=(c == 0), stop=(c == 7)),
                  reads=[r_wkv, r_mn], writes=[r_ps[pb]])
        P.add("act", lambda h, pb=pb, ch=ch: h.activation(out=kmT[:, ch, :], in_=ps[pb][:, 0:256], func=AF.Copy), reads=[r_ps[pb]], writes=[r_kmT])
    vma = A.sb([128, 2, 4, 65], BF16)
    r_vma = Res()
    P.add("pool", lambda h: h.memset(vma[:], 1.0), writes=[r_vma])
    for mt in range(2):
        pb = nps()
        for c in range(8):
            P.add("pe", lambda h, pb=pb, c=c, mt=mt: h.matmul(ps[pb][:, 0:256], mnT[:, c, mt * 128:(mt + 1) * 128], wkv[:, c, 256:512], start=(c == 0), stop=(c == 7)),
                  reads=[r_wkv, r_mn], writes=[r_ps[pb]])
        P.add("dve", lambda h, pb=pb, mt=mt: h.tensor_copy(out=vma[:, mt, :, 0:64], in_=ps[pb][:, 0:256].rearrange("p (h d) -> p h d", d=64)),
              reads=[r_ps[pb]], writes=[r_vma])

    qm = A.sb([128, 2, TL], BF16)
    r_qm = Res()
    P.dma("sp", qm[:], qmT_d.rearrange("(c p) t -> p c t", p=128), writes=[r_qm])
    yT = A.sb([128, 8, TL], BF16)
    r_y = [[Res() for _ in range(NG)] for _ in range(8)]
    pt = [A.sb([128, 512], BF16) for _ in range(3)]
    r_pt = [Res() for _ in range(3)]
    rd = A.sb([128, 512], F32)
    r_rd = Res()
    P.add("pool", lambda h: h.memset(rd[:], 0.0), writes=[r_rd])
    osb = [A.sb([64, 512], F32) for _ in range(2)]
    r_osb = [Res() for _ in range(2)]
    mo = [A.sb([64, 512], BF16) for _ in range(2)]
    r_mo = [Res() for _ in range(2)]
    k = 0
    for G in range(NG):
        gs = slice(G * 512, (G + 1) * 512)
        for hm in range(4):
            ch, r0 = hm // 2, (hm % 2) * 64
            po = nps()
            for mt in range(2):
                pb = nps()
                P.add("pe", lambda h, pb=pb, ch=ch, r0=r0, mt=mt, gs=gs: h.matmul(ps[pb][:], kmT[r0:r0 + 64, ch, mt * 128:(mt + 1) * 128], qm[r0:r0 + 64, ch, gs],
                                                                                 start=True, stop=True), reads=[r_kmT, r_qm], writes=[r_ps[pb]])
                pi = cnt["pt"] % 3
                cnt["pt"] += 1
                P.add("act", lambda h, pb=pb, pi=pi: h.activation(out=pt[pi][:], in_=ps[pb][:], func=AF.Exp), reads=[r_ps[pb]], writes=[r_pt[pi]])
                P.add("pe", lambda h, po=po, pi=pi, mt=mt, hm=hm: h.matmul(ps[po][0:65, :], vma[:, mt, hm, :], pt[pi][:], start=(mt == 0), stop=(mt == 1)),
                      reads=[r_vma, r_pt[pi]], writes=[r_ps[po]])
            P.add("dve", lambda h, po=po: h.reciprocal(out=rd[64:65, :], in_=ps[po][64:65, :]), reads=[r_ps[po], r_rd], writes=[r_rd])
            pbc = nps()
            P.add("pe", lambda h, pbc=pbc: h.matmul(ps[pbc][0:64, :], sel[:, :], rd[:, :], start=True, stop=True), reads=[r_sel, r_rd], writes=[r_ps[pbc]])
            sb = k % 2
            mb = k % 2
            k += 1
            P.add("act", lambda h, po=po, sb=sb: h.activation(out=osb[sb][:], in_=ps[po][0:64, :], func=AF.Copy), reads=[r_ps[po]], writes=[r_osb[sb]])
            P.add("dve", lambda h, sb=sb, mb=mb, pbc=pbc: h.tensor_tensor(out=mo[mb][:], in0=osb[sb][:], in1=ps[pbc][0:64, :], op=ALU.mult),
                  reads=[r_osb[sb], r_ps[pbc]], writes=[r_mo[mb]])
            P.dma("pool", yT[r0:r0 + 64, 6 + ch, gs], mo[mb][:], reads=[r_mo[mb]], writes=[r_y[6 + ch][G]])

    if kind in (0, 1):
        for c in range(6):
            for half in range(2):
                hh = 2 * c + half
                g, u = hh % 4, hh // 4
                P.dma("sp", yT[half * 64:(half + 1) * 64, c, :], recv_o[g, u], writes=[r_y[c][G] for G in range(NG)])
    else:
        lp = A.sb([128, 4, 64], F32)
        r_lp = Res()
        for i, nm in enumerate(("lq1", "lk1", "lq2", "lk2")):
            P.dma("sp", lp[:, i, :], diffp[nm][0].partition_broadcast(128), writes=[r_lp])
        sgc = A.sb([128, 1], F32)
        r_sg = Res()
        P.dma("sp", sgc[:], diffp["sg"], writes=[r_sg])
        lpr = A.sb([128, 2, 64], F32)
        lsum = A.sb([128, 2], F32)
        nlam = A.sb([128, 1], F32)
        r_lam = Res()
        P.add("dve", lambda h: h.tensor_tensor(out=lpr[:, 0, :], in0=lp[:, 0, :], in1=lp[:, 1, :], op=ALU.mult), reads=[r_lp], writes=[r_lam])
        P.add("dve", lambda h: h.tensor_tensor(out=lpr[:, 1, :], in0=lp[:, 2, :], in1=lp[:, 3, :], op=ALU.mult), reads=[r_lp, r_lam], writes=[r_lam])
        P.add("dve", lambda h: h.tensor_reduce(out=lsum[:, :], in_=lpr[:, :, :], axis=AX.X, op=ALU.add), reads=[r_lam], writes=[r_lam])
        P.add("act", lambda h: h.activation(out=lsum[:, :], in_=lsum[:, :], func=AF.Exp), reads=[r_lam], writes=[r_lam])
        P.add("dve", lambda h: h.tensor_tensor(out=nlam[:, :], in0=lsum[:, 1:2], in1=lsum[:, 0:1], op=ALU.subtract), reads=[r_lam], writes=[r_lam])
        P.add("dve", lambda h: h.tensor_scalar(out=nlam[:, :], in0=nlam[:, :], scalar1=-lam_init, scalar2=None, op0=ALU.add), reads=[r_lam], writes=[r_lam])
        P.add("dve", lambda h: h.tensor_scalar(out=sgc[:, :], in0=sgc[:, :], scalar1=(1.0 - lam_init), scalar2=None, op0=ALU.mult), reads=[r_sg], writes=[r_sg])
        m12 = [A.sb([128, 2, 512], BF16) for _ in range(2)]
        r_m12 = [Res() for _ in range(2)]
        od = [A.sb([128, 512], F32) for _ in range(2)]
        r_od = [Res() for _ in range(2)]
        sq2 = [A.sb([128, 512], BF16) for _ in range(2)]
        r_sq2 = [Res() for _ in range(2)]
        rs2 = A.sb([128, 512], F32)
        r_rs2 = Res()
        k = 0
        for G in range(NG):
            gs = slice(G * 512, (G + 1) * 512)
            for c in range(6):
                b = k % 2
                k += 1
                for m_ in range(2):
                    mm = 2 * c + m_
                    g, u = mm // 3, mm % 3
                    P.dma("sp", m12[b][:, m_, :], recv_o[g, u, :, gs], writes=[r_m12[b]])
                P.add("dve", lambda h, b=b: h.scalar_tensor_tensor(out=od[b][:], in0=m12[b][:, 1, :], scalar=nlam[:, 0:1], in1=m12[b][:, 0, :],
                                                                     op0=ALU.mult, op1=ALU.add), reads=[r_m12[b], r_lam], writes=[r_od[b]])
                P.add("act", lambda h, b=b: h.activation(out=sq2[b][:], in_=od[b][:], func=AF.Square), reads=[r_od[b]], writes=[r_sq2[b]])
                pb = nps()
                P.add("pe", lambda h, pb=pb, b=b: h.matmul(ps[pb][:], ones_bf[:], sq2[b][:], start=True, stop=True), reads=[r_sq2[b], r_ones], writes=[r_ps[pb]])
                P.add("act", lambda h, pb=pb: h.activation(out=rs2[:], in_=ps[pb][:], func=AF.Sqrt, scale=1.0 / 128.0, bias=SUBLN_EPS), reads=[r_ps[pb]], writes=[r_rs2])
                P.add("dve", lambda h: h.reciprocal(out=rs2[:], in_=rs2[:]), reads=[r_rs2], writes=[r_rs2])
                P.add("dve", lambda h, b=b, c=c, gs=gs: h.scalar_tensor_tensor(out=yT[:, c, gs], in0=od[b][:], scalar=sgc[:, 0:1], in1=rs2[:],
                                                                                 op0=ALU.mult, op1=ALU.mult), reads=[r_od[b], r_sg, r_rs2], writes=[r_y[c][G]])

    gt = [A.sb([128, 512], BF16) for _ in range(3)]
    r_gt = [Res() for _ in range(3)]
    gview = gateT_d.rearrange("(c p) t -> p c t", p=128)
    kk = 0
    for c in range(8):
        for G in range(NG):
            b = kk % 3
            kk += 1
            gs = slice(G * 512, (G + 1) * 512)
            P.dma("sp", gt[b][:], gview[:, c, gs], writes=[r_gt[b]])
            P.add("act", lambda h, b=b: h.activation(out=gt[b][:], in_=gt[b][:], func=AF.Silu), reads=[r_gt[b]], writes=[r_gt[b]])
            P.add("dve", lambda h, b=b, c=c, gs=gs: h.tensor_tensor(out=yT[:, c, gs], in0=yT[:, c, gs], in1=gt[b][:], op=ALU.mult),
                  reads=[r_gt[b], r_y[c][G]], writes=[r_y[c][G]])

    wo = A.sb([128, 8, D], BF16)
    r_wo = [Res() for _ in range(8)]
    wo_v = wout_d.rearrange("(c p) n -> p c n", p=128)
    for j in range(8):
        b = cnt["st"] % 2
        cnt["st"] += 1
        P.dma("sp", wst[b][:], wo_v[:, :, j * WS:(j + 1) * WS], writes=[r_wst[b]])
        for c in range(8):
            if c % 2 == 0:
                P.add("dve", lambda h, b=b, c=c, j=j: h.tensor_copy(out=wo[:, c, j * WS:(j + 1) * WS], in_=wst[b][:, c, :]), reads=[r_wst[b]], writes=[r_wo[j]])
            else:
                P.add("act", lambda h, b=b, c=c, j=j: h.activation(out=wo[:, c, j * WS:(j + 1) * WS], in_=wst[b][:, c, :], func=AF.Copy), reads=[r_wst[b]], writes=[r_wo[j]])
    for G in range(NG):
        gs = slice(G * 512, (G + 1) * 512)
        for co in range(8):
            pb = nps()
            for c in range(8):
                P.add("pe", lambda h, pb=pb, c=c, co=co, gs=gs: h.matmul(ps[pb][:], wo[:, c, co * 128:(co + 1) * 128], yT[:, c, gs], start=(c == 0), stop=(c == 7)),
                      reads=[r_wo[co], r_y[c][G]], writes=[r_ps[pb]])
            P.add("dve", lambda h, pb=pb, co=co, gs=gs: h.tensor_tensor(out=xT_sb[:, co, gs], in0=xT_sb[:, co, gs], in1=ps[pb][:], op=ALU.add),
                  reads=[r_ps[pb], xT_res[G]], writes=[xT_res[G]])
    if xT_out is not None:
        xo = xT_out.rearrange("(c p) t -> p c t", p=128)
        for G in range(NG):
            gs = slice(G * 512, (G + 1) * 512)
            P.dma("sp", xo[:, :, gs], xT_sb[:, :, gs], reads=[xT_res[G]])
    if final is not None:
        fg = A.sb([128, 8], F32)
        r_fg = Res()
        P.dma("sp", fg[:], final["gcol"], writes=[r_fg])
        ob = [A.sb([128, 512], F32) for _ in range(2)]
        r_ob = [Res() for _ in range(2)]
        fo = final["out"].rearrange("(c p) t -> p c t", p=128)
        k = 0
        for G in range(NG):
            gs = slice(G * 512, (G + 1) * 512)
            pb = nps()
            for c in range(8):
                b = c % 2
                P.add("act", lambda h, b=b, c=c, gs=gs: h.activation(out=sqm[b][:], in_=xT_sb[:, c, gs], func=AF.Square), reads=[xT_res[G]], writes=[r_sqm[b]])
                P.add("pe", lambda h, b=b, c=c, pb=pb: h.matmul(ps[pb][:], ones_bf[:], sqm[b][:], start=(c == 0), stop=(c == 7)), reads=[r_sqm[b], r_ones], writes=[r_ps[pb]])
            P.add("act", lambda h, pb=pb: h.activation(out=rstd[:], in_=ps[pb][:], func=AF.Sqrt, scale=1.0 / D, bias=RMS_EPS), reads=[r_ps[pb]], writes=[r_rstd])
            P.add("dve", lambda h: h.reciprocal(out=rstd[:], in_=rstd[:]), reads=[r_rstd], writes=[r_rstd])
            for c in range(8):
                b = k % 2
                k += 1
                P.add("dve", lambda h, b=b, c=c, gs=gs: h.scalar_tensor_tensor(out=ob[b][:], in0=xT_sb[:, c, gs], scalar=fg[:, c:c + 1], in1=rstd[:],
                                                                                 op0=ALU.mult, op1=ALU.mult), reads=[xT_res[G], r_fg, r_rstd], writes=[r_ob[b]])
                P.dma("sp", fo[:, c, gs], ob[b][:], reads=[r_ob[b]])


def build_phase_c_prog(kind, layer, last):
    nc = bass.Bass("TRN2", target_bir_lowering=False)
    vd = 128 if kind == 2 else 64
    xT_d = nc.dram_tensor("xT", [D, TL], F32, kind="ExternalInput").ap()
    recv_o = nc.dram_tensor("recv_o", [4, 3, vd, TL], BF16, kind="ExternalInput").ap()
    gateT = nc.dram_tensor("gateT", [D, TL], BF16, kind="ExternalInput").ap()
    qmT = nc.dram_tensor("qmT", [256, TL], BF16, kind="ExternalInput").ap()
    memT = nc.dram_tensor("memT", [D, 256], F32, kind="ExternalInput").ap()
    mgcol = nc.dram_tensor("mgcol", [128, 8], F32, kind="ExternalInput").ap()
    wkv = nc.dram_tensor("wkv", [D, 512], F32, kind="ExternalInput").ap()
    wout = nc.dram_tensor("wout", [D, D], F32, kind="ExternalInput").ap()
    sel = nc.dram_tensor("sel", [128, 64], F32, kind="ExternalInput").ap()
    diffp = None
    if kind == 2:
        diffp = {nm: nc.dram_tensor(nm, [1, 64], F32, kind="ExternalInput").ap() for nm in ("lq1", "lk1", "lq2", "lk2")}
        diffp["sg"] = nc.dram_tensor("sg", [128, 1], F32, kind="ExternalInput").ap()
    final = None
    xT_out = None
    if last:
        final = dict(gcol=nc.dram_tensor("fgcol", [128, 8], F32, kind="ExternalInput").ap(),
                     out=nc.dram_tensor("outT", [D, TL], F32, kind="ExternalOutput").ap())
    else:
        xT_out = nc.dram_tensor("xT_out", [D, TL], F32, kind="ExternalOutput").ap()
    P = Prog(nc)
    A = SB(nc)
    xT_sb = A.sb([128, 8, TL], F32)
    xres = [Res() for _ in range(TL // 512)]
    phase_c(nc, P, A, kind, layer, xres, xT_sb, recv_o, gateT, qmT, memT, mgcol, wkv, wout, sel, diffp=diffp, final=final,
            xT_out=xT_out, load_x_from=xT_d)
    P.emit()
    return nc, P


_PROGS = {}
DEBUG = {}


def _prog(key, builder):
    if key not in _PROGS:
        _PROGS[key] = builder()[0]
    return _PROGS[key]


def _col8(v):
    return np.ascontiguousarray(np.asarray(v, np.float32).reshape(8, 128).T)


def kernel_unfused(x, mem, norm_g, w_in, w_out, mem_norm_g, w_mem_kv, diff_lambda_q1, diff_lambda_k1,
                   diff_lambda_q2, diff_lambda_k2, diff_subln_g, final_norm_g):
    x = np.asarray(x, np.float32)
    mem = np.asarray(mem, np.float32)
    cores = list(range(NCORE))
    xT = [np.ascontiguousarray(x[c // 4, (c % 4) * TL:(c % 4 + 1) * TL, :].T) for c in cores]
    memT = [np.ascontiguousarray(mem[b].T) for b in range(B)]
    sel = np.zeros((128, 64), np.float32)
    sel[64, :] = 1.0
    out = None
    for layer in range(DEPTH):
        kind = layer % 3
        vd = 128 if kind == 2 else 64
        last = layer == DEPTH - 1
        ncA = _prog(("A", kind), lambda: build_phase_a_prog(kind))
        wl = np.ascontiguousarray(np.asarray(w_in[layer], np.float32))
        gcol = _col8(norm_g[layer])
        resA = run_bass_kernel_spmd(ncA, [{"xT": xT[c], "w_in": wl, "gcol": gcol} for c in cores], core_ids=cores).results
        in_b = []
        for c in cores:
            b, g = c // 4, c % 4
            rq = np.stack([resA[b * 4 + r]["send_qk"][g] for r in range(4)], axis=0)
            rv = np.stack([resA[b * 4 + r]["send_v"][g] for r in range(4)], axis=0)
            m = {"recv_qk": np.ascontiguousarray(rq), "recv_v": np.ascontiguousarray(rv)}
            m.update(phase_b_consts(kind, g))
            in_b.append(m)
        ncB = _prog(("B", kind), lambda: build_phase_b_prog(kind))
        resB = run_bass_kernel_spmd(ncB, in_b, core_ids=cores).results
        in_c = []
        for c in cores:
            b, r = c // 4, c % 4
            ro = np.stack([resB[b * 4 + g]["oT"][:, :, r * TL:(r + 1) * TL] for g in range(4)], axis=0)
            m = {"xT": xT[c], "recv_o": np.ascontiguousarray(ro), "gateT": resA[c]["gateT"], "qmT": resA[c]["qmT"],
                 "memT": memT[b], "mgcol": _col8(mem_norm_g[layer]),
                 "wkv": np.ascontiguousarray(np.asarray(w_mem_kv[layer], np.float32)),
                 "wout": np.ascontiguousarray(np.asarray(w_out[layer], np.float32)), "sel": sel}
            if kind == 2:
                ci = layer // 3
                m["lq1"] = np.asarray(diff_lambda_q1[ci], np.float32).reshape(1, 64)
                m["lk1"] = np.asarray(diff_lambda_k1[ci], np.float32).reshape(1, 64)
                m["lq2"] = np.asarray(diff_lambda_q2[ci], np.float32).reshape(1, 64)
                m["lk2"] = np.asarray(diff_lambda_k2[ci], np.float32).reshape(1, 64)
                m["sg"] = np.asarray(diff_subln_g[ci], np.float32).reshape(128, 1)
            if last:
                m["fgcol"] = _col8(final_norm_g)
            in_c.append(m)
        ncC = _prog(("C", kind, layer, last), lambda: build_phase_c_prog(kind, layer, last))
        resC = run_bass_kernel_spmd(ncC, in_c, core_ids=cores).results
        if last:
            out = np.empty((B, S, D), np.float32)
            for c in cores:
                out[c // 4, (c % 4) * TL:(c % 4 + 1) * TL, :] = resC[c]["outT"].T
        else:
            xT = [resC[c]["xT_out"] for c in cores]
            if "dump" in DEBUG:
                xs = np.empty((B, S, D), np.float32)
                for c in cores:
                    xs[c // 4, (c % 4) * TL:(c % 4 + 1) * TL, :] = xT[c].T
                DEBUG["dump"].append(xs)
    return out


KINDS = [l % 3 for l in range(DEPTH)]


def build_fused():
    nc = bass.Bass("TRN2", target_bir_lowering=False)

    def din(name, shape, dt=F32):
        return nc.dram_tensor(name, shape, dt, kind="ExternalInput").ap()

    def dint(name, shape, dt):
        return nc.dram_tensor(name, shape, dt, kind="Internal").ap()

    xT_in = din("xT_in", [4, D, TL])
    w_in = din("w_in", [DEPTH, D, INW])
    w_out = din("w_out", [DEPTH, D, D])
    wkv = din("wkv", [DEPTH, D, 512])
    gcols = din("gcols", [DEPTH, 128, 8])
    mgcols = din("mgcols", [DEPTH, 128, 8])
    fgcol = din("fgcol", [128, 8])
    memT = din("memT", [D, 256])
    sel = din("sel", [128, 64])
    diffp = {nm: din(nm, [1, 64]) for nm in ("lq1", "lk1", "lq2", "lk2")}
    diffp["sg"] = din("sg", [128, 1])
    ident = din("ident", [128, 128], BF16)
    kones = din("kones", [35, S], BF16)
    masks = {0: din("masks0", [128, 33, 512], BF16), 1: din("masks1", [128, 4, 512], BF16)}
    masks[2] = masks[1]
    qrow = {k: din(f"qrow{k}", [4, 3, 3, S], BF16) for k in (0, 1, 2)}
    biast = {k: din(f"biast{k}", [4, 128, 3, 64]) for k in (0, 1, 2)}
    outT = nc.dram_tensor("outT", [4, D, TL], F32, kind="ExternalOutput").ap()

    xbuf = dint("xbuf", [2, 4, D, TL], F32)
    qkbuf = dint("qkbuf", [4, 4, 2, 3, 64, TL], BF16)
    vbuf = {64: dint("vbuf64", [4, 4, 3, TL, 64], BF16), 128: dint("vbuf128", [4, 4, 3, TL, 128], BF16)}
    gbuf = dint("gbuf", [4, D, TL], BF16)
    qmbuf = dint("qmbuf", [4, 256, TL], BF16)
    obuf = {64: dint("obuf64", [4, 3, 64, S], BF16), 128: dint("obuf128", [4, 3, 128, S], BF16)}

    P = Prog(nc)
    A = SB(nc, arena_words=48 * 1024 - 64)
    for layer in range(DEPTH):
        kind = KINDS[layer]
        vd = 128 if kind == 2 else 64
        last = layer == DEPTH - 1
        for s_ in range(4):
            A.reset()
            xT_sb = A.sb([128, 8, TL], F32)
            xres = [Res() for _ in range(TL // 512)]
            src = xT_in[s_] if layer == 0 else xbuf[layer % 2, s_]
            phase_a(nc, P, A, kind, xres, xT_sb, w_in[layer], gcols[layer], qkbuf[s_], vbuf[vd][s_], gbuf[s_], qmbuf[s_], load_x_from=src)
            P.barrier()
        for g in range(4):
            A.reset()
            cd = dict(ident=ident, sel=sel, kones=kones, masks=masks[kind], qrow=qrow[kind][g], biast=biast[kind][g])
            phase_b(nc, P, A, kind, qkbuf[:, g], vbuf[vd][:, g], cd, obuf[vd][g])
            P.barrier()
        for s_ in range(4):
            A.reset()
            xT_sb = A.sb([128, 8, TL], F32)
            xres = [Res() for _ in range(TL // 512)]
            src = xT_in[s_] if layer == 0 else xbuf[layer % 2, s_]
            phase_c(nc, P, A, kind, layer, xres, xT_sb, obuf[vd][:, :, :, s_ * TL:(s_ + 1) * TL], gbuf[s_], qmbuf[s_], memT, mgcols[layer],
                    wkv[layer], w_out[layer], sel, diffp=(diffp if kind == 2 else None),
                    final=(dict(gcol=fgcol, out=outT[s_]) if last else None),
                    xT_out=(None if last else xbuf[(layer + 1) % 2, s_]), load_x_from=src)
            P.barrier()
    P.emit()
    return nc, P


_FUSED = {}


def kernel(x, mem, norm_g, w_in, w_out, mem_norm_g, w_mem_kv, diff_lambda_q1, diff_lambda_k1,
           diff_lambda_q2, diff_lambda_k2, diff_subln_g, final_norm_g):
    x = np.asarray(x, np.float32)
    mem = np.asarray(mem, np.float32)
    cores = list(range(NCORE))
    if "nc" not in _FUSED:
        _FUSED["nc"] = build_fused()[0]
    nc = _FUSED["nc"]
    f32 = lambda a: np.ascontiguousarray(np.asarray(a, np.float32))
    sel = np.zeros((128, 64), np.float32)
    sel[64, :] = 1.0
    shared = {
        "w_in": f32(w_in), "w_out": f32(w_out), "wkv": f32(w_mem_kv),
        "gcols": np.stack([_col8(norm_g[l]) for l in range(DEPTH)]),
        "mgcols": np.stack([_col8(mem_norm_g[l]) for l in range(DEPTH)]),
        "fgcol": _col8(final_norm_g), "sel": sel,
        "lq1": f32(diff_lambda_q1[0]).reshape(1, 64), "lk1": f32(diff_lambda_k1[0]).reshape(1, 64),
        "lq2": f32(diff_lambda_q2[0]).reshape(1, 64), "lk2": f32(diff_lambda_k2[0]).reshape(1, 64),
        "sg": f32(diff_subln_g[0]).reshape(128, 1),
    }
    for k in (0, 1, 2):
        cs = [phase_b_consts(k, g) for g in range(4)]
        shared[f"qrow{k}"] = np.stack([c["qrow"] for c in cs])
        shared[f"biast{k}"] = np.stack([c["biast"] for c in cs])
        if k < 2:
            shared[f"masks{k}"] = cs[0]["masks"]
        shared["ident"] = cs[0]["ident"]
        shared["kones"] = cs[0]["kones"]
    in_maps = []
    for c in cores:
        b = c // 4
        m = dict(shared)
        m["xT_in"] = np.ascontiguousarray(x[b].reshape(4, TL, D).transpose(0, 2, 1))
        m["memT"] = np.ascontiguousarray(mem[b].T)
        in_maps.append(m)
    res = run_bass_kernel_spmd(nc, in_maps, core_ids=cores).results
    out = np.empty((B, S, D), np.float32)
    for c in cores:
        b, r = c // 4, c % 4
        out[b, r * TL:(r + 1) * TL, :] = res[c]["outT"][r].T
    return out
```

```python
import numpy as np
import ml_dtypes
import concourse.bass as bass
import concourse.mybir as mybir
from concourse.bass_utils import run_bass_kernel_spmd

F32 = mybir.dt.float32
BF16 = mybir.dt.bfloat16
AF = mybir.ActivationFunctionType
ALU = mybir.AluOpType
AX = mybir.AxisListType
NPBF = ml_dtypes.bfloat16

D = 1024
S = 8192
B = 2
DEPTH = 4
TL = 2048
NCORE = 8
MIXW = 768
INW = 3584
NEG = -30000.0
RMS_EPS = 1e-6
SUBLN_EPS = 1e-5


class Res:
    __slots__ = ("name", "lw", "readers")

    def __init__(self, name=""):
        self.name = name
        self.lw = None
        self.readers = []


class Op:
    __slots__ = ("eng", "fn", "dma", "sem", "semval", "needs_inc", "idx", "waits", "gen")


class Prog:
    ENGS = ("pe", "act", "dve", "pool", "sp")

    def __init__(self, nc, same_sync=("act", "dve", "pool"), ndma=12):
        self.nc = nc
        self.h = dict(pe=nc.tensor, act=nc.scalar, dve=nc.vector, pool=nc.gpsimd, sp=nc.sync)
        self.ops = {e: [] for e in self.ENGS}
        self.obs = {e: {} for e in self.ENGS}
        self.same_sync = set(same_sync)
        self.same_dist = 4
        self.esems = {e: [nc.alloc_semaphore(name=f"es_{e}_0")] for e in ("pe", "act", "dve", "pool")}
        self.gen = {e: 0 for e in ("pe", "act", "dve", "pool", "sp")}
        self.gcount = {e: 0 for e in ("pe", "act", "dve", "pool")}
        self.pending = {e: [] for e in self.ENGS}
        self.ndma = ndma
        self.dsem = {q: [nc.alloc_semaphore(name=f"ds_{q}_{i}") for i in range(ndma)] for q in ("sp", "pool", "act")}
        self.dlast = {q: [None] * ndma for q in ("sp", "pool", "act")}
        self.dcnt = {q: 0 for q in ("sp", "pool", "act")}
        self.all_dma = []

    def add(self, eng, fn, reads=(), writes=(), dma=False):
        op = Op()
        op.eng = eng
        op.fn = fn
        op.dma = dma
        op.needs_inc = False
        op.sem = None
        op.semval = None
        op.idx = len(self.ops[eng])
        op.gen = self.gen[eng]
        deps = []
        if self.pending[eng]:
            deps.extend(self.pending[eng])
            self.pending[eng] = []
        for r in reads:
            if r.lw is not None:
                deps.append(r.lw)
        for w in writes:
            if w.lw is not None:
                deps.append(w.lw)
            deps.extend(w.readers)
        if dma:
            n = self.dcnt[eng]
            slot = n % self.ndma
            prev = self.dlast[eng][slot]
            if prev is not None:
                deps.append(prev)
            self.dlast[eng][slot] = op
            op.sem = self.dsem[eng][slot]
            op.semval = 16 * (n // self.ndma + 1)
            self.dcnt[eng] = n + 1
            self.all_dma.append(op)
        waits = []
        obs = self.obs[eng]
        for p in deps:
            if p is op:
                continue
            if p.dma:
                key = ("d", id(p.sem))
                if obs.get(key, 0) >= p.semval:
                    continue
                obs[key] = p.semval
                waits.append(p)
            else:
                if p.eng == eng and (eng not in self.same_sync or op.idx - p.idx >= self.same_dist):
                    continue
                key = ("e", p.eng)
                if obs.get(key, -1) >= p.idx:
                    continue
                obs[key] = p.idx
                if not p.needs_inc:
                    p.needs_inc = True
                    self.gcount[p.eng] += 1
                waits.append(p)
        op.waits = waits
        for r in reads:
            r.readers.append(op)
        for w in writes:
            w.lw = op
            w.readers = []
        self.ops[eng].append(op)
        return op

    def barrier(self, rotate_at=12000):
        lasts = []
        for e in ("pe", "act", "dve", "pool"):
            for op in reversed(self.ops[e]):
                if not op.dma:
                    if not op.needs_inc:
                        op.needs_inc = True
                        self.gcount[e] += 1
                    lasts.append(op)
                    break
        for q in self.dlast:
            for op in self.dlast[q]:
                if op is not None:
                    lasts.append(op)
        for e in self.ENGS:
            self.pending[e] = list(lasts)
        for e in ("pe", "act", "dve", "pool"):
            if self.gcount[e] > rotate_at:
                self.gen[e] += 1
                self.gcount[e] = 0
                self.esems[e].append(self.nc.alloc_semaphore(name=f"es_{e}_{self.gen[e]}"))

    def dma(self, q, out, in_, reads=(), writes=()):
        return self.add(q, lambda h: h.dma_start(out=out, in_=in_), reads, writes, dma=True)

    def emit(self):
        nc = self.nc
        final_waits = {}
        for op in self.all_dma:
            k = id(op.sem)
            if k not in final_waits or final_waits[k][1] < op.semval:
                final_waits[k] = (op.sem, op.semval)
        for e in ("pe", "act", "dve", "pool"):
            c = {}
            for op in self.ops[e]:
                if op.needs_inc and not op.dma:
                    c[op.gen] = c.get(op.gen, 0) + 1
                    op.sem = self.esems[e][op.gen]
                    op.semval = c[op.gen]
        self.max_semval = {e: sum(1 for o in self.ops[e] if o.needs_inc) for e in ("pe", "act", "dve", "pool")}

        def run(e):
            h = self.h[e]
            for op in self.ops[e]:
                for p in op.waits:
                    h.wait_ge(p.sem, p.semval)
                ins = op.fn(h)
                if op.dma:
                    ins.then_inc(op.sem, 16)
                elif op.needs_inc:
                    ins.then_inc(op.sem, 1)
            if e == "sp":
                for p in self.pending[e]:
                    h.wait_ge(p.sem, p.semval)
                for sem, val in final_waits.values():
                    h.wait_ge(sem, val)

        with nc.Block() as block:
            @block.tensor
            def _(t):
                run("pe")

            @block.scalar
            def _(t):
                run("act")

            @block.vector
            def _(t):
                run("dve")

            @block.gpsimd
            def _(t):
                run("pool")

            @block.sync
            def _(t):
                run("sp")


_LET = "abcdefgh"


def _view(ap2d, shape):
    dims = list(shape[1:])
    if len(dims) > 1:
        names = " ".join(_LET[:len(dims)])
        kw = {_LET[i]: dims[i] for i in range(len(dims))}
        ap2d = ap2d.rearrange(f"p ({names}) -> p {names}", **kw)
    if shape[0] != 128:
        ap2d = ap2d[0:shape[0]]
    return ap2d


class SB:
    def __init__(self, nc, arena_words=None):
        self.nc = nc
        self.n = 0
        self.arena = None
        if arena_words is not None:
            self.arena = nc.alloc_sbuf_tensor("arena", [128, arena_words], F32)
            self.words = arena_words
            self.off = 0
            self.banks = [nc.alloc_psum_tensor(f"bank{i}", [128, 512], F32) for i in range(8)]
            self.pi = 0

    def reset(self, keep=0):
        self.off = keep
        self.pi = 0

    def sb(self, shape, dt, name=None):
        self.n += 1
        if self.arena is None:
            return self.nc.alloc_sbuf_tensor(name or f"sb{self.n}", shape, dt)
        n = int(np.prod(shape[1:]))
        esz = 4 if dt == F32 else 2
        words = (n * esz + 3) // 4
        words = (words + 7) // 8 * 8
        assert self.off + words <= self.words, f"SBUF arena overflow: {self.off}+{words} > {self.words}"
        v = self.arena[:, self.off:self.off + words]
        self.off += words
        if dt != F32:
            v = v.bitcast(dt)
        v = v[:, 0:n]
        return _view(v, shape)

    def ps(self, shape, dt, name=None):
        self.n += 1
        if self.arena is None:
            return self.nc.alloc_psum_tensor(name or f"ps{self.n}", shape, dt)
        assert self.pi < 8, "out of PSUM banks"
        b = self.banks[self.pi]
        self.pi += 1
        n = int(np.prod(shape[1:]))
        return _view(b[:, 0:n], shape)


def unit_cols(kind, g, u):
    if kind in (0, 1):
        hh = 4 * u + g
        return hh * 64, MIXW + hh * 64, 2 * MIXW + hh * 64, 64
    mm = 3 * g + u
    head, m = mm // 2, mm % 2
    return head * 128 + m * 64, MIXW + head * 128 + m * 64, 2 * MIXW + head * 128, 128


def alibi_slopes(n):
    return (2.0 ** (-8.0 * np.arange(1, n + 1) / n)).astype(np.float64)


def phase_a(nc, P, A, kind, xT_res, xT_sb, wA, gcol_d, send_qk, send_v, gateT_d, qmT_d, load_x_from=None):
    vd = 128 if kind == 2 else 64
    NG = TL // 512
    ones_bf = A.sb([128, 128], BF16)
    r_ones = Res()
    P.add("pool", lambda h: h.memset(ones_bf[:], 1.0), writes=[r_ones])
    gcol = A.sb([128, 8], F32)
    r_g = Res()
    P.dma("sp", gcol[:], gcol_d, writes=[r_g])
    if load_x_from is not None:
        xv = load_x_from.rearrange("(c p) t -> p c t", p=128)
        for G in range(NG):
            P.dma("sp" if G % 2 == 0 else "pool", xT_sb[:, :, G * 512:(G + 1) * 512], xv[:, :, G * 512:(G + 1) * 512],
                  writes=[xT_res[G]])

    hT = A.sb([128, 8, TL], BF16)
    r_h = [Res() for _ in range(NG)]
    sq = [A.sb([128, 512], BF16) for _ in range(2)]
    r_sq = [Res() for _ in range(2)]
    ss_ps = A.ps([128, 512], F32)
    r_ss = Res()
    rstd = A.sb([128, 512], F32)
    r_rstd = Res()
    i = 0
    for G in range(NG):
        gs = slice(G * 512, (G + 1) * 512)
        for c in range(8):
            b = i % 2
            i += 1
            P.add("act", lambda h, b=b, c=c, gs=gs: h.activation(out=sq[b][:], in_=xT_sb[:, c, gs], func=AF.Square),
                  reads=[xT_res[G]], writes=[r_sq[b]])
            P.add("pe", lambda h, b=b, c=c: h.matmul(ss_ps[:], ones_bf[:], sq[b][:], start=(c == 0), stop=(c == 7)),
                  reads=[r_sq[b], r_ones], writes=[r_ss])
        P.add("act", lambda h: h.activation(out=rstd[:], in_=ss_ps[:], func=AF.Sqrt, scale=1.0 / D, bias=RMS_EPS),
              reads=[r_ss], writes=[r_rstd])
        P.add("dve", lambda h: h.reciprocal(out=rstd[:], in_=rstd[:]), reads=[r_rstd], writes=[r_rstd])
        for c in range(8):
            P.add("dve", lambda h, c=c, gs=gs: h.tensor_tensor(out=hT[:, c, gs], in0=xT_sb[:, c, gs], in1=rstd[:], op=ALU.mult),
                  reads=[xT_res[G], r_rstd], writes=[r_h[G]])

    CG = 256
    ncg = INW // CG
    wst = [A.sb([128, 8, CG], F32) for _ in range(2)]
    r_wst = [Res() for _ in range(2)]
    wb = [A.sb([128, 8, CG], BF16) for _ in range(2)]
    r_wb = [Res() for _ in range(2)]
    wv = wA.rearrange("(c p) n -> p c n", p=128)
    ev = [A.sb([128, TL], BF16) for _ in range(2)]
    r_ev = [Res() for _ in range(2)]
    vsb = [A.sb([128, 16, CG], BF16) for _ in range(2)]
    r_vsb = [Res() for _ in range(2)]
    pacc = [A.ps([128, 512], F32) for _ in range(4)]
    r_pacc = [Res() for _ in range(4)]
    pi = 0
    evi = 0
    vi = 0
    dest = {}
    for g in range(4):
        for u in range(3):
            qc, kc, vc, _ = unit_cols(kind, g, u)
            dest[qc] = (g, 0, u)
            dest[kc] = (g, 1, u)
    vdest = {}
    for g in range(4):
        for u in range(3):
            qc, kc, vc, _ = unit_cols(kind, g, u)
            vdest.setdefault(vc, []).append((g, u))
    for cg in range(ncg):
        b = cg % 2
        c0 = cg * CG
        P.dma("sp" if cg % 2 == 0 else "pool", wst[b][:], wv[:, :, c0:c0 + CG], writes=[r_wst[b]])
        for c in range(8):
            if c % 2 == 0:
                P.add("dve", lambda h, b=b, c=c: h.tensor_scalar(out=wb[b][:, c, :], in0=wst[b][:, c, :], scalar1=gcol[:, c:c + 1],
                                                                  scalar2=None, op0=ALU.mult),
                      reads=[r_wst[b], r_g], writes=[r_wb[b]])
            else:
                P.add("act", lambda h, b=b, c=c: h.activation(out=wb[b][:, c, :], in_=wst[b][:, c, :], func=AF.Copy, scale=gcol[:, c:c + 1]),
                      reads=[r_wst[b], r_g], writes=[r_wb[b]])
        if 2 * MIXW <= c0 < 3 * MIXW:
            vb = vi % 2
            vi += 1
            for tt in range(16):
                pb = pi % 4
                pi += 1
                for c in range(8):
                    P.add("pe", lambda h, pb=pb, c=c, tt=tt, b=b: h.matmul(pacc[pb][:, 0:CG], hT[:, c, tt * 128:(tt + 1) * 128],
                                                                          wb[b][:, c, :], start=(c == 0), stop=(c == 7)),
                          reads=[r_h[tt // 4], r_wb[b]], writes=[r_pacc[pb]])
                eng = "act" if tt % 2 == 0 else "dve"
                if eng == "act":
                    P.add("act", lambda h, pb=pb, vb=vb, tt=tt: h.activation(out=vsb[vb][:, tt, :], in_=pacc[pb][:, 0:CG], func=AF.Copy),
                          reads=[r_pacc[pb]], writes=[r_vsb[vb]])
                else:
                    P.add("dve", lambda h, pb=pb, vb=vb, tt=tt: h.tensor_copy(out=vsb[vb][:, tt, :], in_=pacc[pb][:, 0:CG]),
                          reads=[r_pacc[pb]], writes=[r_vsb[vb]])
            for off in range(0, CG, vd):
                col = c0 + off
                for (g, u) in vdest.get(col, []):
                    dst = send_v[g, u].rearrange("(t p) v -> p t v", p=128)
                    P.dma("sp", dst, vsb[vb][:, :, off:off + vd], reads=[r_vsb[vb]])
            continue
        for ch in range(CG // 128):
            col = c0 + ch * 128
            isq = col < MIXW or (3 * MIXW <= col < 3 * MIXW + 256)
            eb = evi % 2
            evi += 1
            for G in range(NG):
                gs = slice(G * 512, (G + 1) * 512)
                pb = pi % 4
                pi += 1
                for c in range(8):
                    P.add("pe", lambda h, pb=pb, c=c, gs=gs, b=b, ch=ch: h.matmul(pacc[pb][:], wb[b][:, c, ch * 128:(ch + 1) * 128],
                                                                                  hT[:, c, gs], start=(c == 0), stop=(c == 7)),
                          reads=[r_h[G], r_wb[b]], writes=[r_pacc[pb]])
                sc = 0.125 if isq else 1.0
                if G % 2 == 0:
                    P.add("act", lambda h, pb=pb, eb=eb, gs=gs, sc=sc: h.activation(out=ev[eb][:, gs], in_=pacc[pb][:], func=AF.Copy, scale=sc),
                          reads=[r_pacc[pb]], writes=[r_ev[eb]])
                else:
                    P.add("dve", lambda h, pb=pb, eb=eb, gs=gs, sc=sc: h.tensor_scalar(out=ev[eb][:, gs], in0=pacc[pb][:], scalar1=sc, scalar2=None, op0=ALU.mult),
                          reads=[r_pacc[pb]], writes=[r_ev[eb]])
            if col < 2 * MIXW:
                for half in range(2):
                    g, qk, u = dest[col + half * 64]
                    P.dma("sp", send_qk[g, qk, u], ev[eb][half * 64:(half + 1) * 64, :], reads=[r_ev[eb]])
            elif col < 3 * MIXW + 256:
                r0 = col - 3 * MIXW
                P.dma("sp", qmT_d[r0:r0 + 128, :], ev[eb][:], reads=[r_ev[eb]])
            else:
                r0 = col - (3 * MIXW + 256)
                P.dma("sp", gateT_d[r0:r0 + 128, :], ev[eb][:], reads=[r_ev[eb]])


def build_phase_a_prog(kind):
    nc = bass.Bass("TRN2", target_bir_lowering=False)
    vd = 128 if kind == 2 else 64
    xT_d = nc.dram_tensor("xT", [D, TL], F32, kind="ExternalInput").ap()
    w_d = nc.dram_tensor("w_in", [D, INW], F32, kind="ExternalInput").ap()
    g_d = nc.dram_tensor("gcol", [128, 8], F32, kind="ExternalInput").ap()
    send_qk = nc.dram_tensor("send_qk", [4, 2, 3, 64, TL], BF16, kind="ExternalOutput").ap()
    send_v = nc.dram_tensor("send_v", [4, 3, TL, vd], BF16, kind="ExternalOutput").ap()
    gateT = nc.dram_tensor("gateT", [D, TL], BF16, kind="ExternalOutput").ap()
    qmT = nc.dram_tensor("qmT", [256, TL], BF16, kind="ExternalOutput").ap()
    P = Prog(nc)
    A = SB(nc)
    xT_sb = A.sb([128, 8, TL], F32)
    xres = [Res() for _ in range(TL // 512)]
    phase_a(nc, P, A, kind, xres, xT_sb, w_d, g_d, send_qk, send_v, gateT, qmT, load_x_from=xT_d)
    P.emit()
    return nc, P


DSW = ((128, 1), (512, 4), (2048, 16))


def unit_slope(kind, g, u):
    if kind in (0, 1):
        return alibi_slopes(12)[4 * u + g]
    return alibi_slopes(6)[(3 * g + u) // 2]


def split3(x):
    hi = x.astype(NPBF)
    r = x - hi.astype(np.float64)
    lo = r.astype(NPBF)
    r2 = r - lo.astype(np.float64)
    lo2 = r2.astype(NPBF)
    return hi, lo, lo2


def phase_b_consts(kind, g):
    c = {}
    c["ident"] = np.eye(128, dtype=np.float32).astype(NPBF)
    sel = np.zeros((128, 64), np.float32)
    sel[64, :] = 1.0
    c["sel"] = sel
    t = np.arange(S, dtype=np.float64)
    qrow = np.zeros((3, 3, S), dtype=NPBF)
    biast = np.zeros((128, 3, 64), dtype=np.float32)
    p = np.arange(128, dtype=np.float64)
    for u in range(3):
        sl = unit_slope(kind, g, u)
        x = -sl * (t - (t // 512) * 512)
        hi, lo, lo2 = split3(x)
        qrow[u, 0], qrow[u, 1], qrow[u, 2] = hi, lo, lo2
        for j in range(64):
            jp = 3 - j
            biast[:, u, j] = (sl * (128.0 * jp + p)).astype(np.float32)
    c["qrow"] = qrow
    c["biast"] = biast
    kones = np.zeros((35, S), dtype=np.float32)
    for b in range(32):
        kones[b, b * 256:(b + 1) * 256] = 1.0
    kones[32:35] = 1.0
    c["kones"] = kones.astype(NPBF)
    q = np.arange(512)[None, :]
    if kind == 0:
        tiles = []
        for (w, d) in DSW:
            for jp in range(-w // 128, 4):
                tk = 128 * jp + np.arange(128)[:, None]
                dist = q - tk
                valid = (dist >= 0) & (dist <= w) & (dist % d == 0)
                tiles.append(np.where(valid, 0.0, NEG))
        m = np.stack(tiles, axis=1)
    else:
        tiles = []
        for i in range(4):
            tk = 128 * i + np.arange(128)[:, None]
            tiles.append(np.where(tk > q, NEG, 0.0))
        m = np.stack(tiles, axis=1)
    c["masks"] = m.astype(np.float32).astype(NPBF)
    return c


def phase_b(nc, P, A, kind, recv_qk, recv_v, cd, oT_d):
    vd = 128 if kind == 2 else 64
    VW = vd + 1
    arow = 96 if kind == 1 else 64
    KA = arow + 3
    NGq = S // 512
    nmask = 33 if kind == 0 else 4
    ident = A.sb([128, 128], BF16)
    r_id = Res()
    P.dma("sp", ident[:], cd["ident"], writes=[r_id])
    biast = A.sb([128, 3, 64], F32)
    r_bt = Res()
    P.dma("sp", biast[:], cd["biast"], writes=[r_bt])
    masks = A.sb([128, nmask, 512], BF16)
    r_mk = Res()
    P.dma("pool", masks[:], cd["masks"], writes=[r_mk])
    ones_f = A.sb([128, 64], F32)
    r_of = Res()
    P.dma("sp", ones_f[:], cd["sel"], writes=[r_of])

    nunit_res = 3 if kind == 0 else 2
    ka = [A.sb([128, S], BF16) for _ in range(nunit_res)]
    r_ka = [Res() for _ in range(nunit_res)]
    va = [A.sb([128, 64, VW], BF16) for _ in range(nunit_res)]
    r_va = [Res() for _ in range(nunit_res)]
    for i in range(nunit_res):
        P.add("pool", lambda h, i=i: h.memset(va[i][:, :, 64:65], 1.0), writes=[r_va[i]])
    NQB = 4
    qa = [A.sb([128, 512], BF16) for _ in range(NQB)]
    r_qa = [Res() for _ in range(NQB)]
    NS = 3
    s_ps = [A.ps([128, 512], F32) for _ in range(NS)]
    r_s = [Res() for _ in range(NS)]
    NPT = 4
    pt = [A.sb([128, 512], BF16) for _ in range(NPT)]
    r_pt = [Res() for _ in range(NPT)]
    if kind == 2:
        oa_ps = [A.ps([128, 512], F32) for _ in range(2)]
        ob_ps = [A.ps([128, 512], F32) for _ in range(2)]
        r_oa = [Res() for _ in range(2)]
        r_ob = [Res() for _ in range(2)]
    elif kind == 0:
        oa_ps = [A.ps([128, 512], F32) for _ in range(4)]
        r_oa = [Res() for _ in range(4)]
    else:
        oa_ps = [A.ps([128, 512], F32) for _ in range(2)]
        r_oa = [Res() for _ in range(2)]
    bc_ps = A.ps([128, 512], F32)
    r_bc = Res()
    rd = A.sb([128, 512], F32)
    r_rd = Res()
    P.add("pool", lambda h: h.memset(rd[:], 0.0), writes=[r_rd])
    osb = [A.sb([64, 512], F32) for _ in range(2)]
    r_osb = [Res() for _ in range(2)]
    outb = [A.sb([64, 512], BF16) for _ in range(4)]
    r_outb = [Res() for _ in range(4)]
    if kind == 1:
        gate_ps = A.ps([128, 4, 32], F32)
        r_gate = Res()
        tr_ps = A.ps([128, 512], F32)
        r_tr = Res()
        gm = A.sb([128, 4, 32], F32)
        r_gm = Res()
        P.add("pool", lambda h: h.memset(gm[:], -1e30), writes=[r_gm])
        mx8 = A.sb([128, 4, 8], F32)
        r_mx = Res()
        negm = A.sb([128, 4, 96], BF16)
        r_negm = Res()
        P.add("pool", lambda h: h.memset(negm[:], 0.0), writes=[r_negm])
        kms = A.sb([64, 32], F32)
        kmb = A.sb([64, 32], BF16)
        r_km = Res()

    cnt = dict(q=0, s=0, pt=0, o=0, osb=0, outb=0)

    def load_unit(u, slot):
        for src in range(4):
            P.dma("sp", ka[slot][0:64, src * TL:(src + 1) * TL], recv_qk[src, 1, u], writes=[r_ka[slot]])
        if kind == 1:
            P.dma("pool", ka[slot][64:99, :], cd["kones"], writes=[r_ka[slot]])
        else:
            P.dma("pool", ka[slot][64:67, :], cd["kones"][32:35, :], writes=[r_ka[slot]])
        for src in range(4):
            sv = recv_v[src, u].rearrange("(t p) v -> p t v", p=128)
            if vd == 64:
                P.dma("pool", va[slot][:, src * 16:(src + 1) * 16, 0:64], sv, writes=[r_va[slot]])
            else:
                P.dma("pool", va[slot][:, src * 16:(src + 1) * 16, 0:64], sv[:, :, 0:64], writes=[r_va[slot]])
                P.dma("pool", va[slot][:, src * 16:(src + 1) * 16, 65:129], sv[:, :, 64:128], writes=[r_va[slot]])

    def load_q(u, G):
        qb = cnt["q"] % NQB
        cnt["q"] += 1
        src, lg = G // 4, G % 4
        P.dma("sp", qa[qb][0:64, :], recv_qk[src, 0, u, :, lg * 512:(lg + 1) * 512], writes=[r_qa[qb]])
        P.dma("sp", qa[qb][arow:arow + 3, :], cd["qrow"][u, :, G * 512:(G + 1) * 512], writes=[r_qa[qb]])
        return qb

    def moba_prep(slot, qb, G):
        for i in range(4):
            P.add("pe", lambda h, i=i: h.matmul(gate_ps[:, i, :], qa[qb][0:64, i * 128:(i + 1) * 128], kmb[:, :], start=True, stop=True),
                  reads=[r_qa[qb], r_km], writes=[r_gate])
        for i in range(4):
            n = (4 * G + i) // 2
            if n > 0:
                P.add("dve", lambda h, i=i, n=n: h.tensor_copy(out=gm[:, i, 0:n], in_=gate_ps[:, i, 0:n]), reads=[r_gate], writes=[r_gm])
            P.add("dve", lambda h, i=i: h.max(out=mx8[:, i, :], in_=gm[:, i, :]), reads=[r_gm], writes=[r_mx])
            P.add("dve", lambda h, i=i: h.tensor_scalar(out=negm[:, i, 64:96], in0=gm[:, i, :], scalar1=mx8[:, i, 2:3], scalar2=1.0,
                                                        op0=ALU.is_ge, op1=ALU.subtract), reads=[r_gm, r_mx], writes=[r_negm])
            P.add("dve", lambda h, i=i, n=n: h.memset(negm[:, i, 64 + n:65 + n], 0.0), writes=[r_negm])

    def moba_prep2(slot, qb, G):
        for i in range(4):
            P.add("pe", lambda h, i=i: h.matmul(tr_ps[0:96, i * 128:(i + 1) * 128], negm[:, i, :], ident[:], start=True, stop=True),
                  reads=[r_negm, r_id], writes=[r_tr])
        P.add("act", lambda h: h.activation(out=qa[qb][64:96, :], in_=tr_ps[64:96, :], func=AF.Copy, scale=-NEG),
              reads=[r_tr], writes=[r_qa[qb]])

    def steps_for(u, G):
        out = []
        if kind == 0:
            w, d = DSW[u]
            nb = w // 128
            moff = [0, 5, 13][u]
            for jp in range(-nb, 4):
                kt = 4 * G + jp
                if kt < 0:
                    continue
                out.append((kt, moff + jp + nb, 3 - jp))
        else:
            for kt in range(0, 4 * G + 4):
                jp = kt - 4 * G
                out.append((kt, jp if jp >= 0 else None, 3 - jp))
        return out

    def attend(u, slot, G, qb, ob):
        attend_flat([(u, slot, G, qb, ob)])

    def attend_flat(segs):
        flat = []
        for (u, slot, G, qb, ob) in segs:
            st = steps_for(u, G)
            for i, (kt, mi, bj) in enumerate(st):
                flat.append((u, slot, qb, ob, kt, mi, bj, i == 0, i == len(st) - 1))
        n = len(flat)
        sb_of = {}
        pt_of = {}

        def qk(i):
            u, slot, qb, ob, kt, mi, bj, first, lastf = flat[i]
            sbk = cnt["s"] % NS
            cnt["s"] += 1
            sb_of[i] = sbk
            P.add("pe", lambda h: h.matmul(s_ps[sbk][:], ka[slot][0:KA, kt * 128:(kt + 1) * 128], qa[qb][0:KA, :], start=True, stop=(mi is None)),
                  reads=[r_ka[slot], r_qa[qb]], writes=[r_s[sbk]])
            if mi is not None:
                P.add("pe", lambda h: h.matmul(s_ps[sbk][:], ident[:], masks[:, mi, :], start=False, stop=True),
                      reads=[r_id, r_mk], writes=[r_s[sbk]])

        def ex(i):
            u, slot, qb, ob, kt, mi, bj, first, lastf = flat[i]
            sbk = sb_of[i]
            pb = cnt["pt"] % NPT
            cnt["pt"] += 1
            pt_of[i] = pb
            P.add("act", lambda h: h.activation(out=pt[pb][:], in_=s_ps[sbk][:], func=AF.Exp, bias=biast[:, u, bj:bj + 1], scale=1.0),
                  reads=[r_s[sbk], r_bt], writes=[r_pt[pb]])

        def pv(i):
            u, slot, qb, ob, kt, mi, bj, first, lastf = flat[i]
            pb = pt_of[i]
            P.add("pe", lambda h: h.matmul(oa_ps[ob][0:65, :], va[slot][:, kt, 0:65], pt[pb][:], start=first, stop=lastf),
                  reads=[r_va[slot], r_pt[pb]], writes=[r_oa[ob]])
            if kind == 2:
                P.add("pe", lambda h: h.matmul(ob_ps[ob][0:64, :], va[slot][:, kt, 65:129], pt[pb][:], start=first, stop=lastf),
                      reads=[r_va[slot], r_pt[pb]], writes=[r_ob[ob]])

        LA = 2
        for i in range(min(LA, n)):
            qk(i)
            ex(i)
        for i in range(n):
            if i + LA < n:
                qk(i + LA)
                ex(i + LA)
            pv(i)

    def finish(u, G, obs):
        gs = slice(G * 512, (G + 1) * 512)
        first = True
        for (uu, ob) in obs:
            if first:
                P.add("dve", lambda h, ob=ob: h.tensor_copy(out=rd[64:65, :], in_=oa_ps[ob][64:65, :]), reads=[r_oa[ob]], writes=[r_rd])
                first = False
            else:
                P.add("dve", lambda h, ob=ob: h.tensor_tensor(out=rd[64:65, :], in0=rd[64:65, :], in1=oa_ps[ob][64:65, :], op=ALU.add),
                      reads=[r_oa[ob], r_rd], writes=[r_rd])
        P.add("dve", lambda h: h.reciprocal(out=rd[64:65, :], in_=rd[64:65, :]), reads=[r_rd], writes=[r_rd])

    def finish2(u, G, obs):
        gs = slice(G * 512, (G + 1) * 512)
        P.add("pe", lambda h: h.matmul(bc_ps[0:64, :], ones_f[:, :], rd[:, :], start=True, stop=True),
              reads=[r_of, r_rd], writes=[r_bc])
        for (uu, ob) in obs:
            parts = [(oa_ps, r_oa, 0)] + ([(ob_ps, r_ob, 64)] if kind == 2 else [])
            for (pst, rr, row0) in parts:
                sb = cnt["osb"] % 2
                cnt["osb"] += 1
                bb = cnt["outb"] % 4
                cnt["outb"] += 1
                P.add("act", lambda h, pst=pst, ob=ob, sb=sb: h.activation(out=osb[sb][:], in_=pst[ob][0:64, :], func=AF.Copy),
                      reads=[rr[ob]], writes=[r_osb[sb]])
                P.add("dve", lambda h, sb=sb, bb=bb: h.tensor_tensor(out=outb[bb][:], in0=osb[sb][:], in1=bc_ps[0:64, :], op=ALU.mult),
                      reads=[r_osb[sb], r_bc], writes=[r_outb[bb]])
                P.dma("sp", oT_d[uu, row0:row0 + 64, gs], outb[bb][:], reads=[r_outb[bb]])

    if kind == 0:
        for u in range(3):
            load_unit(u, u)
        pend = None
        for G in range(NGq):
            obs = []
            segs = []
            for idx, u in enumerate((2, 1, 0)):
                qb = load_q(u, G)
                ob = (3 * G + idx) % 4
                segs.append((u, u, G, qb, ob))
                obs.append((u, ob))
            attend_flat(segs[:1])
            if pend is not None:
                finish2(*pend)
            attend_flat(segs[1:])
            finish(0, G, obs)
            pend = (0, G, obs)
        finish2(*pend)
    else:
        load_unit(0, 0)
        for u in range(3):
            slot = u % 2
            if u + 1 < 3:
                load_unit(u + 1, (u + 1) % 2)
            if kind == 1:
                P.add("dve", lambda h, slot=slot: h.tensor_reduce(out=kms[:, :], in_=ka[slot][0:64, :].rearrange("p (b k) -> p b k", k=256),
                                                                   axis=AX.X, op=ALU.add), reads=[r_ka[slot]], writes=[r_km])
                P.add("dve", lambda h: h.tensor_copy(out=kmb[:, :], in_=kms[:, :]), reads=[r_km], writes=[r_km])
                if u > 0:
                    P.add("pool", lambda h: h.memset(gm[:], -1e30), writes=[r_gm])
            qbs = {0: load_q(u, 0)}
            if kind == 1:
                moba_prep(slot, qbs[0], 0)
                moba_prep2(slot, qbs[0], 0)
            pend = None
            for G in range(NGq):
                if G + 1 < NGq:
                    qbs[G + 1] = load_q(u, G + 1)
                    if kind == 1:
                        moba_prep(slot, qbs[G + 1], G + 1)
                ob = cnt["o"] % 2
                cnt["o"] += 1
                attend(u, slot, G, qbs[G], ob)
                if pend is not None:
                    finish2(*pend)
                if kind == 1 and G + 1 < NGq:
                    moba_prep2(slot, qbs[G + 1], G + 1)
                finish(u, G, [(u, ob)])
                pend = (u, G, [(u, ob)])
            finish2(*pend)


def build_phase_b_prog(kind):
    nc = bass.Bass("TRN2", target_bir_lowering=False)
    vd = 128 if kind == 2 else 64
    recv_qk = nc.dram_tensor("recv_qk", [4, 2, 3, 64, TL], BF16, kind="ExternalInput").ap()
    recv_v = nc.dram_tensor("recv_v", [4, 3, TL, vd], BF16, kind="ExternalInput").ap()
    nmask = 33 if kind == 0 else 4
    cd = dict(
        ident=nc.dram_tensor("ident", [128, 128], BF16, kind="ExternalInput").ap(),
        sel=nc.dram_tensor("sel", [128, 64], F32, kind="ExternalInput").ap(),
        qrow=nc.dram_tensor("qrow", [3, 3, S], BF16, kind="ExternalInput").ap(),
        biast=nc.dram_tensor("biast", [128, 3, 64], F32, kind="ExternalInput").ap(),
        kones=nc.dram_tensor("kones", [35, S], BF16, kind="ExternalInput").ap(),
        masks=nc.dram_tensor("masks", [128, nmask, 512], BF16, kind="ExternalInput").ap(),
    )
    oT = nc.dram_tensor("oT", [3, vd, S], BF16, kind="ExternalOutput").ap()
    P = Prog(nc)
    A = SB(nc)
    phase_b(nc, P, A, kind, recv_qk, recv_v, cd, oT)
    P.emit()
    return nc, P


def phase_c(nc, P, A, kind, layer, xT_res, xT_sb, recv_o, gateT_d, qmT_d, memT_d, mgcol_d, wkv_d, wout_d, sel_d,
            diffp=None, final=None, xT_out=None, load_x_from=None):
    vd = 128 if kind == 2 else 64
    NG = TL // 512
    lam_init = 0.8 - 0.6 * float(np.exp(-0.3 * layer))
    if load_x_from is not None:
        xv = load_x_from.rearrange("(c p) t -> p c t", p=128)
        for G in range(NG):
            P.dma("pool", xT_sb[:, :, G * 512:(G + 1) * 512], xv[:, :, G * 512:(G + 1) * 512], writes=[xT_res[G]])
    ones_bf = A.sb([128, 128], BF16)
    r_ones = Res()
    P.add("pool", lambda h: h.memset(ones_bf[:], 1.0), writes=[r_ones])
    sel = A.sb([128, 64], F32)
    r_sel = Res()
    P.dma("sp", sel[:], sel_d, writes=[r_sel])
    mgcol = A.sb([128, 8], F32)
    r_mg = Res()
    P.dma("sp", mgcol[:], mgcol_d, writes=[r_mg])

    NPS = 6
    ps = [A.ps([128, 512], F32) for _ in range(NPS)]
    r_ps = [Res() for _ in range(NPS)]
    cnt = dict(ps=0, pt=0, st=0)

    def nps():
        i = cnt["ps"] % NPS
        cnt["ps"] += 1
        return i

    memT = A.sb([128, 8, 256], F32)
    r_mem = Res()
    P.dma("sp", memT[:], memT_d.rearrange("(c p) t -> p c t", p=128), writes=[r_mem])
    sqm = [A.sb([128, 512], BF16) for _ in range(2)]
    r_sqm = [Res() for _ in range(2)]
    rstd = A.sb([128, 512], F32)
    r_rstd = Res()
    mnT = A.sb([128, 8, 256], BF16)
    r_mn = Res()
    pb = nps()
    for c in range(8):
        b = c % 2
        P.add("act", lambda h, b=b, c=c: h.activation(out=sqm[b][:, 0:256], in_=memT[:, c, :], func=AF.Square), reads=[r_mem], writes=[r_sqm[b]])
        P.add("pe", lambda h, b=b, c=c: h.matmul(ps[pb][:, 0:256], ones_bf[:], sqm[b][:, 0:256], start=(c == 0), stop=(c == 7)),
              reads=[r_sqm[b], r_ones], writes=[r_ps[pb]])
    P.add("act", lambda h: h.activation(out=rstd[:, 0:256], in_=ps[pb][:, 0:256], func=AF.Sqrt, scale=1.0 / D, bias=RMS_EPS), reads=[r_ps[pb]], writes=[r_rstd])
    P.add("dve", lambda h: h.reciprocal(out=rstd[:, 0:256], in_=rstd[:, 0:256]), reads=[r_rstd], writes=[r_rstd])
    for c in range(8):
        P.add("dve", lambda h, c=c: h.tensor_tensor(out=mnT[:, c, :], in0=memT[:, c, :], in1=rstd[:, 0:256], op=ALU.mult), reads=[r_mem, r_rstd], writes=[r_mn])
    WS = 128
    wst = [A.sb([128, 8, WS], F32) for _ in range(2)]
    r_wst = [Res() for _ in range(2)]
    wkv = A.sb([128, 8, 512], BF16)
    r_wkv = Res()
    wkv_v = wkv_d.rearrange("(c p) n -> p c n", p=128)
    for j in range(512 // WS):
        b = cnt["st"] % 2
        cnt["st"] += 1
        P.dma("sp", wst[b][:], wkv_v[:, :, j * WS:(j + 1) * WS], writes=[r_wst[b]])
        for c in range(8):
            if c % 2 == 0:
                P.add("dve", lambda h, b=b, c=c, j=j: h.tensor_scalar(out=wkv[:, c, j * WS:(j + 1) * WS], in0=wst[b][:, c, :], scalar1=mgcol[:, c:c + 1],
                                                                       scalar2=None, op0=ALU.mult), reads=[r_wst[b], r_mg], writes=[r_wkv])
            else:
                P.add("act", lambda h, b=b, c=c, j=j: h.activation(out=wkv[:, c, j * WS:(j + 1) * WS], in_=wst[b][:, c, :], func=AF.Copy, scale=mgcol[:, c:c + 1]),
                      reads=[r_wst[b], r_mg], writes=[r_wkv])
    kmT = A.sb([128, 2, 256], BF16)
    r_kmT = Res()
    for ch in range(2):
        pb = nps()
        for c in range(8):
            P.add("pe", lambda h, pb=pb, c=c, ch=ch: h.matmul(ps[pb][:, 0:256], wkv[:, c, ch * 128:(ch + 1) * 128], mnT[:, c, :], start=(c == 0), stop=(c == 7)),
                  reads=[r_wkv, r_mn], writes=[r_ps[pb]])
        P.add("act", lambda h, pb=pb, ch=ch: h.activation(out=kmT[:, ch, :], in_=ps[pb][:, 0:256], func=AF.Copy), reads=[r_ps[pb]], writes=[r_kmT])
    vma = A.sb([128, 2, 4, 65], BF16)
    r_vma = Res()
    P.add("pool", lambda h: h.memset(vma[:], 1.0), writes=[r_vma])
    for mt in range(2):
        pb = nps()
        for c in range(8):
            P.add("pe", lambda h, pb=pb, c=c, mt=mt: h.matmul(ps[pb][:, 0:256], mnT[:, c, mt * 128:(mt + 1) * 128], wkv[:, c, 256:512], start=(c == 0), stop=(c == 7)),
                  reads=[r_wkv, r_mn], writes=[r_ps[pb]])
        P.add("dve", lambda h, pb=pb, mt=mt: h.tensor_copy(out=vma[:, mt, :, 0:64], in_=ps[pb][:, 0:256].rearrange("p (h d) -> p h d", d=64)),
              reads=[r_ps[pb]], writes=[r_vma])

    qm = A.sb([128, 2, TL], BF16)
    r_qm = Res()
    P.dma("sp", qm[:], qmT_d.rearrange("(c p) t -> p c t", p=128), writes=[r_qm])
    yT = A.sb([128, 8, TL], BF16)
    r_y = [[Res() for _ in range(NG)] for _ in range(8)]
    pt = [A.sb([128, 512], BF16) for _ in range(3)]
    r_pt = [Res() for _ in range(3)]
    rd = A.sb([128, 512], F32)
    r_rd = Res()
    P.add("pool", lambda h: h.memset(rd[:], 0.0), writes=[r_rd])
    osb = [A.sb([64, 512], F32) for _ in range(2)]
    r_osb = [Res() for _ in range(2)]
    mo = [A.sb([64, 512], BF16) for _ in range(2)]
    r_mo = [Res() for _ in range(2)]
    k = 0
    for G in range(NG):
        gs = slice(G * 512, (G + 1) * 512)
        for hm in range(4):
            ch, r0 = hm // 2, (hm % 2) * 64
            po = nps()
            for mt in range(2):
                pb = nps()
                P.add("pe", lambda h, pb=pb, ch=ch, r0=r0, mt=mt, gs=gs: h.matmul(ps[pb][:], kmT[r0:r0 + 64, ch, mt * 128:(mt + 1) * 128], qm[r0:r0 + 64, ch, gs],
                                                                                 start=True, stop=True), reads=[r_kmT, r_qm], writes=[r_ps[pb]])
                pi = cnt["pt"] % 3
                cnt["pt"] += 1
                P.add("act", lambda h, pb=pb, pi=pi: h.activation(out=pt[pi][:], in_=ps[pb][:], func=AF.Exp), reads=[r_ps[pb]], writes=[r_pt[pi]])
                P.add("pe", lambda h, po=po, pi=pi, mt=mt, hm=hm: h.matmul(ps[po][0:65, :], vma[:, mt, hm, :], pt[pi][:], start=(mt == 0), stop=(mt == 1)),
                      reads=[r_vma, r_pt[pi]], writes=[r_ps[po]])
            P.add("dve", lambda h, po=po: h.reciprocal(out=rd[64:65, :], in_=ps[po][64:65, :]), reads=[r_ps[po], r_rd], writes=[r_rd])
            pbc = nps()
            P.add("pe", lambda h, pbc=pbc: h.matmul(ps[pbc][0:64, :], sel[:, :], rd[:, :], start=True, stop=True), reads=[r_sel, r_rd], writes=[r_ps[pbc]])
            sb = k % 2
            mb = k % 2
            k += 1
            P.add("act", lambda h, po=po, sb=sb: h.activation(out=osb[sb][:], in_=ps[po][0:64, :], func=AF.Copy), reads=[r_ps[po]], writes=[r_osb[sb]])
            P.add("dve", lambda h, sb=sb, mb=mb, pbc=pbc: h.tensor_tensor(out=mo[mb][:], in0=osb[sb][:], in1=ps[pbc][0:64, :], op=ALU.mult),
                  reads=[r_osb[sb], r_ps[pbc]], writes=[r_mo[mb]])
            P.dma("pool", yT[r0:r0 + 64, 6 + ch, gs], mo[mb][:], reads=[r_mo[mb]], writes=[r_y[6 + ch][G]])

    if kind in (0, 1):
        for c in range(6):
            for half in range(2):
                hh = 2 * c + half
                g, u = hh % 4, hh // 4
                P.dma("sp", yT[half * 64:(half + 1) * 64, c, :], recv_o[g, u], writes=[r_y[c][G] for G in range(NG)])
    else:
        lp = A.sb([128, 4, 64], F32)
        r_lp = Res()
        for i, nm in enumerate(("lq1", "lk1", "lq2", "lk2")):
            P.dma("sp", lp[:, i, :], diffp[nm][0].partition_broadcast(128), writes=[r_lp])
        sgc = A.sb([128, 1], F32)
        r_sg = Res()
        P.dma("sp", sgc[:], diffp["sg"], writes=[r_sg])
        lpr = A.sb([128, 2, 64], F32)
        lsum = A.sb([128, 2], F32)
        nlam = A.sb([128, 1], F32)
        r_lam = Res()
        P.add("dve", lambda h: h.tensor_tensor(out=lpr[:, 0, :], in0=lp[:, 0, :], in1=lp[:, 1, :], op=ALU.mult), reads=[r_lp], writes=[r_lam])
        P.add("dve", lambda h: h.tensor_tensor(out=lpr[:, 1, :], in0=lp[:, 2, :], in1=lp[:, 3, :], op=ALU.mult), reads=[r_lp, r_lam], writes=[r_lam])
        P.add("dve", lambda h: h.tensor_reduce(out=lsum[:, :], in_=lpr[:, :, :], axis=AX.X, op=ALU.add), reads=[r_lam], writes=[r_lam])
        P.add("act", lambda h: h.activation(out=lsum[:, :], in_=lsum[:, :], func=AF.Exp), reads=[r_lam], writes=[r_lam])
        P.add("dve", lambda h: h.tensor_tensor(out=nlam[:, :], in0=lsum[:, 1:2], in1=lsum[:, 0:1], op=ALU.subtract), reads=[r_lam], writes=[r_lam])
        P.add("dve", lambda h: h.tensor_scalar(out=nlam[:, :], in0=nlam[:, :], scalar1=-lam_init, scalar2=None, op0=ALU.add), reads=[r_lam], writes=[r_lam])
        P.add("dve", lambda h: h.tensor_scalar(out=sgc[:, :], in0=sgc[:, :], scalar1=(1.0 - lam_init), scalar2=None, op0=ALU.mult), reads=[r_sg], writes=[r_sg])
        m12 = [A.sb([128, 2, 512], BF16) for _ in range(2)]
        r_m12 = [Res() for _ in range(2)]
        od = [A.sb([128, 512], F32) for _ in range(2)]
        r_od = [Res() for _ in range(2)]
        sq2 = [A.sb([128, 512], BF16) for _ in range(2)]
        r_sq2 = [Res() for _ in range(2)]
        rs2 = A.sb([128, 512], F32)
        r_rs2 = Res()
        k = 0
        for G in range(NG):
            gs = slice(G * 512, (G + 1) * 512)
            for c in range(6):
                b = k % 2
                k += 1
                for m_ in range(2):
                    mm = 2 * c + m_
                    g, u = mm // 3, mm % 3
                    P.dma("sp", m12[b][:, m_, :], recv_o[g, u, :, gs], writes=[r_m12[b]])
                P.add("dve", lambda h, b=b: h.scalar_tensor_tensor(out=od[b][:], in0=m12[b][:, 1, :], scalar=nlam[:, 0:1], in1=m12[b][:, 0, :],
                                                                     op0=ALU.mult, op1=ALU.add), reads=[r_m12[b], r_lam], writes=[r_od[b]])
                P.add("act", lambda h, b=b: h.activation(out=sq2[b][:], in_=od[b][:], func=AF.Square), reads=[r_od[b]], writes=[r_sq2[b]])
                pb = nps()
                P.add("pe", lambda h, pb=pb, b=b: h.matmul(ps[pb][:], ones_bf[:], sq2[b][:], start=True, stop=True), reads=[r_sq2[b], r_ones], writes=[r_ps[pb]])
                P.add("act", lambda h, pb=pb: h.activation(out=rs2[:], in_=ps[pb][:], func=AF.Sqrt, scale=1.0 / 128.0, bias=SUBLN_EPS), reads=[r_ps[pb]], writes=[r_rs2])
                P.add("dve", lambda h: h.reciprocal(out=rs2[:], in_=rs2[:]), reads=[r_rs2], writes=[r_rs2])
                P.add("dve", lambda h, b=b, c=c, gs=gs: h.scalar_tensor_tensor(out=yT[:, c, gs], in0=od[b][:], scalar=sgc[:, 0:1], in1=rs2[:],
                                                                                 op0=ALU.mult, op1=ALU.mult), reads=[r_od[b], r_sg, r_rs2], writes=[r_y[c][G]])

    gt = [A.sb([128, 512], BF16) for _ in range(3)]
    r_gt = [Res() for _ in range(3)]
    gview = gateT_d.rearrange("(c p) t -> p c t", p=128)
    kk = 0
    for c in range(8):
        for G in range(NG):
            b = kk % 3
            kk += 1
            gs = slice(G * 512, (G + 1) * 512)
            P.dma("sp", gt[b][:], gview[:, c, gs], writes=[r_gt[b]])
            P.add("act", lambda h, b=b: h.activation(out=gt[b][:], in_=gt[b][:], func=AF.Silu), reads=[r_gt[b]], writes=[r_gt[b]])
            P.add("dve", lambda h, b=b, c=c, gs=gs: h.tensor_tensor(out=yT[:, c, gs], in0=yT[:, c, gs], in1=gt[b][:], op=ALU.mult),
                  reads=[r_gt[b], r_y[c][G]], writes=[r_y[c][G]])

    wo = A.sb([128, 8, D], BF16)
    r_wo = [Res() for _ in range(8)]
    wo_v = wout_d.rearrange("(c p) n -> p c n", p=128)
    for j in range(8):
        b = cnt["st"] % 2
        cnt["st"] += 1
        P.dma("sp", wst[b][:], wo_v[:, :, j * WS:(j + 1) * WS], writes=[r_wst[b]])
        for c in range(8):
            if c % 2 == 0:
                P.add("dve", lambda h, b=b, c=c, j=j: h.tensor_copy(out=wo[:, c, j * WS:(j + 1) * WS], in_=wst[b][:, c, :]), reads=[r_wst[b]], writes=[r_wo[j]])
            else:
                P.add("act", lambda h, b=b, c=c, j=j: h.activation(out=wo[:, c, j * WS:(j + 1) * WS], in_=wst[b][:, c, :], func=AF.Copy), reads=[r_wst[b]], writes=[r_wo[j]])
    for G in range(NG):
        gs = slice(G * 512, (G + 1) * 512)
        for co in range(8):
            pb = nps()
            for c in range(8):
                P.add("pe", lambda h, pb=pb, c=c, co=co, gs=gs: h.matmul(ps[pb][:], wo[:, c, co * 128:(co + 1) * 128], yT[:, c, gs], start=(c == 0), stop=(c == 7)),
                      reads=[r_wo[co], r_y[c][G]], writes=[r_ps[pb]])
            P.add("dve", lambda h, pb=pb, co=co, gs=gs: h.tensor_tensor(out=xT_sb[:, co, gs], in0=xT_sb[:, co, gs], in1=ps[pb][:], op=ALU.add),
                  reads=[r_ps[pb], xT_res[G]], writes=[xT_res[G]])
    if xT_out is not None:
        xo = xT_out.rearrange("(c p) t -> p c t", p=128)
        for G in range(NG):
            gs = slice(G * 512, (G + 1) * 512)
            P.dma("sp", xo[:, :, gs], xT_sb[:, :, gs], reads=[xT_res[G]])
    if final is not None:
        fg = A.sb([128, 8], F32)
        r_fg = Res()
        P.dma("sp", fg[:], final["gcol"], writes=[r_fg])
        ob = [A.sb([128, 512], F32) for _ in range(2)]
        r_ob = [Res() for _ in range(2)]
        fo = final["out"].rearrange("(c p) t -> p c t", p=128)
        k = 0
        for G in range(NG):
            gs = slice(G * 512, (G + 1) * 512)
            pb = nps()
            for c in range(8):
                b = c % 2
                P.add("act", lambda h, b=b, c=c, gs=gs: h.activation(out=sqm[b][:], in_=xT_sb[:, c, gs], func=AF.Square), reads=[xT_res[G]], writes=[r_sqm[b]])
                P.add("pe", lambda h, b=b, c=c, pb=pb: h.matmul(ps[pb][:], ones_bf[:], sqm[b][:], start=(c == 0), stop=(c == 7)), reads=[r_sqm[b], r_ones], writes=[r_ps[pb]])
            P.add("act", lambda h, pb=pb: h.activation(out=rstd[:], in_=ps[pb][:], func=AF.Sqrt, scale=1.0 / D, bias=RMS_EPS), reads=[r_ps[pb]], writes=[r_rstd])
            P.add("dve", lambda h: h.reciprocal(out=rstd[:], in_=rstd[:]), reads=[r_rstd], writes=[r_rstd])
            for c in range(8):
                b = k % 2
                k += 1
                P.add("dve", lambda h, b=b, c=c, gs=gs: h.scalar_tensor_tensor(out=ob[b][:], in0=xT_sb[:, c, gs], scalar=fg[:, c:c + 1], in1=rstd[:],
                                                                                 op0=ALU.mult, op1=ALU.mult), reads=[xT_res[G], r_fg, r_rstd], writes=[r_ob[b]])
                P.dma("sp", fo[:, c, gs], ob[b][:], reads=[r_ob[b]])


def build_phase_c_prog(kind, layer, last):
    nc = bass.Bass("TRN2", target_bir_lowering=False)
    vd = 128 if kind == 2 else 64
    xT_d = nc.dram_tensor("xT", [D, TL], F32, kind="ExternalInput").ap()
    recv_o = nc.dram_tensor("recv_o", [4, 3, vd, TL], BF16, kind="ExternalInput").ap()
    gateT = nc.dram_tensor("gateT", [D, TL], BF16, kind="ExternalInput").ap()
    qmT = nc.dram_tensor("qmT", [256, TL], BF16, kind="ExternalInput").ap()
    memT = nc.dram_tensor("memT", [D, 256], F32, kind="ExternalInput").ap()
    mgcol = nc.dram_tensor("mgcol", [128, 8], F32, kind="ExternalInput").ap()
    wkv = nc.dram_tensor("wkv", [D, 512], F32, kind="ExternalInput").ap()
    wout = nc.dram_tensor("wout", [D, D], F32, kind="ExternalInput").ap()
    sel = nc.dram_tensor("sel", [128, 64], F32, kind="ExternalInput").ap()
    diffp = None
    if kind == 2:
        diffp = {nm: nc.dram_tensor(nm, [1, 64], F32, kind="ExternalInput").ap() for nm in ("lq1", "lk1", "lq2", "lk2")}
        diffp["sg"] = nc.dram_tensor("sg", [128, 1], F32, kind="ExternalInput").ap()
    final = None
    xT_out = None
    if last:
        final = dict(gcol=nc.dram_tensor("fgcol", [128, 8], F32, kind="ExternalInput").ap(),
                     out=nc.dram_tensor("outT", [D, TL], F32, kind="ExternalOutput").ap())
    else:
        xT_out = nc.dram_tensor("xT_out", [D, TL], F32, kind="ExternalOutput").ap()
    P = Prog(nc)
    A = SB(nc)
    xT_sb = A.sb([128, 8, TL], F32)
    xres = [Res() for _ in range(TL // 512)]
    phase_c(nc, P, A, kind, layer, xres, xT_sb, recv_o, gateT, qmT, memT, mgcol, wkv, wout, sel, diffp=diffp, final=final,
            xT_out=xT_out, load_x_from=xT_d)
    P.emit()
    return nc, P


_PROGS = {}
DEBUG = {}


def _prog(key, builder):
    if key not in _PROGS:
        _PROGS[key] = builder()[0]
    return _PROGS[key]


def _col8(v):
    return np.ascontiguousarray(np.asarray(v, np.float32).reshape(8, 128).T)


def kernel_unfused(x, mem, norm_g, w_in, w_out, mem_norm_g, w_mem_kv, diff_lambda_q1, diff_lambda_k1,
                   diff_lambda_q2, diff_lambda_k2, diff_subln_g, final_norm_g):
    x = np.asarray(x, np.float32)
    mem = np.asarray(mem, np.float32)
    cores = list(range(NCORE))
    xT = [np.ascontiguousarray(x[c // 4, (c % 4) * TL:(c % 4 + 1) * TL, :].T) for c in cores]
    memT = [np.ascontiguousarray(mem[b].T) for b in range(B)]
    sel = np.zeros((128, 64), np.float32)
    sel[64, :] = 1.0
    out = None
    for layer in range(DEPTH):
        kind = layer % 3
        vd = 128 if kind == 2 else 64
        last = layer == DEPTH - 1
        ncA = _prog(("A", kind), lambda: build_phase_a_prog(kind))
        wl = np.ascontiguousarray(np.asarray(w_in[layer], np.float32))
        gcol = _col8(norm_g[layer])
        resA = run_bass_kernel_spmd(ncA, [{"xT": xT[c], "w_in": wl, "gcol": gcol} for c in cores], core_ids=cores).results
        in_b = []
        for c in cores:
            b, g = c // 4, c % 4
            rq = np.stack([resA[b * 4 + r]["send_qk"][g] for r in range(4)], axis=0)
            rv = np.stack([resA[b * 4 + r]["send_v"][g] for r in range(4)], axis=0)
            m = {"recv_qk": np.ascontiguousarray(rq), "recv_v": np.ascontiguousarray(rv)}
            m.update(phase_b_consts(kind, g))
            in_b.append(m)
        ncB = _prog(("B", kind), lambda: build_phase_b_prog(kind))
        resB = run_bass_kernel_spmd(ncB, in_b, core_ids=cores).results
        in_c = []
        for c in cores:
            b, r = c // 4, c % 4
            ro = np.stack([resB[b * 4 + g]["oT"][:, :, r * TL:(r + 1) * TL] for g in range(4)], axis=0)
            m = {"xT": xT[c], "recv_o": np.ascontiguousarray(ro), "gateT": resA[c]["gateT"], "qmT": resA[c]["qmT"],
                 "memT": memT[b], "mgcol": _col8(mem_norm_g[layer]),
                 "wkv": np.ascontiguousarray(np.asarray(w_mem_kv[layer], np.float32)),
                 "wout": np.ascontiguousarray(np.asarray(w_out[layer], np.float32)), "sel": sel}
            if kind == 2:
                ci = layer // 3
                m["lq1"] = np.asarray(diff_lambda_q1[ci], np.float32).reshape(1, 64)
                m["lk1"] = np.asarray(diff_lambda_k1[ci], np.float32).reshape(1, 64)
                m["lq2"] = np.asarray(diff_lambda_q2[ci], np.float32).reshape(1, 64)
                m["lk2"] = np.asarray(diff_lambda_k2[ci], np.float32).reshape(1, 64)
                m["sg"] = np.asarray(diff_subln_g[ci], np.float32).reshape(128, 1)
            if last:
                m["fgcol"] = _col8(final_norm_g)
            in_c.append(m)
        ncC = _prog(("C", kind, layer, last), lambda: build_phase_c_prog(kind, layer, last))
        resC = run_bass_kernel_spmd(ncC, in_c, core_ids=cores).results
        if last:
            out = np.empty((B, S, D), np.float32)
            for c in cores:
                out[c // 4, (c % 4) * TL:(c % 4 + 1) * TL, :] = resC[c]["outT"].T
        else:
            xT = [resC[c]["xT_out"] for c in cores]
            if "dump" in DEBUG:
                xs = np.empty((B, S, D), np.float32)
                for c in cores:
                    xs[c // 4, (c % 4) * TL:(c % 4 + 1) * TL, :] = xT[c].T
                DEBUG["dump"].append(xs)
    return out


KINDS = [l % 3 for l in range(DEPTH)]


def build_fused():
    nc = bass.Bass("TRN2", target_bir_lowering=False)

    def din(name, shape, dt=F32):
        return nc.dram_tensor(name, shape, dt, kind="ExternalInput").ap()

    def dint(name, shape, dt):
        return nc.dram_tensor(name, shape, dt, kind="Internal").ap()

    xT_in = din("xT_in", [4, D, TL])
    w_in = din("w_in", [DEPTH, D, INW])
    w_out = din("w_out", [DEPTH, D, D])
    wkv = din("wkv", [DEPTH, D, 512])
    gcols = din("gcols", [DEPTH, 128, 8])
    mgcols = din("mgcols", [DEPTH, 128, 8])
    fgcol = din("fgcol", [128, 8])
    memT = din("memT", [D, 256])
    sel = din("sel", [128, 64])
    diffp = {nm: din(nm, [1, 64]) for nm in ("lq1", "lk1", "lq2", "lk2")}
    diffp["sg"] = din("sg", [128, 1])
    ident = din("ident", [128, 128], BF16)
    kones = din("kones", [35, S], BF16)
    masks = {0: din("masks0", [128, 33, 512], BF16), 1: din("masks1", [128, 4, 512], BF16)}
    masks[2] = masks[1]
    qrow = {k: din(f"qrow{k}", [4, 3, 3, S], BF16) for k in (0, 1, 2)}
    biast = {k: din(f"biast{k}", [4, 128, 3, 64]) for k in (0, 1, 2)}
    outT = nc.dram_tensor("outT", [4, D, TL], F32, kind="ExternalOutput").ap()

    xbuf = dint("xbuf", [2, 4, D, TL], F32)
    qkbuf = dint("qkbuf", [4, 4, 2, 3, 64, TL], BF16)
    vbuf = {64: dint("vbuf64", [4, 4, 3, TL, 64], BF16), 128: dint("vbuf128", [4, 4, 3, TL, 128], BF16)}
    gbuf = dint("gbuf", [4, D, TL], BF16)
    qmbuf = dint("qmbuf", [4, 256, TL], BF16)
    obuf = {64: dint("obuf64", [4, 3, 64, S], BF16), 128: dint("obuf128", [4, 3, 128, S], BF16)}

    P = Prog(nc)
    A = SB(nc, arena_words=48 * 1024 - 64)
    for layer in range(DEPTH):
        kind = KINDS[layer]
        vd = 128 if kind == 2 else 64
        last = layer == DEPTH - 1
        for s_ in range(4):
            A.reset()
            xT_sb = A.sb([128, 8, TL], F32)
            xres = [Res() for _ in range(TL // 512)]
            src = xT_in[s_] if layer == 0 else xbuf[layer % 2, s_]
            phase_a(nc, P, A, kind, xres, xT_sb, w_in[layer], gcols[layer], qkbuf[s_], vbuf[vd][s_], gbuf[s_], qmbuf[s_], load_x_from=src)
            P.barrier()
        for g in range(4):
            A.reset()
            cd = dict(ident=ident, sel=sel, kones=kones, masks=masks[kind], qrow=qrow[kind][g], biast=biast[kind][g])
            phase_b(nc, P, A, kind, qkbuf[:, g], vbuf[vd][:, g], cd, obuf[vd][g])
            P.barrier()
        for s_ in range(4):
            A.reset()
            xT_sb = A.sb([128, 8, TL], F32)
            xres = [Res() for _ in range(TL // 512)]
            src = xT_in[s_] if layer == 0 else xbuf[layer % 2, s_]
            phase_c(nc, P, A, kind, layer, xres, xT_sb, obuf[vd][:, :, :, s_ * TL:(s_ + 1) * TL], gbuf[s_], qmbuf[s_], memT, mgcols[layer],
                    wkv[layer], w_out[layer], sel, diffp=(diffp if kind == 2 else None),
                    final=(dict(gcol=fgcol, out=outT[s_]) if last else None),
                    xT_out=(None if last else xbuf[(layer + 1) % 2, s_]), load_x_from=src)
            P.barrier()
    P.emit()
    return nc, P


_FUSED = {}


def kernel(x, mem, norm_g, w_in, w_out, mem_norm_g, w_mem_kv, diff_lambda_q1, diff_lambda_k1,
           diff_lambda_q2, diff_lambda_k2, diff_subln_g, final_norm_g):
    x = np.asarray(x, np.float32)
    mem = np.asarray(mem, np.float32)
    cores = list(range(NCORE))
    if "nc" not in _FUSED:
        _FUSED["nc"] = build_fused()[0]
    nc = _FUSED["nc"]
    f32 = lambda a: np.ascontiguousarray(np.asarray(a, np.float32))
    sel = np.zeros((128, 64), np.float32)
    sel[64, :] = 1.0
    shared = {
        "w_in": f32(w_in), "w_out": f32(w_out), "wkv": f32(w_mem_kv),
        "gcols": np.stack([_col8(norm_g[l]) for l in range(DEPTH)]),
        "mgcols": np.stack([_col8(mem_norm_g[l]) for l in range(DEPTH)]),
        "fgcol": _col8(final_norm_g), "sel": sel,
        "lq1": f32(diff_lambda_q1[0]).reshape(1, 64), "lk1": f32(diff_lambda_k1[0]).reshape(1, 64),
        "lq2": f32(diff_lambda_q2[0]).reshape(1, 64), "lk2": f32(diff_lambda_k2[0]).reshape(1, 64),
        "sg": f32(diff_subln_g[0]).reshape(128, 1),
    }
    for k in (0, 1, 2):
        cs = [phase_b_consts(k, g) for g in range(4)]
        shared[f"qrow{k}"] = np.stack([c["qrow"] for c in cs])
        shared[f"biast{k}"] = np.stack([c["biast"] for c in cs])
        if k < 2:
            shared[f"masks{k}"] = cs[0]["masks"]
        shared["ident"] = cs[0]["ident"]
        shared["kones"] = cs[0]["kones"]
    in_maps = []
    for c in cores:
        b = c // 4
        m = dict(shared)
        m["xT_in"] = np.ascontiguousarray(x[b].reshape(4, TL, D).transpose(0, 2, 1))
        m["memT"] = np.ascontiguousarray(mem[b].T)
        in_maps.append(m)
    res = run_bass_kernel_spmd(nc, in_maps, core_ids=cores).results
    out = np.empty((B, S, D), np.float32)
    for c in cores:
        b, r = c // 4, c % 4
        out[b, r * TL:(r + 1) * TL, :] = res[c]["outT"][r].T
    return out
```

```python
import numpy as np
import ml_dtypes
import concourse.bass as bass
import concourse.mybir as mybir
from concourse.bass_utils import run_bass_kernel_spmd

F32 = mybir.dt.float32
BF16 = mybir.dt.bfloat16
AF = mybir.ActivationFunctionType
ALU = mybir.AluOpType
AX = mybir.AxisListType
NPBF = ml_dtypes.bfloat16

D = 1024
S = 8192
B = 2
DEPTH = 4
TL = 2048
NCORE = 8
MIXW = 768
INW = 3584
NEG = -30000.0
RMS_EPS = 1e-6
SUBLN_EPS = 1e-5


class LazyAP:
    def __init__(self, base, off, ops=()):
        self.base = base
        self.off = off
        self.ops = tuple(ops)

    def __getitem__(self, idx):
        if not isinstance(idx, tuple):
            idx = (idx,)
        return LazyAP(self.base, self.off, self.ops + (("idx", idx),))

    def rearrange(self, pat, **kw):
        return LazyAP(self.base, self.off, self.ops + (("re", pat, kw),))

    def make(self, h):
        ap = self.base
        for op in self.ops:
            if op[0] == "idx":
                ap = ap[(slice(None),) + op[1]]
            else:
                lhs, rhs = op[1].split("->")
                ap = ap.rearrange("zz " + lhs.strip() + " -> zz " + rhs.strip(), **op[2])
        nd = len(ap.shape)
        names = _LET[:nd - 1]
        pat = "o " + " ".join(names) + " -> (o " + names[0] + ")" + ("".join(" " + n for n in names[1:]))
        key = (id(h), self.off)
        if key not in _PID_CACHE:
            _PID_CACHE[key] = (h.partition_id() + self.off) % 4
        return ap[bass.ds(_PID_CACHE[key], 1)].rearrange(pat)


_PID_CACHE = {}


def _resolve(ap, h):
    return ap.make(h) if isinstance(ap, LazyAP) else ap


def core_slice(ap, off):
    return LazyAP(ap, off)


class Res:
    __slots__ = ("name", "lw", "readers")

    def __init__(self, name=""):
        self.name = name
        self.lw = None
        self.readers = []


class Op:
    __slots__ = ("eng", "fn", "dma", "sem", "semval", "needs_inc", "idx", "waits", "gen")


class Prog:
    ENGS = ("pe", "act", "dve", "pool", "sp")

    def __init__(self, nc, same_sync=("act", "dve", "pool"), ndma=12):
        self.nc = nc
        self.h = dict(pe=nc.tensor, act=nc.scalar, dve=nc.vector, pool=nc.gpsimd, sp=nc.sync)
        self.ops = {e: [] for e in self.ENGS}
        self.obs = {e: {} for e in self.ENGS}
        self.same_sync = set(same_sync)
        self.same_dist = 4
        self.esems = {e: [nc.alloc_semaphore(name=f"es_{e}_0")] for e in ("pe", "act", "dve", "pool")}
        self.gen = {e: 0 for e in ("pe", "act", "dve", "pool", "sp")}
        self.gcount = {e: 0 for e in ("pe", "act", "dve", "pool")}
        self.pending = {e: [] for e in self.ENGS}
        self.ndma = ndma
        self.dsem = {q: [nc.alloc_semaphore(name=f"ds_{q}_{i}") for i in range(ndma)] for q in ("sp", "pool", "act")}
        self.dlast = {q: [None] * ndma for q in ("sp", "pool", "act")}
        self.dcnt = {q: 0 for q in ("sp", "pool", "act")}
        self.all_dma = []

    def add(self, eng, fn, reads=(), writes=(), dma=False):
        op = Op()
        op.eng = eng
        op.fn = fn
        op.dma = dma
        op.needs_inc = False
        op.sem = None
        op.semval = None
        op.idx = len(self.ops[eng])
        op.gen = self.gen[eng]
        deps = []
        if self.pending[eng]:
            deps.extend(self.pending[eng])
            self.pending[eng] = []
        for r in reads:
            if r.lw is not None:
                deps.append(r.lw)
        for w in writes:
            if w.lw is not None:
                deps.append(w.lw)
            deps.extend(w.readers)
        if dma:
            n = self.dcnt[eng]
            slot = n % self.ndma
            prev = self.dlast[eng][slot]
            if prev is not None:
                deps.append(prev)
            self.dlast[eng][slot] = op
            op.sem = self.dsem[eng][slot]
            op.semval = 16 * (n // self.ndma + 1)
            self.dcnt[eng] = n + 1
            self.all_dma.append(op)
        waits = []
        obs = self.obs[eng]
        for p in deps:
            if p is op:
                continue
            if p.dma:
                key = ("d", id(p.sem))
                if obs.get(key, 0) >= p.semval:
                    continue
                obs[key] = p.semval
                waits.append(p)
            else:
                if p.eng == eng and (eng not in self.same_sync or op.idx - p.idx >= self.same_dist):
                    continue
                key = ("e", p.eng)
                if obs.get(key, -1) >= p.idx:
                    continue
                obs[key] = p.idx
                if not p.needs_inc:
                    p.needs_inc = True
                    self.gcount[p.eng] += 1
                waits.append(p)
        op.waits = waits
        for r in reads:
            r.readers.append(op)
        for w in writes:
            w.lw = op
            w.readers = []
        self.ops[eng].append(op)
        return op

    def barrier(self, rotate_at=12000):
        lasts = []
        for e in ("pe", "act", "dve", "pool"):
            for op in reversed(self.ops[e]):
                if not op.dma:
                    if not op.needs_inc:
                        op.needs_inc = True
                        self.gcount[e] += 1
                    lasts.append(op)
                    break
        for q in self.dlast:
            for op in self.dlast[q]:
                if op is not None:
                    lasts.append(op)
        for e in self.ENGS:
            self.pending[e] = list(lasts)
        for e in ("pe", "act", "dve", "pool"):
            if self.gcount[e] > rotate_at:
                self.gen[e] += 1
                self.gcount[e] = 0
                self.esems[e].append(self.nc.alloc_semaphore(name=f"es_{e}_{self.gen[e]}"))

    def dma(self, q, out, in_, reads=(), writes=()):
        return self.add(q, lambda h: h.dma_start(out=_resolve(out, h), in_=_resolve(in_, h)), reads, writes, dma=True)

    def emit(self):
        nc = self.nc
        final_waits = {}
        for op in self.all_dma:
            k = id(op.sem)
            if k not in final_waits or final_waits[k][1] < op.semval:
                final_waits[k] = (op.sem, op.semval)
        for e in ("pe", "act", "dve", "pool"):
            c = {}
            for op in self.ops[e]:
                if op.needs_inc and not op.dma:
                    c[op.gen] = c.get(op.gen, 0) + 1
                    op.sem = self.esems[e][op.gen]
                    op.semval = c[op.gen]
        self.max_semval = {e: sum(1 for o in self.ops[e] if o.needs_inc) for e in ("pe", "act", "dve", "pool")}

        def run(e):
            h = self.h[e]
            for op in self.ops[e]:
                for p in op.waits:
                    h.wait_ge(p.sem, p.semval)
                ins = op.fn(h)
                if op.dma:
                    ins.then_inc(op.sem, 16)
                elif op.needs_inc:
                    ins.then_inc(op.sem, 1)
            if e == "sp":
                for p in self.pending[e]:
                    h.wait_ge(p.sem, p.semval)
                for sem, val in final_waits.values():
                    h.wait_ge(sem, val)

        with nc.Block() as block:
            @block.tensor
            def _(t):
                run("pe")

            @block.scalar
            def _(t):
                run("act")

            @block.vector
            def _(t):
                run("dve")

            @block.gpsimd
            def _(t):
                run("pool")

            @block.sync
            def _(t):
                run("sp")


_LET = "abcdefgh"


def _view(ap2d, shape):
    dims = list(shape[1:])
    if len(dims) > 1:
        names = " ".join(_LET[:len(dims)])
        kw = {_LET[i]: dims[i] for i in range(len(dims))}
        ap2d = ap2d.rearrange(f"p ({names}) -> p {names}", **kw)
    if shape[0] != 128:
        ap2d = ap2d[0:shape[0]]
    return ap2d


class SB:
    def __init__(self, nc, arena_words=None):
        self.nc = nc
        self.n = 0
        self.arena = None
        if arena_words is not None:
            self.arena = nc.alloc_sbuf_tensor("arena", [128, arena_words], F32)
            self.words = arena_words
            self.off = 0
            self.banks = [nc.alloc_psum_tensor(f"bank{i}", [128, 512], F32) for i in range(8)]
            self.pi = 0

    def reset(self, keep=0):
        self.off = keep
        self.pi = 0

    def sb(self, shape, dt, name=None):
        self.n += 1
        if self.arena is None:
            return self.nc.alloc_sbuf_tensor(name or f"sb{self.n}", shape, dt)
        n = int(np.prod(shape[1:]))
        esz = 4 if dt == F32 else 2
        words = (n * esz + 3) // 4
        words = (words + 7) // 8 * 8
        assert self.off + words <= self.words, f"SBUF arena overflow: {self.off}+{words} > {self.words}"
        v = self.arena[:, self.off:self.off + words]
        self.off += words
        if dt != F32:
            v = v.bitcast(dt)
        v = v[:, 0:n]
        return _view(v, shape)

    def ps(self, shape, dt, name=None):
        self.n += 1
        if self.arena is None:
            return self.nc.alloc_psum_tensor(name or f"ps{self.n}", shape, dt)
        assert self.pi < 8, "out of PSUM banks"
        b = self.banks[self.pi]
        self.pi += 1
        n = int(np.prod(shape[1:]))
        return _view(b[:, 0:n], shape)


def unit_cols(kind, g, u):
    if kind in (0, 1):
        hh = 4 * u + g
        return hh * 64, MIXW + hh * 64, 2 * MIXW + hh * 64, 64
    mm = 3 * g + u
    head, m = mm // 2, mm % 2
    return head * 128 + m * 64, MIXW + head * 128 + m * 64, 2 * MIXW + head * 128, 128


def alibi_slopes(n):
    return (2.0 ** (-8.0 * np.arange(1, n + 1) / n)).astype(np.float64)


def phase_a(nc, P, A, kind, xT_res, xT_sb, wA, gcol_d, send_qk, send_v, gateT_d, qmT_d, load_x_from=None):
    vd = 128 if kind == 2 else 64
    NG = TL // 512
    ones_bf = A.sb([128, 128], BF16)
    r_ones = Res()
    P.add("pool", lambda h: h.memset(ones_bf[:], 1.0), writes=[r_ones])
    gcol = A.sb([128, 8], F32)
    r_g = Res()
    P.dma("sp", gcol[:], gcol_d, writes=[r_g])
    if load_x_from is not None:
        xv = load_x_from.rearrange("(c p) t -> p c t", p=128)
        for G in range(NG):
            P.dma("sp", xT_sb[:, :, G * 512:(G + 1) * 512], xv[:, :, G * 512:(G + 1) * 512], writes=[xT_res[G]])

    hT = A.sb([128, 8, TL], BF16)
    r_h = [Res() for _ in range(NG)]
    sq = [A.sb([128, 512], BF16) for _ in range(2)]
    r_sq = [Res() for _ in range(2)]
    ss_ps = A.ps([128, 512], F32)
    r_ss = Res()
    rstd = A.sb([128, 512], F32)
    r_rstd = Res()
    i = 0
    for G in range(NG):
        gs = slice(G * 512, (G + 1) * 512)
        for c in range(8):
            b = i % 2
            i += 1
            P.add("act", lambda h, b=b, c=c, gs=gs: h.activation(out=sq[b][:], in_=xT_sb[:, c, gs], func=AF.Square),
                  reads=[xT_res[G]], writes=[r_sq[b]])
            P.add("pe", lambda h, b=b, c=c: h.matmul(ss_ps[:], ones_bf[:], sq[b][:], start=(c == 0), stop=(c == 7)),
                  reads=[r_sq[b], r_ones], writes=[r_ss])
        P.add("act", lambda h: h.activation(out=rstd[:], in_=ss_ps[:], func=AF.Sqrt, scale=1.0 / D, bias=RMS_EPS),
              reads=[r_ss], writes=[r_rstd])
        P.add("dve", lambda h: h.reciprocal(out=rstd[:], in_=rstd[:]), reads=[r_rstd], writes=[r_rstd])
        for c in range(8):
            P.add("dve", lambda h, c=c, gs=gs: h.tensor_tensor(out=hT[:, c, gs], in0=xT_sb[:, c, gs], in1=rstd[:], op=ALU.mult),
                  reads=[xT_res[G], r_rstd], writes=[r_h[G]])

    CG = 256
    ncg = INW // CG
    wst = [A.sb([128, 8, CG], F32) for _ in range(2)]
    r_wst = [Res() for _ in range(2)]
    wb = [A.sb([128, 8, CG], BF16) for _ in range(2)]
    r_wb = [Res() for _ in range(2)]
    wv = wA.rearrange("(c p) n -> p c n", p=128)
    ev = [A.sb([128, TL], BF16) for _ in range(2)]
    r_ev = [Res() for _ in range(2)]
    vsb = [A.sb([128, 16, CG], BF16) for _ in range(2)]
    r_vsb = [Res() for _ in range(2)]
    pacc = [A.ps([128, 512], F32) for _ in range(4)]
    r_pacc = [Res() for _ in range(4)]
    pi = 0
    evi = 0
    vi = 0
    dest = {}
    for g in range(4):
        for u in range(3):
            qc, kc, vc, _ = unit_cols(kind, g, u)
            dest[qc] = (g, 0, u)
            dest[kc] = (g, 1, u)
    vdest = {}
    for g in range(4):
        for u in range(3):
            qc, kc, vc, _ = unit_cols(kind, g, u)
            vdest.setdefault(vc, []).append((g, u))
    for cg in range(ncg):
        b = cg % 2
        c0 = cg * CG
        P.dma("sp" if cg % 2 == 0 else "pool", wst[b][:], wv[:, :, c0:c0 + CG], writes=[r_wst[b]])
        for c in range(8):
            if c % 2 == 0:
                P.add("dve", lambda h, b=b, c=c: h.tensor_scalar(out=wb[b][:, c, :], in0=wst[b][:, c, :], scalar1=gcol[:, c:c + 1],
                                                                  scalar2=None, op0=ALU.mult),
                      reads=[r_wst[b], r_g], writes=[r_wb[b]])
            else:
                P.add("act", lambda h, b=b, c=c: h.activation(out=wb[b][:, c, :], in_=wst[b][:, c, :], func=AF.Copy, scale=gcol[:, c:c + 1]),
                      reads=[r_wst[b], r_g], writes=[r_wb[b]])
        if 2 * MIXW <= c0 < 3 * MIXW:
            vb = vi % 2
            vi += 1
            for tt in range(16):
                pb = pi % 4
                pi += 1
                for c in range(8):
                    P.add("pe", lambda h, pb=pb, c=c, tt=tt, b=b: h.matmul(pacc[pb][:, 0:CG], hT[:, c, tt * 128:(tt + 1) * 128],
                                                                          wb[b][:, c, :], start=(c == 0), stop=(c == 7)),
                          reads=[r_h[tt // 4], r_wb[b]], writes=[r_pacc[pb]])
                eng = "act" if tt % 2 == 0 else "dve"
                if eng == "act":
                    P.add("act", lambda h, pb=pb, vb=vb, tt=tt: h.activation(out=vsb[vb][:, tt, :], in_=pacc[pb][:, 0:CG], func=AF.Copy),
                          reads=[r_pacc[pb]], writes=[r_vsb[vb]])
                else:
                    P.add("dve", lambda h, pb=pb, vb=vb, tt=tt: h.tensor_copy(out=vsb[vb][:, tt, :], in_=pacc[pb][:, 0:CG]),
                          reads=[r_pacc[pb]], writes=[r_vsb[vb]])
            for off in range(0, CG, vd):
                col = c0 + off
                for (g, u) in vdest.get(col, []):
                    dst = send_v[g, u].rearrange("(t p) v -> p t v", p=128)
                    P.dma("sp", dst, vsb[vb][:, :, off:off + vd], reads=[r_vsb[vb]])
            continue
        for ch in range(CG // 128):
            col = c0 + ch * 128
            isq = col < MIXW or (3 * MIXW <= col < 3 * MIXW + 256)
            eb = evi % 2
            evi += 1
            for G in range(NG):
                gs = slice(G * 512, (G + 1) * 512)
                pb = pi % 4
                pi += 1
                for c in range(8):
                    P.add("pe", lambda h, pb=pb, c=c, gs=gs, b=b, ch=ch: h.matmul(pacc[pb][:], wb[b][:, c, ch * 128:(ch + 1) * 128],
                                                                                  hT[:, c, gs], start=(c == 0), stop=(c == 7)),
                          reads=[r_h[G], r_wb[b]], writes=[r_pacc[pb]])
                sc = 0.125 if isq else 1.0
                if G % 2 == 0:
                    P.add("act", lambda h, pb=pb, eb=eb, gs=gs, sc=sc: h.activation(out=ev[eb][:, gs], in_=pacc[pb][:], func=AF.Copy, scale=sc),
                          reads=[r_pacc[pb]], writes=[r_ev[eb]])
                else:
                    P.add("dve", lambda h, pb=pb, eb=eb, gs=gs, sc=sc: h.tensor_scalar(out=ev[eb][:, gs], in0=pacc[pb][:], scalar1=sc, scalar2=None, op0=ALU.mult),
                          reads=[r_pacc[pb]], writes=[r_ev[eb]])
            if col < 2 * MIXW:
                for half in range(2):
                    g, qk, u = dest[col + half * 64]
                    P.dma("sp", send_qk[g, qk, u], ev[eb][half * 64:(half + 1) * 64, :], reads=[r_ev[eb]])
            elif col < 3 * MIXW + 256:
                r0 = col - 3 * MIXW
                P.dma("sp", qmT_d[r0:r0 + 128, :], ev[eb][:], reads=[r_ev[eb]])
            else:
                r0 = col - (3 * MIXW + 256)
                P.dma("sp", gateT_d[r0:r0 + 128, :], ev[eb][:], reads=[r_ev[eb]])


def build_phase_a_prog(kind):
    nc = bass.Bass("TRN2", target_bir_lowering=False)
    vd = 128 if kind == 2 else 64
    xT_d = nc.dram_tensor("xT", [D, TL], F32, kind="ExternalInput").ap()
    w_d = nc.dram_tensor("w_in", [D, INW], F32, kind="ExternalInput").ap()
    g_d = nc.dram_tensor("gcol", [128, 8], F32, kind="ExternalInput").ap()
    send_qk = nc.dram_tensor("send_qk", [4, 2, 3, 64, TL], BF16, kind="ExternalOutput").ap()
    send_v = nc.dram_tensor("send_v", [4, 3, TL, vd], BF16, kind="ExternalOutput").ap()
    gateT = nc.dram_tensor("gateT", [D, TL], BF16, kind="ExternalOutput").ap()
    qmT = nc.dram_tensor("qmT", [256, TL], BF16, kind="ExternalOutput").ap()
    P = Prog(nc)
    A = SB(nc)
    xT_sb = A.sb([128, 8, TL], F32)
    xres = [Res() for _ in range(TL // 512)]
    phase_a(nc, P, A, kind, xres, xT_sb, w_d, g_d, send_qk, send_v, gateT, qmT, load_x_from=xT_d)
    P.emit()
    return nc, P


DSW = ((128, 1), (512, 4), (2048, 16))


def unit_slope(kind, g, u):
    if kind in (0, 1):
        return alibi_slopes(12)[4 * u + g]
    return alibi_slopes(6)[(3 * g + u) // 2]


def split3(x):
    hi = x.astype(NPBF)
    r = x - hi.astype(np.float64)
    lo = r.astype(NPBF)
    r2 = r - lo.astype(np.float64)
    lo2 = r2.astype(NPBF)
    return hi, lo, lo2


def phase_b_consts(kind, g):
    c = {}
    c["ident"] = np.eye(128, dtype=np.float32).astype(NPBF)
    sel = np.zeros((128, 64), np.float32)
    sel[64, :] = 1.0
    c["sel"] = sel
    t = np.arange(S, dtype=np.float64)
    qrow = np.zeros((3, 3, S), dtype=NPBF)
    biast = np.zeros((128, 3, 64), dtype=np.float32)
    p = np.arange(128, dtype=np.float64)
    for u in range(3):
        sl = unit_slope(kind, g, u)
        x = -sl * (t - (t // 512) * 512)
        hi, lo, lo2 = split3(x)
        qrow[u, 0], qrow[u, 1], qrow[u, 2] = hi, lo, lo2
        for j in range(64):
            jp = 3 - j
            biast[:, u, j] = (sl * (128.0 * jp + p)).astype(np.float32)
    c["qrow"] = qrow
    c["biast"] = biast
    kones = np.zeros((35, S), dtype=np.float32)
    for b in range(32):
        kones[b, b * 256:(b + 1) * 256] = 1.0
    kones[32:35] = 1.0
    c["kones"] = kones.astype(NPBF)
    q = np.arange(512)[None, :]
    if kind == 0:
        tiles = []
        for (w, d) in DSW:
            for jp in range(-w // 128, 4):
                tk = 128 * jp + np.arange(128)[:, None]
                dist = q - tk
                valid = (dist >= 0) & (dist <= w) & (dist % d == 0)
                tiles.append(np.where(valid, 0.0, NEG))
        m = np.stack(tiles, axis=1)
    else:
        tiles = []
        for i in range(4):
            tk = 128 * i + np.arange(128)[:, None]
            tiles.append(np.where(tk > q, NEG, 0.0))
        m = np.stack(tiles, axis=1)
    c["masks"] = m.astype(np.float32).astype(NPBF)
    return c


def phase_b(nc, P, A, kind, recv_qk, recv_v, cd, oT_d):
    vd = 128 if kind == 2 else 64
    VW = vd + 1
    arow = 96 if kind == 1 else 64
    KA = arow + 3
    NGq = S // 512
    nmask = 33 if kind == 0 else 4
    ident = A.sb([128, 128], BF16)
    r_id = Res()
    P.dma("sp", ident[:], cd["ident"], writes=[r_id])
    biast = A.sb([128, 3, 64], F32)
    r_bt = Res()
    P.dma("sp", biast[:], cd["biast"], writes=[r_bt])
    masks = A.sb([128, nmask, 512], BF16)
    r_mk = Res()
    P.dma("pool", masks[:], cd["masks"], writes=[r_mk])
    ones_f = A.sb([128, 64], F32)
    r_of = Res()
    P.dma("sp", ones_f[:], cd["sel"], writes=[r_of])

    nunit_res = 3 if kind == 0 else 2
    ka = [A.sb([128, S], BF16) for _ in range(nunit_res)]
    r_ka = [Res() for _ in range(nunit_res)]
    va = [A.sb([128, 64, VW], BF16) for _ in range(nunit_res)]
    r_va = [Res() for _ in range(nunit_res)]
    for i in range(nunit_res):
        P.add("pool", lambda h, i=i: h.memset(va[i][:, :, 64:65], 1.0), writes=[r_va[i]])
    NQB = 4
    qa = [A.sb([128, 512], BF16) for _ in range(NQB)]
    r_qa = [Res() for _ in range(NQB)]
    NS = 3
    s_ps = [A.ps([128, 512], F32) for _ in range(NS)]
    r_s = [Res() for _ in range(NS)]
    NPT = 4
    pt = [A.sb([128, 512], BF16) for _ in range(NPT)]
    r_pt = [Res() for _ in range(NPT)]
    if kind == 2:
        oa_ps = [A.ps([128, 512], F32) for _ in range(2)]
        ob_ps = [A.ps([128, 512], F32) for _ in range(2)]
        r_oa = [Res() for _ in range(2)]
        r_ob = [Res() for _ in range(2)]
    elif kind == 0:
        oa_ps = [A.ps([128, 512], F32) for _ in range(4)]
        r_oa = [Res() for _ in range(4)]
    else:
        oa_ps = [A.ps([128, 512], F32) for _ in range(2)]
        r_oa = [Res() for _ in range(2)]
    bc_ps = A.ps([128, 512], F32)
    r_bc = Res()
    rd = A.sb([128, 512], F32)
    r_rd = Res()
    P.add("pool", lambda h: h.memset(rd[:], 0.0), writes=[r_rd])
    osb = [A.sb([64, 512], F32) for _ in range(2)]
    r_osb = [Res() for _ in range(2)]
    outb = [A.sb([64, 512], BF16) for _ in range(4)]
    r_outb = [Res() for _ in range(4)]
    if kind == 1:
        gate_ps = A.ps([128, 4, 32], F32)
        r_gate = Res()
        tr_ps = A.ps([128, 512], F32)
        r_tr = Res()
        gm = A.sb([128, 4, 32], F32)
        r_gm = Res()
        P.add("pool", lambda h: h.memset(gm[:], -1e30), writes=[r_gm])
        mx8 = A.sb([128, 4, 8], F32)
        r_mx = Res()
        negm = A.sb([128, 4, 96], BF16)
        r_negm = Res()
        P.add("pool", lambda h: h.memset(negm[:], 0.0), writes=[r_negm])
        kms = A.sb([64, 32], F32)
        kmb = A.sb([64, 32], BF16)
        r_km = Res()

    cnt = dict(q=0, s=0, pt=0, o=0, osb=0, outb=0)

    def load_unit(u, slot):
        for src in range(4):
            P.dma("sp", ka[slot][0:64, src * TL:(src + 1) * TL], recv_qk[src, 1, u], writes=[r_ka[slot]])
        if kind == 1:
            P.dma("pool", ka[slot][64:99, :], cd["kones"], writes=[r_ka[slot]])
        else:
            P.dma("pool", ka[slot][64:67, :], cd["kones"][32:35, :], writes=[r_ka[slot]])
        for src in range(4):
            sv = recv_v[src, u].rearrange("(t p) v -> p t v", p=128)
            if vd == 64:
                P.dma("pool", va[slot][:, src * 16:(src + 1) * 16, 0:64], sv, writes=[r_va[slot]])
            else:
                P.dma("pool", va[slot][:, src * 16:(src + 1) * 16, 0:64], sv[:, :, 0:64], writes=[r_va[slot]])
                P.dma("pool", va[slot][:, src * 16:(src + 1) * 16, 65:129], sv[:, :, 64:128], writes=[r_va[slot]])

    def load_q(u, G):
        qb = cnt["q"] % NQB
        cnt["q"] += 1
        src, lg = G // 4, G % 4
        P.dma("sp", qa[qb][0:64, :], recv_qk[src, 0, u, :, lg * 512:(lg + 1) * 512], writes=[r_qa[qb]])
        P.dma("sp", qa[qb][arow:arow + 3, :], cd["qrow"][u, :, G * 512:(G + 1) * 512], writes=[r_qa[qb]])
        return qb

    def moba_prep(slot, qb, G):
        for i in range(4):
            P.add("pe", lambda h, i=i: h.matmul(gate_ps[:, i, :], qa[qb][0:64, i * 128:(i + 1) * 128], kmb[:, :], start=True, stop=True),
                  reads=[r_qa[qb], r_km], writes=[r_gate])
        for i in range(4):
            n = (4 * G + i) // 2
            if n > 0:
                P.add("dve", lambda h, i=i, n=n: h.tensor_copy(out=gm[:, i, 0:n], in_=gate_ps[:, i, 0:n]), reads=[r_gate], writes=[r_gm])
            P.add("dve", lambda h, i=i: h.max(out=mx8[:, i, :], in_=gm[:, i, :]), reads=[r_gm], writes=[r_mx])
            P.add("dve", lambda h, i=i: h.tensor_scalar(out=negm[:, i, 64:96], in0=gm[:, i, :], scalar1=mx8[:, i, 2:3], scalar2=1.0,
                                                        op0=ALU.is_ge, op1=ALU.subtract), reads=[r_gm, r_mx], writes=[r_negm])
            P.add("dve", lambda h, i=i, n=n: h.memset(negm[:, i, 64 + n:65 + n], 0.0), writes=[r_negm])

    def moba_prep2(slot, qb, G):
        for i in range(4):
            P.add("pe", lambda h, i=i: h.matmul(tr_ps[0:96, i * 128:(i + 1) * 128], negm[:, i, :], ident[:], start=True, stop=True),
                  reads=[r_negm, r_id], writes=[r_tr])
        P.add("act", lambda h: h.activation(out=qa[qb][64:96, :], in_=tr_ps[64:96, :], func=AF.Copy, scale=-NEG),
              reads=[r_tr], writes=[r_qa[qb]])

    def steps_for(u, G):
        out = []
        if kind == 0:
            w, d = DSW[u]
            nb = w // 128
            moff = [0, 5, 13][u]
            for jp in range(-nb, 4):
                kt = 4 * G + jp
                if kt < 0:
                    continue
                out.append((kt, moff + jp + nb, 3 - jp))
        else:
            for kt in range(0, 4 * G + 4):
                jp = kt - 4 * G
                out.append((kt, jp if jp >= 0 else None, 3 - jp))
        return out

    def attend(u, slot, G, qb, ob):
        attend_flat([(u, slot, G, qb, ob)])

    def attend_flat(segs):
        flat = []
        for (u, slot, G, qb, ob) in segs:
            st = steps_for(u, G)
            for i, (kt, mi, bj) in enumerate(st):
                flat.append((u, slot, qb, ob, kt, mi, bj, i == 0, i == len(st) - 1))
        n = len(flat)
        sb_of = {}
        pt_of = {}

        def qk(i):
            u, slot, qb, ob, kt, mi, bj, first, lastf = flat[i]
            sbk = cnt["s"] % NS
            cnt["s"] += 1
            sb_of[i] = sbk
            P.add("pe", lambda h: h.matmul(s_ps[sbk][:], ka[slot][0:KA, kt * 128:(kt + 1) * 128], qa[qb][0:KA, :], start=True, stop=(mi is None)),
                  reads=[r_ka[slot], r_qa[qb]], writes=[r_s[sbk]])
            if mi is not None:
                P.add("pe", lambda h: h.matmul(s_ps[sbk][:], ident[:], masks[:, mi, :], start=False, stop=True),
                      reads=[r_id, r_mk], writes=[r_s[sbk]])

        def ex(i):
            u, slot, qb, ob, kt, mi, bj, first, lastf = flat[i]
            sbk = sb_of[i]
            pb = cnt["pt"] % NPT
            cnt["pt"] += 1
            pt_of[i] = pb
            P.add("act", lambda h: h.activation(out=pt[pb][:], in_=s_ps[sbk][:], func=AF.Exp, bias=biast[:, u, bj:bj + 1], scale=1.0),
                  reads=[r_s[sbk], r_bt], writes=[r_pt[pb]])

        def pv(i):
            u, slot, qb, ob, kt, mi, bj, first, lastf = flat[i]
            pb = pt_of[i]
            P.add("pe", lambda h: h.matmul(oa_ps[ob][0:65, :], va[slot][:, kt, 0:65], pt[pb][:], start=first, stop=lastf),
                  reads=[r_va[slot], r_pt[pb]], writes=[r_oa[ob]])
            if kind == 2:
                P.add("pe", lambda h: h.matmul(ob_ps[ob][0:64, :], va[slot][:, kt, 65:129], pt[pb][:], start=first, stop=lastf),
                      reads=[r_va[slot], r_pt[pb]], writes=[r_ob[ob]])

        LA = 2
        for i in range(min(LA, n)):
            qk(i)
            ex(i)
        for i in range(n):
            if i + LA < n:
                qk(i + LA)
                ex(i + LA)
            pv(i)

    def finish(u, G, obs):
        gs = slice(G * 512, (G + 1) * 512)
        first = True
        for (uu, ob) in obs:
            if first:
                P.add("dve", lambda h, ob=ob: h.tensor_copy(out=rd[64:65, :], in_=oa_ps[ob][64:65, :]), reads=[r_oa[ob]], writes=[r_rd])
                first = False
            else:
                P.add("dve", lambda h, ob=ob: h.tensor_tensor(out=rd[64:65, :], in0=rd[64:65, :], in1=oa_ps[ob][64:65, :], op=ALU.add),
                      reads=[r_oa[ob], r_rd], writes=[r_rd])
        P.add("dve", lambda h: h.reciprocal(out=rd[64:65, :], in_=rd[64:65, :]), reads=[r_rd], writes=[r_rd])

    def finish2(u, G, obs):
        gs = slice(G * 512, (G + 1) * 512)
        P.add("pe", lambda h: h.matmul(bc_ps[0:64, :], ones_f[:, :], rd[:, :], start=True, stop=True),
              reads=[r_of, r_rd], writes=[r_bc])
        for (uu, ob) in obs:
            parts = [(oa_ps, r_oa, 0)] + ([(ob_ps, r_ob, 64)] if kind == 2 else [])
            for (pst, rr, row0) in parts:
                sb = cnt["osb"] % 2
                cnt["osb"] += 1
                bb = cnt["outb"] % 4
                cnt["outb"] += 1
                P.add("act", lambda h, pst=pst, ob=ob, sb=sb: h.activation(out=osb[sb][:], in_=pst[ob][0:64, :], func=AF.Copy),
                      reads=[rr[ob]], writes=[r_osb[sb]])
                P.add("dve", lambda h, sb=sb, bb=bb: h.tensor_tensor(out=outb[bb][:], in0=osb[sb][:], in1=bc_ps[0:64, :], op=ALU.mult),
                      reads=[r_osb[sb], r_bc], writes=[r_outb[bb]])
                if len(oT_d.shape) == 4:
                    dst = oT_d[G // 4, uu, row0:row0 + 64, (G % 4) * 512:(G % 4 + 1) * 512]
                else:
                    dst = oT_d[uu, row0:row0 + 64, gs]
                P.dma("sp", dst, outb[bb][:], reads=[r_outb[bb]])

    if kind == 0:
        for u in range(3):
            load_unit(u, u)
        pend = None
        for G in range(NGq):
            obs = []
            segs = []
            for idx, u in enumerate((2, 1, 0)):
                qb = load_q(u, G)
                ob = (3 * G + idx) % 4
                segs.append((u, u, G, qb, ob))
                obs.append((u, ob))
            attend_flat(segs[:1])
            if pend is not None:
                finish2(*pend)
            attend_flat(segs[1:])
            finish(0, G, obs)
            pend = (0, G, obs)
        finish2(*pend)
    else:
        load_unit(0, 0)
        for u in range(3):
            slot = u % 2
            if u + 1 < 3:
                load_unit(u + 1, (u + 1) % 2)
            if kind == 1:
                P.add("dve", lambda h, slot=slot: h.tensor_reduce(out=kms[:, :], in_=ka[slot][0:64, :].rearrange("p (b k) -> p b k", k=256),
                                                                   axis=AX.X, op=ALU.add), reads=[r_ka[slot]], writes=[r_km])
                P.add("dve", lambda h: h.tensor_copy(out=kmb[:, :], in_=kms[:, :]), reads=[r_km], writes=[r_km])
                if u > 0:
                    P.add("pool", lambda h: h.memset(gm[:], -1e30), writes=[r_gm])
            qbs = {0: load_q(u, 0)}
            if kind == 1:
                moba_prep(slot, qbs[0], 0)
                moba_prep2(slot, qbs[0], 0)
            pend = None
            for G in range(NGq):
                if G + 1 < NGq:
                    qbs[G + 1] = load_q(u, G + 1)
                    if kind == 1:
                        moba_prep(slot, qbs[G + 1], G + 1)
                ob = cnt["o"] % 2
                cnt["o"] += 1
                attend(u, slot, G, qbs[G], ob)
                if pend is not None:
                    finish2(*pend)
                if kind == 1 and G + 1 < NGq:
                    moba_prep2(slot, qbs[G + 1], G + 1)
                finish(u, G, [(u, ob)])
                pend = (u, G, [(u, ob)])
            finish2(*pend)


def build_phase_b_prog(kind):
    nc = bass.Bass("TRN2", target_bir_lowering=False)
    vd = 128 if kind == 2 else 64
    recv_qk = nc.dram_tensor("recv_qk", [4, 2, 3, 64, TL], BF16, kind="ExternalInput").ap()
    recv_v = nc.dram_tensor("recv_v", [4, 3, TL, vd], BF16, kind="ExternalInput").ap()
    nmask = 33 if kind == 0 else 4
    cd = dict(
        ident=nc.dram_tensor("ident", [128, 128], BF16, kind="ExternalInput").ap(),
        sel=nc.dram_tensor("sel", [128, 64], F32, kind="ExternalInput").ap(),
        qrow=nc.dram_tensor("qrow", [3, 3, S], BF16, kind="ExternalInput").ap(),
        biast=nc.dram_tensor("biast", [128, 3, 64], F32, kind="ExternalInput").ap(),
        kones=nc.dram_tensor("kones", [35, S], BF16, kind="ExternalInput").ap(),
        masks=nc.dram_tensor("masks", [128, nmask, 512], BF16, kind="ExternalInput").ap(),
    )
    oT = nc.dram_tensor("oT", [3, vd, S], BF16, kind="ExternalOutput").ap()
    P = Prog(nc)
    A = SB(nc)
    phase_b(nc, P, A, kind, recv_qk, recv_v, cd, oT)
    P.emit()
    return nc, P


def phase_c(nc, P, A, kind, layer, xT_res, xT_sb, recv_o, gateT_d, qmT_d, memT_d, mgcol_d, wkv_d, wout_d, sel_d,
            diffp=None, final=None, xT_out=None, load_x_from=None):
    vd = 128 if kind == 2 else 64
    NG = TL // 512
    lam_init = 0.8 - 0.6 * float(np.exp(-0.3 * layer))
    if load_x_from is not None:
        xv = load_x_from.rearrange("(c p) t -> p c t", p=128)
        for G in range(NG):
            P.dma("sp", xT_sb[:, :, G * 512:(G + 1) * 512], xv[:, :, G * 512:(G + 1) * 512], writes=[xT_res[G]])
    ones_bf = A.sb([128, 128], BF16)
    r_ones = Res()
    P.add("pool", lambda h: h.memset(ones_bf[:], 1.0), writes=[r_ones])
    sel = A.sb([128, 64], F32)
    r_sel = Res()
    P.dma("sp", sel[:], sel_d, writes=[r_sel])
    mgcol = A.sb([128, 8], F32)
    r_mg = Res()
    P.dma("sp", mgcol[:], mgcol_d, writes=[r_mg])

    NPS = 6
    ps = [A.ps([128, 512], F32) for _ in range(NPS)]
    r_ps = [Res() for _ in range(NPS)]
    cnt = dict(ps=0, pt=0, st=0)

    def nps():
        i = cnt["ps"] % NPS
        cnt["ps"] += 1
        return i

    memT = A.sb([128, 8, 256], F32)
    r_mem = Res()
    P.dma("sp", memT[:], memT_d.rearrange("(c p) t -> p c t", p=128), writes=[r_mem])
    sqm = [A.sb([128, 512], BF16) for _ in range(2)]
    r_sqm = [Res() for _ in range(2)]
    rstd = A.sb([128, 512], F32)
    r_rstd = Res()
    mnT = A.sb([128, 8, 256], BF16)
    r_mn = Res()
    pb = nps()
    for c in range(8):
        b = c % 2
        P.add("act", lambda h, b=b, c=c: h.activation(out=sqm[b][:, 0:256], in_=memT[:, c, :], func=AF.Square), reads=[r_mem], writes=[r_sqm[b]])
        P.add("pe", lambda h, b=b, c=c: h.matmul(ps[pb][:, 0:256], ones_bf[:], sqm[b][:, 0:256], start=(c == 0), stop=(c == 7)),
              reads=[r_sqm[b], r_ones], writes=[r_ps[pb]])
    P.add("act", lambda h: h.activation(out=rstd[:, 0:256], in_=ps[pb][:, 0:256], func=AF.Sqrt, scale=1.0 / D, bias=RMS_EPS), reads=[r_ps[pb]], writes=[r_rstd])
    P.add("dve", lambda h: h.reciprocal(out=rstd[:, 0:256], in_=rstd[:, 0:256]), reads=[r_rstd], writes=[r_rstd])
    for c in range(8):
        P.add("dve", lambda h, c=c: h.tensor_tensor(out=mnT[:, c, :], in0=memT[:, c, :], in1=rstd[:, 0:256], op=ALU.mult), reads=[r_mem, r_rstd], writes=[r_mn])
    WS = 128
    wst = [A.sb([128, 8, WS], F32) for _ in range(2)]
    r_wst = [Res() for _ in range(2)]
    wkv = A.sb([128, 8, 512], BF16)
    r_wkv = Res()
    wkv_v = wkv_d.rearrange("(c p) n -> p c n", p=128)
    for j in range(512 // WS):
        b = cnt["st"] % 2
        cnt["st"] += 1
        P.dma("sp", wst[b][:], wkv_v[:, :, j * WS:(j + 1) * WS], writes=[r_wst[b]])
        for c in range(8):
            if c % 2 == 0:
                P.add("dve", lambda h, b=b, c=c, j=j: h.tensor_scalar(out=wkv[:, c, j * WS:(j + 1) * WS], in0=wst[b][:, c, :], scalar1=mgcol[:, c:c + 1],
                                                                       scalar2=None, op0=ALU.mult), reads=[r_wst[b], r_mg], writes=[r_wkv])
            else:
                P.add("act", lambda h, b=b, c=c, j=j: h.activation(out=wkv[:, c, j * WS:(j + 1) * WS], in_=wst[b][:, c, :], func=AF.Copy, scale=mgcol[:, c:c + 1]),
                      reads=[r_wst[b], r_mg], writes=[r_wkv])
    kmT = A.sb([128, 2, 256], BF16)
    r_kmT = Res()
    for ch in range(2):
        pb = nps()
        for c in range(8):
            P.add("pe", lambda h, pb=pb, c=c, ch=ch: h.matmul(ps[pb][:, 0:256], wkv[:, c, ch * 128:(ch + 1) * 128], mnT[:, c, :], start=(c == 0), stop=(c == 7)),
                  reads=[r_wkv, r_mn], writes=[r_ps[pb]])
        P.add("act", lambda h, pb=pb, ch=ch: h.activation(out=kmT[:, ch, :], in_=ps[pb][:, 0:256], func=AF.Copy), reads=[r_ps[pb]], writes=[r_kmT])
    vma = A.sb([128, 2, 4, 65], BF16)
    r_vma = Res()
    P.add("pool", lambda h: h.memset(vma[:], 1.0), writes=[r_vma])
    for mt in range(2):
        pb = nps()
        for c in range(8):
            P.add("pe", lambda h, pb=pb, c=c, mt=mt: h.matmul(ps[pb][:, 0:256], mnT[:, c, mt * 128:(mt + 1) * 128], wkv[:, c, 256:512], start=(c == 0), stop=(c == 7)),
                  reads=[r_wkv, r_mn], writes=[r_ps[pb]])
        P.add("dve", lambda h, pb=pb, mt=mt: h.tensor_copy(out=vma[:, mt, :, 0:64], in_=ps[pb][:, 0:256].rearrange("p (h d) -> p h d", d=64)),
              reads=[r_ps[pb]], writes=[r_vma])

    qm = A.sb([128, 2, TL], BF16)
    r_qm = Res()
    P.dma("sp", qm[:], qmT_d.rearrange("(c p) t -> p c t", p=128), writes=[r_qm])
    yT = A.sb([128, 8, TL], BF16)
    r_y = [[Res() for _ in range(NG)] for _ in range(8)]
    pt = [A.sb([128, 512], BF16) for _ in range(3)]
    r_pt = [Res() for _ in range(3)]
    rd = A.sb([128, 512], F32)
    r_rd = Res()
    P.add("pool", lambda h: h.memset(rd[:], 0.0), writes=[r_rd])
    osb = [A.sb([64, 512], F32) for _ in range(2)]
    r_osb = [Res() for _ in range(2)]
    mo = [A.sb([64, 512], BF16) for _ in range(2)]
    r_mo = [Res() for _ in range(2)]
    k = 0
    for G in range(NG):
        gs = slice(G * 512, (G + 1) * 512)
        for hm in range(4):
            ch, r0 = hm // 2, (hm % 2) * 64
            po = nps()
            for mt in range(2):
                pb = nps()
                P.add("pe", lambda h, pb=pb, ch=ch, r0=r0, mt=mt, gs=gs: h.matmul(ps[pb][:], kmT[r0:r0 + 64, ch, mt * 128:(mt + 1) * 128], qm[r0:r0 + 64, ch, gs],
                                                                                 start=True, stop=True), reads=[r_kmT, r_qm], writes=[r_ps[pb]])
                pi = cnt["pt"] % 3
                cnt["pt"] += 1
                P.add("act", lambda h, pb=pb, pi=pi: h.activation(out=pt[pi][:], in_=ps[pb][:], func=AF.Exp), reads=[r_ps[pb]], writes=[r_pt[pi]])
                P.add("pe", lambda h, po=po, pi=pi, mt=mt, hm=hm: h.matmul(ps[po][0:65, :], vma[:, mt, hm, :], pt[pi][:], start=(mt == 0), stop=(mt == 1)),
                      reads=[r_vma, r_pt[pi]], writes=[r_ps[po]])
            P.add("dve", lambda h, po=po: h.reciprocal(out=rd[64:65, :], in_=ps[po][64:65, :]), reads=[r_ps[po], r_rd], writes=[r_rd])
            pbc = nps()
            P.add("pe", lambda h, pbc=pbc: h.matmul(ps[pbc][0:64, :], sel[:, :], rd[:, :], start=True, stop=True), reads=[r_sel, r_rd], writes=[r_ps[pbc]])
            sb = k % 2
            mb = k % 2
            k += 1
            P.add("act", lambda h, po=po, sb=sb: h.activation(out=osb[sb][:], in_=ps[po][0:64, :], func=AF.Copy), reads=[r_ps[po]], writes=[r_osb[sb]])
            P.add("dve", lambda h, sb=sb, mb=mb, pbc=pbc: h.tensor_tensor(out=mo[mb][:], in0=osb[sb][:], in1=ps[pbc][0:64, :], op=ALU.mult),
                  reads=[r_osb[sb], r_ps[pbc]], writes=[r_mo[mb]])
            P.dma("pool", yT[r0:r0 + 64, 6 + ch, gs], mo[mb][:], reads=[r_mo[mb]], writes=[r_y[6 + ch][G]])

    if kind in (0, 1):
        for c in range(6):
            for half in range(2):
                hh = 2 * c + half
                g, u = hh % 4, hh // 4
                P.dma("sp", yT[half * 64:(half + 1) * 64, c, :], recv_o[g, u], writes=[r_y[c][G] for G in range(NG)])
    else:
        lp = A.sb([128, 4, 64], F32)
        r_lp = Res()
        for i, nm in enumerate(("lq1", "lk1", "lq2", "lk2")):
            P.dma("sp", lp[:, i, :], diffp[nm][0].partition_broadcast(128), writes=[r_lp])
        sgc = A.sb([128, 1], F32)
        r_sg = Res()
        P.dma("sp", sgc[:], diffp["sg"], writes=[r_sg])
        lpr = A.sb([128, 2, 64], F32)
        lsum = A.sb([128, 2], F32)
        nlam = A.sb([128, 1], F32)
        r_lam = Res()
        P.add("dve", lambda h: h.tensor_tensor(out=lpr[:, 0, :], in0=lp[:, 0, :], in1=lp[:, 1, :], op=ALU.mult), reads=[r_lp], writes=[r_lam])
        P.add("dve", lambda h: h.tensor_tensor(out=lpr[:, 1, :], in0=lp[:, 2, :], in1=lp[:, 3, :], op=ALU.mult), reads=[r_lp, r_lam], writes=[r_lam])
        P.add("dve", lambda h: h.tensor_reduce(out=lsum[:, :], in_=lpr[:, :, :], axis=AX.X, op=ALU.add), reads=[r_lam], writes=[r_lam])
        P.add("act", lambda h: h.activation(out=lsum[:, :], in_=lsum[:, :], func=AF.Exp), reads=[r_lam], writes=[r_lam])
        P.add("dve", lambda h: h.tensor_tensor(out=nlam[:, :], in0=lsum[:, 1:2], in1=lsum[:, 0:1], op=ALU.subtract), reads=[r_lam], writes=[r_lam])
        P.add("dve", lambda h: h.tensor_scalar(out=nlam[:, :], in0=nlam[:, :], scalar1=-lam_init, scalar2=None, op0=ALU.add), reads=[r_lam], writes=[r_lam])
        P.add("dve", lambda h: h.tensor_scalar(out=sgc[:, :], in0=sgc[:, :], scalar1=(1.0 - lam_init), scalar2=None, op0=ALU.mult), reads=[r_sg], writes=[r_sg])
        m12 = [A.sb([128, 2, 512], BF16) for _ in range(2)]
        r_m12 = [Res() for _ in range(2)]
        od = [A.sb([128, 512], F32) for _ in range(2)]
        r_od = [Res() for _ in range(2)]
        sq2 = [A.sb([128, 512], BF16) for _ in range(2)]
        r_sq2 = [Res() for _ in range(2)]
        rs2 = A.sb([128, 512], F32)
        r_rs2 = Res()
        k = 0
        for G in range(NG):
            gs = slice(G * 512, (G + 1) * 512)
            for c in range(6):
                b = k % 2
                k += 1
                for m_ in range(2):
                    mm = 2 * c + m_
                    g, u = mm // 3, mm % 3
                    P.dma("sp", m12[b][:, m_, :], recv_o[g, u, :, gs], writes=[r_m12[b]])
                P.add("dve", lambda h, b=b: h.scalar_tensor_tensor(out=od[b][:], in0=m12[b][:, 1, :], scalar=nlam[:, 0:1], in1=m12[b][:, 0, :],
                                                                     op0=ALU.mult, op1=ALU.add), reads=[r_m12[b], r_lam], writes=[r_od[b]])
                P.add("act", lambda h, b=b: h.activation(out=sq2[b][:], in_=od[b][:], func=AF.Square), reads=[r_od[b]], writes=[r_sq2[b]])
                pb = nps()
                P.add("pe", lambda h, pb=pb, b=b: h.matmul(ps[pb][:], ones_bf[:], sq2[b][:], start=True, stop=True), reads=[r_sq2[b], r_ones], writes=[r_ps[pb]])
                P.add("act", lambda h, pb=pb: h.activation(out=rs2[:], in_=ps[pb][:], func=AF.Sqrt, scale=1.0 / 128.0, bias=SUBLN_EPS), reads=[r_ps[pb]], writes=[r_rs2])
                P.add("dve", lambda h: h.reciprocal(out=rs2[:], in_=rs2[:]), reads=[r_rs2], writes=[r_rs2])
                P.add("dve", lambda h, b=b, c=c, gs=gs: h.scalar_tensor_tensor(out=yT[:, c, gs], in0=od[b][:], scalar=sgc[:, 0:1], in1=rs2[:],
                                                                                 op0=ALU.mult, op1=ALU.mult), reads=[r_od[b], r_sg, r_rs2], writes=[r_y[c][G]])

    gt = [A.sb([128, 512], BF16) for _ in range(3)]
    r_gt = [Res() for _ in range(3)]
    gview = gateT_d.rearrange("(c p) t -> p c t", p=128)
    kk = 0
    for c in range(8):
        for G in range(NG):
            b = kk % 3
            kk += 1
            gs = slice(G * 512, (G + 1) * 512)
            P.dma("sp", gt[b][:], gview[:, c, gs], writes=[r_gt[b]])
            P.add("act", lambda h, b=b: h.activation(out=gt[b][:], in_=gt[b][:], func=AF.Silu), reads=[r_gt[b]], writes=[r_gt[b]])
            P.add("dve", lambda h, b=b, c=c, gs=gs: h.tensor_tensor(out=yT[:, c, gs], in0=yT[:, c, gs], in1=gt[b][:], op=ALU.mult),
                  reads=[r_gt[b], r_y[c][G]], writes=[r_y[c][G]])

    wo = A.sb([128, 8, D], BF16)
    r_wo = [Res() for _ in range(8)]
    wo_v = wout_d.rearrange("(c p) n -> p c n", p=128)
    for j in range(8):
        b = cnt["st"] % 2
        cnt["st"] += 1
        P.dma("sp", wst[b][:], wo_v[:, :, j * WS:(j + 1) * WS], writes=[r_wst[b]])
        for c in range(8):
            if c % 2 == 0:
                P.add("dve", lambda h, b=b, c=c, j=j: h.tensor_copy(out=wo[:, c, j * WS:(j + 1) * WS], in_=wst[b][:, c, :]), reads=[r_wst[b]], writes=[r_wo[j]])
            else:
                P.add("act", lambda h, b=b, c=c, j=j: h.activation(out=wo[:, c, j * WS:(j + 1) * WS], in_=wst[b][:, c, :], func=AF.Copy), reads=[r_wst[b]], writes=[r_wo[j]])
    for G in range(NG):
        gs = slice(G * 512, (G + 1) * 512)
        for co in range(8):
            pb = nps()
            for c in range(8):
                P.add("pe", lambda h, pb=pb, c=c, co=co, gs=gs: h.matmul(ps[pb][:], wo[:, c, co * 128:(co + 1) * 128], yT[:, c, gs], start=(c == 0), stop=(c == 7)),
                      reads=[r_wo[co], r_y[c][G]], writes=[r_ps[pb]])
            P.add("dve", lambda h, pb=pb, co=co, gs=gs: h.tensor_tensor(out=xT_sb[:, co, gs], in0=xT_sb[:, co, gs], in1=ps[pb][:], op=ALU.add),
                  reads=[r_ps[pb], xT_res[G]], writes=[xT_res[G]])
    if xT_out is not None:
        xo = xT_out.rearrange("(c p) t -> p c t", p=128)
        for G in range(NG):
            gs = slice(G * 512, (G + 1) * 512)
            P.dma("sp", xo[:, :, gs], xT_sb[:, :, gs], reads=[xT_res[G]])
    if final is not None:
        fg = A.sb([128, 8], F32)
        r_fg = Res()
        P.dma("sp", fg[:], final["gcol"], writes=[r_fg])
        ob = [A.sb([128, 512], F32) for _ in range(2)]
        r_ob = [Res() for _ in range(2)]
        fo = final["out"].rearrange("(c p) t -> p c t", p=128)
        k = 0
        for G in range(NG):
            gs = slice(G * 512, (G + 1) * 512)
            pb = nps()
            for c in range(8):
                b = c % 2
                P.add("act", lambda h, b=b, c=c, gs=gs: h.activation(out=sqm[b][:], in_=xT_sb[:, c, gs], func=AF.Square), reads=[xT_res[G]], writes=[r_sqm[b]])
                P.add("pe", lambda h, b=b, c=c, pb=pb: h.matmul(ps[pb][:], ones_bf[:], sqm[b][:], start=(c == 0), stop=(c == 7)), reads=[r_sqm[b], r_ones], writes=[r_ps[pb]])
            P.add("act", lambda h, pb=pb: h.activation(out=rstd[:], in_=ps[pb][:], func=AF.Sqrt, scale=1.0 / D, bias=RMS_EPS), reads=[r_ps[pb]], writes=[r_rstd])
            P.add("dve", lambda h: h.reciprocal(out=rstd[:], in_=rstd[:]), reads=[r_rstd], writes=[r_rstd])
            for c in range(8):
                b = k % 2
                k += 1
                P.add("dve", lambda h, b=b, c=c, gs=gs: h.scalar_tensor_tensor(out=ob[b][:], in0=xT_sb[:, c, gs], scalar=fg[:, c:c + 1], in1=rstd[:],
                                                                                 op0=ALU.mult, op1=ALU.mult), reads=[xT_res[G], r_fg, r_rstd], writes=[r_ob[b]])
                P.dma("sp", fo[:, c, gs], ob[b][:], reads=[r_ob[b]])


def build_phase_c_prog(kind, layer, last):
    nc = bass.Bass("TRN2", target_bir_lowering=False)
    vd = 128 if kind == 2 else 64
    xT_d = nc.dram_tensor("xT", [D, TL], F32, kind="ExternalInput").ap()
    recv_o = nc.dram_tensor("recv_o", [4, 3, vd, TL], BF16, kind="ExternalInput").ap()
    gateT = nc.dram_tensor("gateT", [D, TL], BF16, kind="ExternalInput").ap()
    qmT = nc.dram_tensor("qmT", [256, TL], BF16, kind="ExternalInput").ap()
    memT = nc.dram_tensor("memT", [D, 256], F32, kind="ExternalInput").ap()
    mgcol = nc.dram_tensor("mgcol", [128, 8], F32, kind="ExternalInput").ap()
    wkv = nc.dram_tensor("wkv", [D, 512], F32, kind="ExternalInput").ap()
    wout = nc.dram_tensor("wout", [D, D], F32, kind="ExternalInput").ap()
    sel = nc.dram_tensor("sel", [128, 64], F32, kind="ExternalInput").ap()
    diffp = None
    if kind == 2:
        diffp = {nm: nc.dram_tensor(nm, [1, 64], F32, kind="ExternalInput").ap() for nm in ("lq1", "lk1", "lq2", "lk2")}
        diffp["sg"] = nc.dram_tensor("sg", [128, 1], F32, kind="ExternalInput").ap()
    final = None
    xT_out = None
    if last:
        final = dict(gcol=nc.dram_tensor("fgcol", [128, 8], F32, kind="ExternalInput").ap(),
                     out=nc.dram_tensor("outT", [D, TL], F32, kind="ExternalOutput").ap())
    else:
        xT_out = nc.dram_tensor("xT_out", [D, TL], F32, kind="ExternalOutput").ap()
    P = Prog(nc)
    A = SB(nc)
    xT_sb = A.sb([128, 8, TL], F32)
    xres = [Res() for _ in range(TL // 512)]
    phase_c(nc, P, A, kind, layer, xres, xT_sb, recv_o, gateT, qmT, memT, mgcol, wkv, wout, sel, diffp=diffp, final=final,
            xT_out=xT_out, load_x_from=xT_d)
    P.emit()
    return nc, P


_PROGS = {}
DEBUG = {}


def _prog(key, builder):
    if key not in _PROGS:
        _PROGS[key] = builder()[0]
    return _PROGS[key]


def _col8(v):
    return np.ascontiguousarray(np.asarray(v, np.float32).reshape(8, 128).T)


def kernel_unfused(x, mem, norm_g, w_in, w_out, mem_norm_g, w_mem_kv, diff_lambda_q1, diff_lambda_k1,
                   diff_lambda_q2, diff_lambda_k2, diff_subln_g, final_norm_g):
    x = np.asarray(x, np.float32)
    mem = np.asarray(mem, np.float32)
    cores = list(range(NCORE))
    xT = [np.ascontiguousarray(x[c // 4, (c % 4) * TL:(c % 4 + 1) * TL, :].T) for c in cores]
    memT = [np.ascontiguousarray(mem[b].T) for b in range(B)]
    sel = np.zeros((128, 64), np.float32)
    sel[64, :] = 1.0
    out = None
    for layer in range(DEPTH):
        kind = layer % 3
        vd = 128 if kind == 2 else 64
        last = layer == DEPTH - 1
        ncA = _prog(("A", kind), lambda: build_phase_a_prog(kind))
        wl = np.ascontiguousarray(np.asarray(w_in[layer], np.float32))
        gcol = _col8(norm_g[layer])
        resA = run_bass_kernel_spmd(ncA, [{"xT": xT[c], "w_in": wl, "gcol": gcol} for c in cores], core_ids=cores).results
        in_b = []
        for c in cores:
            b, g = c // 4, c % 4
            rq = np.stack([resA[b * 4 + r]["send_qk"][g] for r in range(4)], axis=0)
            rv = np.stack([resA[b * 4 + r]["send_v"][g] for r in range(4)], axis=0)
            m = {"recv_qk": np.ascontiguousarray(rq), "recv_v": np.ascontiguousarray(rv)}
            m.update(phase_b_consts(kind, g))
            in_b.append(m)
        ncB = _prog(("B", kind), lambda: build_phase_b_prog(kind))
        resB = run_bass_kernel_spmd(ncB, in_b, core_ids=cores).results
        in_c = []
        for c in cores:
            b, r = c // 4, c % 4
            ro = np.stack([resB[b * 4 + g]["oT"][:, :, r * TL:(r + 1) * TL] for g in range(4)], axis=0)
            m = {"xT": xT[c], "recv_o": np.ascontiguousarray(ro), "gateT": resA[c]["gateT"], "qmT": resA[c]["qmT"],
                 "memT": memT[b], "mgcol": _col8(mem_norm_g[layer]),
                 "wkv": np.ascontiguousarray(np.asarray(w_mem_kv[layer], np.float32)),
                 "wout": np.ascontiguousarray(np.asarray(w_out[layer], np.float32)), "sel": sel}
            if kind == 2:
                ci = layer // 3
                m["lq1"] = np.asarray(diff_lambda_q1[ci], np.float32).reshape(1, 64)
                m["lk1"] = np.asarray(diff_lambda_k1[ci], np.float32).reshape(1, 64)
                m["lq2"] = np.asarray(diff_lambda_q2[ci], np.float32).reshape(1, 64)
                m["lk2"] = np.asarray(diff_lambda_k2[ci], np.float32).reshape(1, 64)
                m["sg"] = np.asarray(diff_subln_g[ci], np.float32).reshape(128, 1)
            if last:
                m["fgcol"] = _col8(final_norm_g)
            in_c.append(m)
        ncC = _prog(("C", kind, layer, last), lambda: build_phase_c_prog(kind, layer, last))
        resC = run_bass_kernel_spmd(ncC, in_c, core_ids=cores).results
        if last:
            out = np.empty((B, S, D), np.float32)
            for c in cores:
                out[c // 4, (c % 4) * TL:(c % 4 + 1) * TL, :] = resC[c]["outT"].T
        else:
            xT = [resC[c]["xT_out"] for c in cores]
            if "dump" in DEBUG:
                xs = np.empty((B, S, D), np.float32)
                for c in cores:
                    xs[c // 4, (c % 4) * TL:(c % 4 + 1) * TL, :] = xT[c].T
                DEBUG["dump"].append(xs)
    return out


KINDS = [l % 3 for l in range(DEPTH)]


def build_fused():
    nc = bass.Bass("TRN2", target_bir_lowering=False)
    _PID_CACHE.clear()

    def din(name, shape, dt=F32):
        return nc.dram_tensor(name, shape, dt, kind="ExternalInput").ap()

    def dint(name, shape, dt):
        return nc.dram_tensor(name, shape, dt, kind="Internal").ap()

    xT_in = din("xT_in", [4, D, TL])
    w_in = din("w_in", [DEPTH, D, INW])
    w_out = din("w_out", [DEPTH, D, D])
    wkv = din("wkv", [DEPTH, D, 512])
    gcols = din("gcols", [DEPTH, 128, 8])
    mgcols = din("mgcols", [DEPTH, 128, 8])
    fgcol = din("fgcol", [128, 8])
    memT = din("memT", [D, 256])
    sel = din("sel", [128, 64])
    diffp = {nm: din(nm, [1, 64]) for nm in ("lq1", "lk1", "lq2", "lk2")}
    diffp["sg"] = din("sg", [128, 1])
    ident = din("ident", [128, 128], BF16)
    kones = din("kones", [35, S], BF16)
    masks = {0: din("masks0", [128, 33, 512], BF16), 1: din("masks1", [128, 4, 512], BF16)}
    masks[2] = masks[1]
    qrow = {k: din(f"qrow{k}", [4, 3, 3, S], BF16) for k in (0, 1, 2)}
    biast = {k: din(f"biast{k}", [4, 128, 3, 64]) for k in (0, 1, 2)}
    outT = nc.dram_tensor("outT", [D, TL], F32, kind="ExternalOutput").ap()

    xbuf = dint("xbuf", [2, 4, D, TL], F32)
    qkbuf = dint("qkbuf", [4, 4, 2, 3, 64, TL], BF16)
    vbuf = {64: dint("vbuf64", [4, 4, 3, TL, 64], BF16), 128: dint("vbuf128", [4, 4, 3, TL, 128], BF16)}
    gbuf = dint("gbuf", [4, D, TL], BF16)
    qmbuf = dint("qmbuf", [4, 256, TL], BF16)
    obuf = {64: dint("obuf64", [4, 4, 3, 64, TL], BF16), 128: dint("obuf128", [4, 4, 3, 128, TL], BF16)}

    stage = dict(x=dint("st_x", [D, TL], F32), qk=dint("st_qk", [4, 2, 3, 64, TL], BF16), v=dint("st_v", [4, 3, TL, 64], BF16),
                 g=dint("st_g", [D, TL], BF16), qm=dint("st_qm", [256, TL], BF16), o=dint("st_o", [4, 3, 64, TL], BF16))
    P = Prog(nc)
    A = SB(nc, arena_words=48 * 1024 - 64)
    for layer in range(DEPTH):
        kind = KINDS[layer]
        vd = 128 if kind == 2 else 64
        last = layer == DEPTH - 1
        if last:
            for off in (3, 0):
                A.reset()
                P.dma("sp", stage["x"], core_slice(xbuf[layer % 2], off))
                P.barrier()
                xT_sb = A.sb([128, 8, TL], F32)
                xres = [Res() for _ in range(TL // 512)]
                phase_a(nc, P, A, kind, xres, xT_sb, w_in[layer], gcols[layer], stage["qk"], stage["v"], stage["g"], stage["qm"],
                        load_x_from=stage["x"])
                P.barrier()
                for nm, buf in (("qk", qkbuf), ("v", vbuf[vd]), ("g", gbuf), ("qm", qmbuf)):
                    P.dma("sp", core_slice(buf, off), stage[nm])
                P.barrier()
        else:
            for s_ in range(4):
                A.reset()
                xT_sb = A.sb([128, 8, TL], F32)
                xres = [Res() for _ in range(TL // 512)]
                src = xT_in[s_] if layer == 0 else xbuf[layer % 2, s_]
                phase_a(nc, P, A, kind, xres, xT_sb, w_in[layer], gcols[layer], qkbuf[s_], vbuf[vd][s_], gbuf[s_], qmbuf[s_], load_x_from=src)
                P.barrier()
        for g in range(4):
            A.reset()
            cd = dict(ident=ident, sel=sel, kones=kones, masks=masks[kind], qrow=qrow[kind][g], biast=biast[kind][g])
            phase_b(nc, P, A, kind, qkbuf[:, g], vbuf[vd][:, g], cd, obuf[vd][:, g])
            P.barrier()
        if last:
            A.reset()
            P.dma("sp", stage["x"], core_slice(xbuf[layer % 2], 0))
            P.dma("sp", stage["o"], core_slice(obuf[vd], 0))
            P.dma("sp", stage["g"], core_slice(gbuf, 0))
            P.dma("sp", stage["qm"], core_slice(qmbuf, 0))
            P.barrier()
            xT_sb = A.sb([128, 8, TL], F32)
            xres = [Res() for _ in range(TL // 512)]
            phase_c(nc, P, A, kind, layer, xres, xT_sb, stage["o"], stage["g"], stage["qm"], memT, mgcols[layer],
                    wkv[layer], w_out[layer], sel, diffp=None, final=dict(gcol=fgcol, out=outT), xT_out=None, load_x_from=stage["x"])
            P.barrier()
        else:
            for s_ in range(4):
                A.reset()
                xT_sb = A.sb([128, 8, TL], F32)
                xres = [Res() for _ in range(TL // 512)]
                src = xT_in[s_] if layer == 0 else xbuf[layer % 2, s_]
                phase_c(nc, P, A, kind, layer, xres, xT_sb, obuf[vd][s_], gbuf[s_], qmbuf[s_], memT, mgcols[layer],
                        wkv[layer], w_out[layer], sel, diffp=(diffp if kind == 2 else None), final=None,
                        xT_out=xbuf[(layer + 1) % 2, s_], load_x_from=src)
                P.barrier()
    P.emit()
    return nc, P


_FUSED = {}


def kernel(x, mem, norm_g, w_in, w_out, mem_norm_g, w_mem_kv, diff_lambda_q1, diff_lambda_k1,
           diff_lambda_q2, diff_lambda_k2, diff_subln_g, final_norm_g):
    x = np.asarray(x, np.float32)
    mem = np.asarray(mem, np.float32)
    cores = list(range(NCORE))
    if "nc" not in _FUSED:
        _FUSED["nc"] = build_fused()[0]
    nc = _FUSED["nc"]
    f32 = lambda a: np.ascontiguousarray(np.asarray(a, np.float32))
    sel = np.zeros((128, 64), np.float32)
    sel[64, :] = 1.0
    shared = {
        "w_in": f32(w_in), "w_out": f32(w_out), "wkv": f32(w_mem_kv),
        "gcols": np.stack([_col8(norm_g[l]) for l in range(DEPTH)]),
        "mgcols": np.stack([_col8(mem_norm_g[l]) for l in range(DEPTH)]),
        "fgcol": _col8(final_norm_g), "sel": sel,
        "lq1": f32(diff_lambda_q1[0]).reshape(1, 64), "lk1": f32(diff_lambda_k1[0]).reshape(1, 64),
        "lq2": f32(diff_lambda_q2[0]).reshape(1, 64), "lk2": f32(diff_lambda_k2[0]).reshape(1, 64),
        "sg": f32(diff_subln_g[0]).reshape(128, 1),
    }
    for k in (0, 1, 2):
        cs = [phase_b_consts(k, g) for g in range(4)]
        shared[f"qrow{k}"] = np.stack([c["qrow"] for c in cs])
        shared[f"biast{k}"] = np.stack([c["biast"] for c in cs])
        if k < 2:
            shared[f"masks{k}"] = cs[0]["masks"]
        shared["ident"] = cs[0]["ident"]
        shared["kones"] = cs[0]["kones"]
    in_maps = []
    for c in cores:
        b = c // 4
        m = dict(shared)
        m["xT_in"] = np.ascontiguousarray(x[b].reshape(4, TL, D).transpose(0, 2, 1))
        m["memT"] = np.ascontiguousarray(mem[b].T)
        in_maps.append(m)
    res = run_bass_kernel_spmd(nc, in_maps, core_ids=cores).results
    out = np.empty((B, S, D), np.float32)
    for c in cores:
        b, r = c // 4, c % 4
        out[b, r * TL:(r + 1) * TL, :] = res[c]["outT"].T
    return out
```

```python
import numpy as np
import ml_dtypes
import concourse.bass as bass
import concourse.mybir as mybir
from concourse.bass_utils import run_bass_kernel_spmd

F32 = mybir.dt.float32
BF16 = mybir.dt.bfloat16
AF = mybir.ActivationFunctionType
ALU = mybir.AluOpType
AX = mybir.AxisListType
NPBF = ml_dtypes.bfloat16

D = 1024
S = 8192
B = 2
DEPTH = 4
TL = 2048
NCORE = 8
MIXW = 768
INW = 3584
NEG = -30000.0
RMS_EPS = 1e-6
SUBLN_EPS = 1e-5


class LazyAP:
    def __init__(self, base, off, ops=()):
        self.base = base
        self.off = off
        self.ops = tuple(ops)

    def __getitem__(self, idx):
        if not isinstance(idx, tuple):
            idx = (idx,)
        return LazyAP(self.base, self.off, self.ops + (("idx", idx),))

    def rearrange(self, pat, **kw):
        return LazyAP(self.base, self.off, self.ops + (("re", pat, kw),))

    def make(self, h):
        ap = self.base
        for op in self.ops:
            if op[0] == "idx":
                ap = ap[(slice(None),) + op[1]]
            else:
                lhs, rhs = op[1].split("->")
                ap = ap.rearrange("zz " + lhs.strip() + " -> zz " + rhs.strip(), **op[2])
        nd = len(ap.shape)
        names = _LET[:nd - 1]
        pat = "o " + " ".join(names) + " -> (o " + names[0] + ")" + ("".join(" " + n for n in names[1:]))
        key = (id(h), self.off)
        if key not in _PID_CACHE:
            _PID_CACHE[key] = (h.partition_id() + self.off) % 4
        return ap[bass.ds(_PID_CACHE[key], 1)].rearrange(pat)


_PID_CACHE = {}


def _resolve(ap, h):
    return ap.make(h) if isinstance(ap, LazyAP) else ap


def core_slice(ap, off):
    return LazyAP(ap, off)


class Res:
    __slots__ = ("name", "lw", "readers")

    def __init__(self, name=""):
        self.name = name
        self.lw = None
        self.readers = []


class Op:
    __slots__ = ("eng", "fn", "dma", "sem", "semval", "needs_inc", "idx", "waits", "gen")


class Prog:
    ENGS = ("pe", "act", "dve", "pool", "sp")

    def __init__(self, nc, same_sync=("act", "dve", "pool"), ndma=12):
        self.nc = nc
        self.h = dict(pe=nc.tensor, act=nc.scalar, dve=nc.vector, pool=nc.gpsimd, sp=nc.sync)
        self.ops = {e: [] for e in self.ENGS}
        self.obs = {e: {} for e in self.ENGS}
        self.same_sync = set(same_sync)
        self.same_dist = 4
        self.esems = {e: [nc.alloc_semaphore(name=f"es_{e}_0")] for e in ("pe", "act", "dve", "pool")}
        self.gen = {e: 0 for e in ("pe", "act", "dve", "pool", "sp")}
        self.gcount = {e: 0 for e in ("pe", "act", "dve", "pool")}
        self.pending = {e: [] for e in self.ENGS}
        self.ndma = ndma
        self.dsem = {q: [nc.alloc_semaphore(name=f"ds_{q}_{i}") for i in range(ndma)] for q in ("sp", "pool", "act")}
        self.dlast = {q: [None] * ndma for q in ("sp", "pool", "act")}
        self.dcnt = {q: 0 for q in ("sp", "pool", "act")}
        self.all_dma = []

    def add(self, eng, fn, reads=(), writes=(), dma=False):
        op = Op()
        op.eng = eng
        op.fn = fn
        op.dma = dma
        op.needs_inc = False
        op.sem = None
        op.semval = None
        op.idx = len(self.ops[eng])
        op.gen = self.gen[eng]
        deps = []
        if self.pending[eng]:
            deps.extend(self.pending[eng])
            self.pending[eng] = []
        for r in reads:
            if r.lw is not None:
                deps.append(r.lw)
        for w in writes:
            if w.lw is not None:
                deps.append(w.lw)
            deps.extend(w.readers)
        if dma:
            n = self.dcnt[eng]
            slot = n % self.ndma
            prev = self.dlast[eng][slot]
            if prev is not None:
                deps.append(prev)
            self.dlast[eng][slot] = op
            op.sem = self.dsem[eng][slot]
            op.semval = 16 * (n // self.ndma + 1)
            self.dcnt[eng] = n + 1
            self.all_dma.append(op)
        waits = []
        obs = self.obs[eng]
        for p in deps:
            if p is op:
                continue
            if p.dma:
                key = ("d", id(p.sem))
                if obs.get(key, 0) >= p.semval:
                    continue
                obs[key] = p.semval
                waits.append(p)
            else:
                if p.eng == eng and (eng not in self.same_sync or op.idx - p.idx >= self.same_dist):
                    continue
                key = ("e", p.eng)
                if obs.get(key, -1) >= p.idx:
                    continue
                obs[key] = p.idx
                if not p.needs_inc:
                    p.needs_inc = True
                    self.gcount[p.eng] += 1
                waits.append(p)
        op.waits = waits
        for r in reads:
            r.readers.append(op)
        for w in writes:
            w.lw = op
            w.readers = []
        self.ops[eng].append(op)
        return op

    def barrier(self, rotate_at=12000):
        lasts = []
        for e in ("pe", "act", "dve", "pool"):
            for op in reversed(self.ops[e]):
                if not op.dma:
                    if not op.needs_inc:
                        op.needs_inc = True
                        self.gcount[e] += 1
                    lasts.append(op)
                    break
        for q in self.dlast:
            for op in self.dlast[q]:
                if op is not None:
                    lasts.append(op)
        for e in self.ENGS:
            self.pending[e] = list(lasts)
        for e in ("pe", "act", "dve", "pool"):
            if self.gcount[e] > rotate_at:
                self.gen[e] += 1
                self.gcount[e] = 0
                self.esems[e].append(self.nc.alloc_semaphore(name=f"es_{e}_{self.gen[e]}"))

    def dma(self, q, out, in_, reads=(), writes=()):
        return self.add(q, lambda h: h.dma_start(out=_resolve(out, h), in_=_resolve(in_, h)), reads, writes, dma=True)

    def emit(self):
        nc = self.nc
        final_waits = {}
        for op in self.all_dma:
            k = id(op.sem)
            if k not in final_waits or final_waits[k][1] < op.semval:
                final_waits[k] = (op.sem, op.semval)
        for e in ("pe", "act", "dve", "pool"):
            c = {}
            for op in self.ops[e]:
                if op.needs_inc and not op.dma:
                    c[op.gen] = c.get(op.gen, 0) + 1
                    op.sem = self.esems[e][op.gen]
                    op.semval = c[op.gen]
        self.max_semval = {e: sum(1 for o in self.ops[e] if o.needs_inc) for e in ("pe", "act", "dve", "pool")}

        def run(e):
            h = self.h[e]
            for op in self.ops[e]:
                for p in op.waits:
                    h.wait_ge(p.sem, p.semval)
                ins = op.fn(h)
                if op.dma:
                    ins.then_inc(op.sem, 16)
                elif op.needs_inc:
                    ins.then_inc(op.sem, 1)
            if e == "sp":
                for p in self.pending[e]:
                    h.wait_ge(p.sem, p.semval)
                for sem, val in final_waits.values():
                    h.wait_ge(sem, val)

        with nc.Block() as block:
            @block.tensor
            def _(t):
                run("pe")

            @block.scalar
            def _(t):
                run("act")

            @block.vector
            def _(t):
                run("dve")

            @block.gpsimd
            def _(t):
                run("pool")

            @block.sync
            def _(t):
                run("sp")


_LET = "abcdefgh"


def _view(ap2d, shape):
    dims = list(shape[1:])
    if len(dims) > 1:
        names = " ".join(_LET[:len(dims)])
        kw = {_LET[i]: dims[i] for i in range(len(dims))}
        ap2d = ap2d.rearrange(f"p ({names}) -> p {names}", **kw)
    if shape[0] != 128:
        ap2d = ap2d[0:shape[0]]
    return ap2d


class SB:
    def __init__(self, nc, arena_words=None):
        self.nc = nc
        self.n = 0
        self.arena = None
        if arena_words is not None:
            self.arena = nc.alloc_sbuf_tensor("arena", [128, arena_words], F32)
            self.words = arena_words
            self.off = 0
            self.banks = [nc.alloc_psum_tensor(f"bank{i}", [128, 512], F32) for i in range(8)]
            self.pi = 0

    def reset(self, keep=0):
        self.off = keep
        self.pi = 0

    def sb(self, shape, dt, name=None):
        self.n += 1
        if self.arena is None:
            return self.nc.alloc_sbuf_tensor(name or f"sb{self.n}", shape, dt)
        n = int(np.prod(shape[1:]))
        esz = 4 if dt == F32 else 2
        words = (n * esz + 3) // 4
        words = (words + 7) // 8 * 8
        assert self.off + words <= self.words, f"SBUF arena overflow: {self.off}+{words} > {self.words}"
        v = self.arena[:, self.off:self.off + words]
        self.off += words
        if dt != F32:
            v = v.bitcast(dt)
        v = v[:, 0:n]
        return _view(v, shape)

    def ps(self, shape, dt, name=None):
        self.n += 1
        if self.arena is None:
            return self.nc.alloc_psum_tensor(name or f"ps{self.n}", shape, dt)
        assert self.pi < 8, "out of PSUM banks"
        b = self.banks[self.pi]
        self.pi += 1
        n = int(np.prod(shape[1:]))
        return _view(b[:, 0:n], shape)


def unit_cols(kind, g, u):
    if kind in (0, 1):
        hh = 4 * u + g
        return hh * 64, MIXW + hh * 64, 2 * MIXW + hh * 64, 64
    mm = 3 * g + u
    head, m = mm // 2, mm % 2
    return head * 128 + m * 64, MIXW + head * 128 + m * 64, 2 * MIXW + head * 128, 128


def alibi_slopes(n):
    return (2.0 ** (-8.0 * np.arange(1, n + 1) / n)).astype(np.float64)


def phase_a(nc, P, A, kind, xT_res, xT_sb, wA, gcol_d, send_qk, send_v, gateT_d, qmT_d, load_x_from=None):
    vd = 128 if kind == 2 else 64
    NG = TL // 512
    ones_bf = A.sb([128, 128], BF16)
    r_ones = Res()
    P.add("pool", lambda h: h.memset(ones_bf[:], 1.0), writes=[r_ones])
    gcol = A.sb([128, 8], F32)
    r_g = Res()
    P.dma("sp", gcol[:], gcol_d, writes=[r_g])
    if load_x_from is not None:
        xv = load_x_from.rearrange("(c p) t -> p c t", p=128)
        for G in range(NG):
            P.dma("sp", xT_sb[:, :, G * 512:(G + 1) * 512], xv[:, :, G * 512:(G + 1) * 512], writes=[xT_res[G]])

    hT = A.sb([128, 8, TL], BF16)
    r_h = [Res() for _ in range(NG)]
    sq = [A.sb([128, 512], BF16) for _ in range(2)]
    r_sq = [Res() for _ in range(2)]
    ss_ps = A.ps([128, 512], F32)
    r_ss = Res()
    rstd = A.sb([128, 512], F32)
    r_rstd = Res()
    i = 0
    for G in range(NG):
        gs = slice(G * 512, (G + 1) * 512)
        for c in range(8):
            b = i % 2
            i += 1
            P.add("act", lambda h, b=b, c=c, gs=gs: h.activation(out=sq[b][:], in_=xT_sb[:, c, gs], func=AF.Square),
                  reads=[xT_res[G]], writes=[r_sq[b]])
            P.add("pe", lambda h, b=b, c=c: h.matmul(ss_ps[:], ones_bf[:], sq[b][:], start=(c == 0), stop=(c == 7)),
                  reads=[r_sq[b], r_ones], writes=[r_ss])
        P.add("act", lambda h: h.activation(out=rstd[:], in_=ss_ps[:], func=AF.Sqrt, scale=1.0 / D, bias=RMS_EPS),
              reads=[r_ss], writes=[r_rstd])
        P.add("dve", lambda h: h.reciprocal(out=rstd[:], in_=rstd[:]), reads=[r_rstd], writes=[r_rstd])
        for c in range(8):
            P.add("dve", lambda h, c=c, gs=gs: h.tensor_tensor(out=hT[:, c, gs], in0=xT_sb[:, c, gs], in1=rstd[:], op=ALU.mult),
                  reads=[xT_res[G], r_rstd], writes=[r_h[G]])

    CG = 256
    ncg = INW // CG
    wst = [A.sb([128, 8, CG], F32) for _ in range(2)]
    r_wst = [Res() for _ in range(2)]
    wb = [A.sb([128, 8, CG], BF16) for _ in range(2)]
    r_wb = [Res() for _ in range(2)]
    wv = wA.rearrange("(c p) n -> p c n", p=128)
    ev = [A.sb([128, TL], BF16) for _ in range(2)]
    r_ev = [Res() for _ in range(2)]
    vsb = [A.sb([128, 16, CG], BF16) for _ in range(2)]
    r_vsb = [Res() for _ in range(2)]
    pacc = [A.ps([128, 512], F32) for _ in range(4)]
    r_pacc = [Res() for _ in range(4)]
    pi = 0
    evi = 0
    vi = 0
    dest = {}
    for g in range(4):
        for u in range(3):
            qc, kc, vc, _ = unit_cols(kind, g, u)
            dest[qc] = (g, 0, u)
            dest[kc] = (g, 1, u)
    vdest = {}
    for g in range(4):
        for u in range(3):
            qc, kc, vc, _ = unit_cols(kind, g, u)
            vdest.setdefault(vc, []).append((g, u))
    for cg in range(ncg):
        b = cg % 2
        c0 = cg * CG
        P.dma("pool", wst[b][:], wv[:, :, c0:c0 + CG], writes=[r_wst[b]])
        for c in range(8):
            if c % 2 == 0:
                P.add("dve", lambda h, b=b, c=c: h.tensor_scalar(out=wb[b][:, c, :], in0=wst[b][:, c, :], scalar1=gcol[:, c:c + 1],
                                                                  scalar2=None, op0=ALU.mult),
                      reads=[r_wst[b], r_g], writes=[r_wb[b]])
            else:
                P.add("act", lambda h, b=b, c=c: h.activation(out=wb[b][:, c, :], in_=wst[b][:, c, :], func=AF.Copy, scale=gcol[:, c:c + 1]),
                      reads=[r_wst[b], r_g], writes=[r_wb[b]])
        if 2 * MIXW <= c0 < 3 * MIXW:
            vb = vi % 2
            vi += 1
            for tt in range(16):
                pb = pi % 4
                pi += 1
                for c in range(8):
                    P.add("pe", lambda h, pb=pb, c=c, tt=tt, b=b: h.matmul(pacc[pb][:, 0:CG], hT[:, c, tt * 128:(tt + 1) * 128],
                                                                          wb[b][:, c, :], start=(c == 0), stop=(c == 7)),
                          reads=[r_h[tt // 4], r_wb[b]], writes=[r_pacc[pb]])
                eng = "act" if tt % 2 == 0 else "dve"
                if eng == "act":
                    P.add("act", lambda h, pb=pb, vb=vb, tt=tt: h.activation(out=vsb[vb][:, tt, :], in_=pacc[pb][:, 0:CG], func=AF.Copy),
                          reads=[r_pacc[pb]], writes=[r_vsb[vb]])
                else:
                    P.add("dve", lambda h, pb=pb, vb=vb, tt=tt: h.tensor_copy(out=vsb[vb][:, tt, :], in_=pacc[pb][:, 0:CG]),
                          reads=[r_pacc[pb]], writes=[r_vsb[vb]])
            for off in range(0, CG, vd):
                col = c0 + off
                for (g, u) in vdest.get(col, []):
                    dst = send_v[g, u].rearrange("(t p) v -> p t v", p=128)
                    P.dma("sp", dst, vsb[vb][:, :, off:off + vd], reads=[r_vsb[vb]])
            continue
        for ch in range(CG // 128):
            col = c0 + ch * 128
            isq = col < MIXW or (3 * MIXW <= col < 3 * MIXW + 256)
            eb = evi % 2
            evi += 1
            for G in range(NG):
                gs = slice(G * 512, (G + 1) * 512)
                pb = pi % 4
                pi += 1
                for c in range(8):
                    P.add("pe", lambda h, pb=pb, c=c, gs=gs, b=b, ch=ch: h.matmul(pacc[pb][:], wb[b][:, c, ch * 128:(ch + 1) * 128],
                                                                                  hT[:, c, gs], start=(c == 0), stop=(c == 7)),
                          reads=[r_h[G], r_wb[b]], writes=[r_pacc[pb]])
                sc = 0.125 if isq else 1.0
                if G % 2 == 0:
                    P.add("act", lambda h, pb=pb, eb=eb, gs=gs, sc=sc: h.activation(out=ev[eb][:, gs], in_=pacc[pb][:], func=AF.Copy, scale=sc),
                          reads=[r_pacc[pb]], writes=[r_ev[eb]])
                else:
                    P.add("dve", lambda h, pb=pb, eb=eb, gs=gs, sc=sc: h.tensor_scalar(out=ev[eb][:, gs], in0=pacc[pb][:], scalar1=sc, scalar2=None, op0=ALU.mult),
                          reads=[r_pacc[pb]], writes=[r_ev[eb]])
            if col < 2 * MIXW:
                for half in range(2):
                    g, qk, u = dest[col + half * 64]
                    P.dma("sp", send_qk[g, qk, u], ev[eb][half * 64:(half + 1) * 64, :], reads=[r_ev[eb]])
            elif col < 3 * MIXW + 256:
                r0 = col - 3 * MIXW
                P.dma("sp", qmT_d[r0:r0 + 128, :], ev[eb][:], reads=[r_ev[eb]])
            else:
                r0 = col - (3 * MIXW + 256)
                P.dma("sp", gateT_d[r0:r0 + 128, :], ev[eb][:], reads=[r_ev[eb]])


def build_phase_a_prog(kind):
    nc = bass.Bass("TRN2", target_bir_lowering=False)
    vd = 128 if kind == 2 else 64
    xT_d = nc.dram_tensor("xT", [D, TL], F32, kind="ExternalInput").ap()
    w_d = nc.dram_tensor("w_in", [D, INW], F32, kind="ExternalInput").ap()
    g_d = nc.dram_tensor("gcol", [128, 8], F32, kind="ExternalInput").ap()
    send_qk = nc.dram_tensor("send_qk", [4, 2, 3, 64, TL], BF16, kind="ExternalOutput").ap()
    send_v = nc.dram_tensor("send_v", [4, 3, TL, vd], BF16, kind="ExternalOutput").ap()
    gateT = nc.dram_tensor("gateT", [D, TL], BF16, kind="ExternalOutput").ap()
    qmT = nc.dram_tensor("qmT", [256, TL], BF16, kind="ExternalOutput").ap()
    P = Prog(nc)
    A = SB(nc)
    xT_sb = A.sb([128, 8, TL], F32)
    xres = [Res() for _ in range(TL // 512)]
    phase_a(nc, P, A, kind, xres, xT_sb, w_d, g_d, send_qk, send_v, gateT, qmT, load_x_from=xT_d)
    P.emit()
    return nc, P


DSW = ((128, 1), (512, 4), (2048, 16))


def unit_slope(kind, g, u):
    if kind in (0, 1):
        return alibi_slopes(12)[4 * u + g]
    return alibi_slopes(6)[(3 * g + u) // 2]


def split3(x):
    hi = x.astype(NPBF)
    r = x - hi.astype(np.float64)
    lo = r.astype(NPBF)
    r2 = r - lo.astype(np.float64)
    lo2 = r2.astype(NPBF)
    return hi, lo, lo2


def phase_b_consts(kind, g):
    c = {}
    c["ident"] = np.eye(128, dtype=np.float32).astype(NPBF)
    sel = np.zeros((128, 64), np.float32)
    sel[64, :] = 1.0
    c["sel"] = sel
    t = np.arange(S, dtype=np.float64)
    qrow = np.zeros((3, 3, S), dtype=NPBF)
    biast = np.zeros((128, 3, 64), dtype=np.float32)
    p = np.arange(128, dtype=np.float64)
    for u in range(3):
        sl = unit_slope(kind, g, u)
        x = -sl * (t - (t // 512) * 512)
        hi, lo, lo2 = split3(x)
        qrow[u, 0], qrow[u, 1], qrow[u, 2] = hi, lo, lo2
        for j in range(64):
            jp = 3 - j
            biast[:, u, j] = (sl * (128.0 * jp + p)).astype(np.float32)
    c["qrow"] = qrow
    c["biast"] = biast
    kones = np.zeros((35, S), dtype=np.float32)
    for b in range(32):
        kones[b, b * 256:(b + 1) * 256] = 1.0
    kones[32:35] = 1.0
    c["kones"] = kones.astype(NPBF)
    q = np.arange(512)[None, :]
    if kind == 0:
        tiles = []
        for (w, d) in DSW:
            for jp in range(-w // 128, 4):
                tk = 128 * jp + np.arange(128)[:, None]
                dist = q - tk
                valid = (dist >= 0) & (dist <= w) & (dist % d == 0)
                tiles.append(np.where(valid, 0.0, NEG))
        m = np.stack(tiles, axis=1)
    else:
        tiles = []
        for i in range(4):
            tk = 128 * i + np.arange(128)[:, None]
            tiles.append(np.where(tk > q, NEG, 0.0))
        m = np.stack(tiles, axis=1)
    c["masks"] = m.astype(np.float32).astype(NPBF)
    return c


def phase_b(nc, P, A, kind, recv_qk, recv_v, cd, oT_d):
    vd = 128 if kind == 2 else 64
    VW = vd + 1
    arow = 96 if kind == 1 else 64
    KA = arow + 3
    NGq = S // 512
    nmask = 33 if kind == 0 else 4
    ident = A.sb([128, 128], BF16)
    r_id = Res()
    P.dma("sp", ident[:], cd["ident"], writes=[r_id])
    biast = A.sb([128, 3, 64], F32)
    r_bt = Res()
    P.dma("sp", biast[:], cd["biast"], writes=[r_bt])
    masks = A.sb([128, nmask, 512], BF16)
    r_mk = Res()
    P.dma("pool", masks[:], cd["masks"], writes=[r_mk])
    ones_f = A.sb([128, 64], F32)
    r_of = Res()
    P.dma("sp", ones_f[:], cd["sel"], writes=[r_of])

    nunit_res = 3 if kind == 0 else 2
    ka = [A.sb([128, S], BF16) for _ in range(nunit_res)]
    r_ka = [Res() for _ in range(nunit_res)]
    va = [A.sb([128, 64, VW], BF16) for _ in range(nunit_res)]
    r_va = [Res() for _ in range(nunit_res)]
    for i in range(nunit_res):
        P.add("pool", lambda h, i=i: h.memset(va[i][:, :, 64:65], 1.0), writes=[r_va[i]])
    NQB = 4
    qa = [A.sb([128, 512], BF16) for _ in range(NQB)]
    r_qa = [Res() for _ in range(NQB)]
    NS = 3
    s_ps = [A.ps([128, 512], F32) for _ in range(NS)]
    r_s = [Res() for _ in range(NS)]
    NPT = 4
    pt = [A.sb([128, 512], BF16) for _ in range(NPT)]
    r_pt = [Res() for _ in range(NPT)]
    if kind == 2:
        oa_ps = [A.ps([128, 512], F32) for _ in range(2)]
        ob_ps = [A.ps([128, 512], F32) for _ in range(2)]
        r_oa = [Res() for _ in range(2)]
        r_ob = [Res() for _ in range(2)]
    elif kind == 0:
        oa_ps = [A.ps([128, 512], F32) for _ in range(4)]
        r_oa = [Res() for _ in range(4)]
    else:
        oa_ps = [A.ps([128, 512], F32) for _ in range(2)]
        r_oa = [Res() for _ in range(2)]
    bc_ps = A.ps([128, 512], F32)
    r_bc = Res()
    rd = A.sb([128, 512], F32)
    r_rd = Res()
    P.add("pool", lambda h: h.memset(rd[:], 0.0), writes=[r_rd])
    osb = [A.sb([64, 512], F32) for _ in range(2)]
    r_osb = [Res() for _ in range(2)]
    outb = [A.sb([64, 512], BF16) for _ in range(4)]
    r_outb = [Res() for _ in range(4)]
    if kind == 1:
        gate_ps = A.ps([128, 4, 32], F32)
        r_gate = Res()
        tr_ps = A.ps([128, 512], F32)
        r_tr = Res()
        gm = A.sb([128, 4, 32], F32)
        r_gm = Res()
        P.add("pool", lambda h: h.memset(gm[:], -1e30), writes=[r_gm])
        mx8 = A.sb([128, 4, 8], F32)
        r_mx = Res()
        negm = A.sb([128, 4, 96], BF16)
        r_negm = Res()
        P.add("pool", lambda h: h.memset(negm[:], 0.0), writes=[r_negm])
        kms = A.sb([64, 32], F32)
        kmb = A.sb([64, 32], BF16)
        r_km = Res()

    cnt = dict(q=0, s=0, pt=0, o=0, osb=0, outb=0)

    def load_unit(u, slot):
        for src in range(4):
            P.dma("sp", ka[slot][0:64, src * TL:(src + 1) * TL], recv_qk[src, 1, u], writes=[r_ka[slot]])
        if kind == 1:
            P.dma("pool", ka[slot][64:99, :], cd["kones"], writes=[r_ka[slot]])
        else:
            P.dma("pool", ka[slot][64:67, :], cd["kones"][32:35, :], writes=[r_ka[slot]])
        for src in range(4):
            sv = recv_v[src, u].rearrange("(t p) v -> p t v", p=128)
            if vd == 64:
                P.dma("pool", va[slot][:, src * 16:(src + 1) * 16, 0:64], sv, writes=[r_va[slot]])
            else:
                P.dma("pool", va[slot][:, src * 16:(src + 1) * 16, 0:64], sv[:, :, 0:64], writes=[r_va[slot]])
                P.dma("pool", va[slot][:, src * 16:(src + 1) * 16, 65:129], sv[:, :, 64:128], writes=[r_va[slot]])

    def load_q(u, G):
        qb = cnt["q"] % NQB
        cnt["q"] += 1
        src, lg = G // 4, G % 4
        P.dma("sp", qa[qb][0:64, :], recv_qk[src, 0, u, :, lg * 512:(lg + 1) * 512], writes=[r_qa[qb]])
        P.dma("sp", qa[qb][arow:arow + 3, :], cd["qrow"][u, :, G * 512:(G + 1) * 512], writes=[r_qa[qb]])
        return qb

    def moba_prep(slot, qb, G):
        for i in range(4):
            P.add("pe", lambda h, i=i: h.matmul(gate_ps[:, i, :], qa[qb][0:64, i * 128:(i + 1) * 128], kmb[:, :], start=True, stop=True),
                  reads=[r_qa[qb], r_km], writes=[r_gate])
        for i in range(4):
            n = (4 * G + i) // 2
            if n > 0:
                P.add("dve", lambda h, i=i, n=n: h.tensor_copy(out=gm[:, i, 0:n], in_=gate_ps[:, i, 0:n]), reads=[r_gate], writes=[r_gm])
            P.add("dve", lambda h, i=i: h.max(out=mx8[:, i, :], in_=gm[:, i, :]), reads=[r_gm], writes=[r_mx])
            P.add("dve", lambda h, i=i: h.tensor_scalar(out=negm[:, i, 64:96], in0=gm[:, i, :], scalar1=mx8[:, i, 2:3], scalar2=1.0,
                                                        op0=ALU.is_ge, op1=ALU.subtract), reads=[r_gm, r_mx], writes=[r_negm])
            P.add("dve", lambda h, i=i, n=n: h.memset(negm[:, i, 64 + n:65 + n], 0.0), writes=[r_negm])

    def moba_prep2(slot, qb, G):
        for i in range(4):
            P.add("pe", lambda h, i=i: h.matmul(tr_ps[0:96, i * 128:(i + 1) * 128], negm[:, i, :], ident[:], start=True, stop=True),
                  reads=[r_negm, r_id], writes=[r_tr])
        P.add("act", lambda h: h.activation(out=qa[qb][64:96, :], in_=tr_ps[64:96, :], func=AF.Copy, scale=-NEG),
              reads=[r_tr], writes=[r_qa[qb]])

    def steps_for(u, G):
        out = []
        if kind == 0:
            w, d = DSW[u]
            nb = w // 128
            moff = [0, 5, 13][u]
            for jp in range(-nb, 4):
                kt = 4 * G + jp
                if kt < 0:
                    continue
                out.append((kt, moff + jp + nb, 3 - jp))
        else:
            for kt in range(0, 4 * G + 4):
                jp = kt - 4 * G
                out.append((kt, jp if jp >= 0 else None, 3 - jp))
        return out

    def attend(u, slot, G, qb, ob):
        attend_flat([(u, slot, G, qb, ob)])

    def attend_flat(segs):
        flat = []
        for (u, slot, G, qb, ob) in segs:
            st = steps_for(u, G)
            for i, (kt, mi, bj) in enumerate(st):
                flat.append((u, slot, qb, ob, kt, mi, bj, i == 0, i == len(st) - 1))
        n = len(flat)
        sb_of = {}
        pt_of = {}

        def qk(i):
            u, slot, qb, ob, kt, mi, bj, first, lastf = flat[i]
            sbk = cnt["s"] % NS
            cnt["s"] += 1
            sb_of[i] = sbk
            P.add("pe", lambda h: h.matmul(s_ps[sbk][:], ka[slot][0:KA, kt * 128:(kt + 1) * 128], qa[qb][0:KA, :], start=True, stop=(mi is None)),
                  reads=[r_ka[slot], r_qa[qb]], writes=[r_s[sbk]])
            if mi is not None:
                P.add("pe", lambda h: h.matmul(s_ps[sbk][:], ident[:], masks[:, mi, :], start=False, stop=True),
                      reads=[r_id, r_mk], writes=[r_s[sbk]])

        def ex(i):
            u, slot, qb, ob, kt, mi, bj, first, lastf = flat[i]
            sbk = sb_of[i]
            pb = cnt["pt"] % NPT
            cnt["pt"] += 1
            pt_of[i] = pb
            P.add("act", lambda h: h.activation(out=pt[pb][:], in_=s_ps[sbk][:], func=AF.Exp, bias=biast[:, u, bj:bj + 1], scale=1.0),
                  reads=[r_s[sbk], r_bt], writes=[r_pt[pb]])

        def pv(i):
            u, slot, qb, ob, kt, mi, bj, first, lastf = flat[i]
            pb = pt_of[i]
            P.add("pe", lambda h: h.matmul(oa_ps[ob][0:65, :], va[slot][:, kt, 0:65], pt[pb][:], start=first, stop=lastf),
                  reads=[r_va[slot], r_pt[pb]], writes=[r_oa[ob]])
            if kind == 2:
                P.add("pe", lambda h: h.matmul(ob_ps[ob][0:64, :], va[slot][:, kt, 65:129], pt[pb][:], start=first, stop=lastf),
                      reads=[r_va[slot], r_pt[pb]], writes=[r_ob[ob]])

        LA = 2
        for i in range(min(LA, n)):
            qk(i)
            ex(i)
        for i in range(n):
            if i + LA < n:
                qk(i + LA)
                ex(i + LA)
            pv(i)

    def finish(u, G, obs):
        gs = slice(G * 512, (G + 1) * 512)
        first = True
        for (uu, ob) in obs:
            if first:
                P.add("dve", lambda h, ob=ob: h.tensor_copy(out=rd[64:65, :], in_=oa_ps[ob][64:65, :]), reads=[r_oa[ob]], writes=[r_rd])
                first = False
            else:
                P.add("dve", lambda h, ob=ob: h.tensor_tensor(out=rd[64:65, :], in0=rd[64:65, :], in1=oa_ps[ob][64:65, :], op=ALU.add),
                      reads=[r_oa[ob], r_rd], writes=[r_rd])
        P.add("dve", lambda h: h.reciprocal(out=rd[64:65, :], in_=rd[64:65, :]), reads=[r_rd], writes=[r_rd])

    def finish2(u, G, obs):
        gs = slice(G * 512, (G + 1) * 512)
        P.add("pe", lambda h: h.matmul(bc_ps[0:64, :], ones_f[:, :], rd[:, :], start=True, stop=True),
              reads=[r_of, r_rd], writes=[r_bc])
        for (uu, ob) in obs:
            parts = [(oa_ps, r_oa, 0)] + ([(ob_ps, r_ob, 64)] if kind == 2 else [])
            for (pst, rr, row0) in parts:
                sb = cnt["osb"] % 2
                cnt["osb"] += 1
                bb = cnt["outb"] % 4
                cnt["outb"] += 1
                P.add("act", lambda h, pst=pst, ob=ob, sb=sb: h.activation(out=osb[sb][:], in_=pst[ob][0:64, :], func=AF.Copy),
                      reads=[rr[ob]], writes=[r_osb[sb]])
                P.add("dve", lambda h, sb=sb, bb=bb: h.tensor_tensor(out=outb[bb][:], in0=osb[sb][:], in1=bc_ps[0:64, :], op=ALU.mult),
                      reads=[r_osb[sb], r_bc], writes=[r_outb[bb]])
                if len(oT_d.shape) == 4:
                    dst = oT_d[G // 4, uu, row0:row0 + 64, (G % 4) * 512:(G % 4 + 1) * 512]
                else:
                    dst = oT_d[uu, row0:row0 + 64, gs]
                P.dma("sp", dst, outb[bb][:], reads=[r_outb[bb]])

    if kind == 0:
        for u in range(3):
            load_unit(u, u)
        pend = None
        for G in range(NGq):
            obs = []
            segs = []
            for idx, u in enumerate((2, 1, 0)):
                qb = load_q(u, G)
                ob = (3 * G + idx) % 4
                segs.append((u, u, G, qb, ob))
                obs.append((u, ob))
            attend_flat(segs[:1])
            if pend is not None:
                finish2(*pend)
            attend_flat(segs[1:])
            finish(0, G, obs)
            pend = (0, G, obs)
        finish2(*pend)
    else:
        load_unit(0, 0)
        for u in range(3):
            slot = u % 2
            if u + 1 < 3:
                load_unit(u + 1, (u + 1) % 2)
            if kind == 1:
                P.add("dve", lambda h, slot=slot: h.tensor_reduce(out=kms[:, :], in_=ka[slot][0:64, :].rearrange("p (b k) -> p b k", k=256),
                                                                   axis=AX.X, op=ALU.add), reads=[r_ka[slot]], writes=[r_km])
                P.add("dve", lambda h: h.tensor_copy(out=kmb[:, :], in_=kms[:, :]), reads=[r_km], writes=[r_km])
                if u > 0:
                    P.add("pool", lambda h: h.memset(gm[:], -1e30), writes=[r_gm])
            qbs = {0: load_q(u, 0)}
            if kind == 1:
                moba_prep(slot, qbs[0], 0)
                moba_prep2(slot, qbs[0], 0)
            pend = None
            for G in range(NGq):
                if G + 1 < NGq:
                    qbs[G + 1] = load_q(u, G + 1)
                    if kind == 1:
                        moba_prep(slot, qbs[G + 1], G + 1)
                ob = cnt["o"] % 2
                cnt["o"] += 1
                attend(u, slot, G, qbs[G], ob)
                if pend is not None:
                    finish2(*pend)
                if kind == 1 and G + 1 < NGq:
                    moba_prep2(slot, qbs[G + 1], G + 1)
                finish(u, G, [(u, ob)])
                pend = (u, G, [(u, ob)])
            finish2(*pend)


def build_phase_b_prog(kind):
    nc = bass.Bass("TRN2", target_bir_lowering=False)
    vd = 128 if kind == 2 else 64
    recv_qk = nc.dram_tensor("recv_qk", [4, 2, 3, 64, TL], BF16, kind="ExternalInput").ap()
    recv_v = nc.dram_tensor("recv_v", [4, 3, TL, vd], BF16, kind="ExternalInput").ap()
    nmask = 33 if kind == 0 else 4
    cd = dict(
        ident=nc.dram_tensor("ident", [128, 128], BF16, kind="ExternalInput").ap(),
        sel=nc.dram_tensor("sel", [128, 64], F32, kind="ExternalInput").ap(),
        qrow=nc.dram_tensor("qrow", [3, 3, S], BF16, kind="ExternalInput").ap(),
        biast=nc.dram_tensor("biast", [128, 3, 64], F32, kind="ExternalInput").ap(),
        kones=nc.dram_tensor("kones", [35, S], BF16, kind="ExternalInput").ap(),
        masks=nc.dram_tensor("masks", [128, nmask, 512], BF16, kind="ExternalInput").ap(),
    )
    oT = nc.dram_tensor("oT", [3, vd, S], BF16, kind="ExternalOutput").ap()
    P = Prog(nc)
    A = SB(nc)
    phase_b(nc, P, A, kind, recv_qk, recv_v, cd, oT)
    P.emit()
    return nc, P


def phase_c(nc, P, A, kind, layer, xT_res, xT_sb, recv_o, gateT_d, qmT_d, memT_d, mgcol_d, wkv_d, wout_d, sel_d,
            diffp=None, final=None, xT_out=None, load_x_from=None):
    vd = 128 if kind == 2 else 64
    NG = TL // 512
    lam_init = 0.8 - 0.6 * float(np.exp(-0.3 * layer))
    if load_x_from is not None:
        xv = load_x_from.rearrange("(c p) t -> p c t", p=128)
        for G in range(NG):
            P.dma("sp", xT_sb[:, :, G * 512:(G + 1) * 512], xv[:, :, G * 512:(G + 1) * 512], writes=[xT_res[G]])
    ones_bf = A.sb([128, 128], BF16)
    r_ones = Res()
    P.add("pool", lambda h: h.memset(ones_bf[:], 1.0), writes=[r_ones])
    sel = A.sb([128, 64], F32)
    r_sel = Res()
    P.dma("sp", sel[:], sel_d, writes=[r_sel])
    mgcol = A.sb([128, 8], F32)
    r_mg = Res()
    P.dma("sp", mgcol[:], mgcol_d, writes=[r_mg])

    NPS = 6
    ps = [A.ps([128, 512], F32) for _ in range(NPS)]
    r_ps = [Res() for _ in range(NPS)]
    cnt = dict(ps=0, pt=0, st=0)

    def nps():
        i = cnt["ps"] % NPS
        cnt["ps"] += 1
        return i

    memT = A.sb([128, 8, 256], F32)
    r_mem = Res()
    P.dma("sp", memT[:], memT_d.rearrange("(c p) t -> p c t", p=128), writes=[r_mem])
    sqm = [A.sb([128, 512], BF16) for _ in range(2)]
    r_sqm = [Res() for _ in range(2)]
    rstd = A.sb([128, 512], F32)
    r_rstd = Res()
    mnT = A.sb([128, 8, 256], BF16)
    r_mn = Res()
    pb = nps()
    for c in range(8):
        b = c % 2
        P.add("act", lambda h, b=b, c=c: h.activation(out=sqm[b][:, 0:256], in_=memT[:, c, :], func=AF.Square), reads=[r_mem], writes=[r_sqm[b]])
        P.add("pe", lambda h, b=b, c=c: h.matmul(ps[pb][:, 0:256], ones_bf[:], sqm[b][:, 0:256], start=(c == 0), stop=(c == 7)),
              reads=[r_sqm[b], r_ones], writes=[r_ps[pb]])
    P.add("act", lambda h: h.activation(out=rstd[:, 0:256], in_=ps[pb][:, 0:256], func=AF.Sqrt, scale=1.0 / D, bias=RMS_EPS), reads=[r_ps[pb]], writes=[r_rstd])
    P.add("dve", lambda h: h.reciprocal(out=rstd[:, 0:256], in_=rstd[:, 0:256]), reads=[r_rstd], writes=[r_rstd])
    for c in range(8):
        P.add("dve", lambda h, c=c: h.tensor_tensor(out=mnT[:, c, :], in0=memT[:, c, :], in1=rstd[:, 0:256], op=ALU.mult), reads=[r_mem, r_rstd], writes=[r_mn])
    WS = 128
    wst = [A.sb([128, 8, WS], F32) for _ in range(2)]
    r_wst = [Res() for _ in range(2)]
    wkv = A.sb([128, 8, 512], BF16)
    r_wkv = Res()
    wkv_v = wkv_d.rearrange("(c p) n -> p c n", p=128)
    for j in range(512 // WS):
        b = cnt["st"] % 2
        cnt["st"] += 1
        P.dma("sp", wst[b][:], wkv_v[:, :, j * WS:(j + 1) * WS], writes=[r_wst[b]])
        for c in range(8):
            if c % 2 == 0:
                P.add("dve", lambda h, b=b, c=c, j=j: h.tensor_scalar(out=wkv[:, c, j * WS:(j + 1) * WS], in0=wst[b][:, c, :], scalar1=mgcol[:, c:c + 1],
                                                                       scalar2=None, op0=ALU.mult), reads=[r_wst[b], r_mg], writes=[r_wkv])
            else:
                P.add("act", lambda h, b=b, c=c, j=j: h.activation(out=wkv[:, c, j * WS:(j + 1) * WS], in_=wst[b][:, c, :], func=AF.Copy, scale=mgcol[:, c:c + 1]),
                      reads=[r_wst[b], r_mg], writes=[r_wkv])
    kmT = A.sb([128, 2, 256], BF16)
    r_kmT = Res()
    for ch in range(2):
        pb = nps()
        for c in range(8):
            P.add("pe", lambda h, pb=pb, c=c, ch=ch: h.matmul(ps[pb][:, 0:256], wkv[:, c, ch * 128:(ch + 1) * 128], mnT[:, c, :], start=(c == 0), stop=(c == 7)),
                  reads=[r_wkv, r_mn], writes=[r_ps[pb]])
        P.add("act", lambda h, pb=pb, ch=ch: h.activation(out=kmT[:, ch, :], in_=ps[pb][:, 0:256], func=AF.Copy), reads=[r_ps[pb]], writes=[r_kmT])
    vma = A.sb([128, 2, 4, 65], BF16)
    r_vma = Res()
    P.add("pool", lambda h: h.memset(vma[:], 1.0), writes=[r_vma])
    for mt in range(2):
        pb = nps()
        for c in range(8):
            P.add("pe", lambda h, pb=pb, c=c, mt=mt: h.matmul(ps[pb][:, 0:256], mnT[:, c, mt * 128:(mt + 1) * 128], wkv[:, c, 256:512], start=(c == 0), stop=(c == 7)),
                  reads=[r_wkv, r_mn], writes=[r_ps[pb]])
        P.add("dve", lambda h, pb=pb, mt=mt: h.tensor_copy(out=vma[:, mt, :, 0:64], in_=ps[pb][:, 0:256].rearrange("p (h d) -> p h d", d=64)),
              reads=[r_ps[pb]], writes=[r_vma])

    qm = A.sb([128, 2, TL], BF16)
    r_qm = Res()
    P.dma("sp", qm[:], qmT_d.rearrange("(c p) t -> p c t", p=128), writes=[r_qm])
    yT = A.sb([128, 8, TL], BF16)
    r_y = [[Res() for _ in range(NG)] for _ in range(8)]
    pt = [A.sb([128, 512], BF16) for _ in range(3)]
    r_pt = [Res() for _ in range(3)]
    rd = A.sb([128, 512], F32)
    r_rd = Res()
    P.add("pool", lambda h: h.memset(rd[:], 0.0), writes=[r_rd])
    osb = [A.sb([64, 512], F32) for _ in range(2)]
    r_osb = [Res() for _ in range(2)]
    mo = [A.sb([64, 512], BF16) for _ in range(2)]
    r_mo = [Res() for _ in range(2)]
    k = 0
    for G in range(NG):
        gs = slice(G * 512, (G + 1) * 512)
        for hm in range(4):
            ch, r0 = hm // 2, (hm % 2) * 64
            po = nps()
            for mt in range(2):
                pb = nps()
                P.add("pe", lambda h, pb=pb, ch=ch, r0=r0, mt=mt, gs=gs: h.matmul(ps[pb][:], kmT[r0:r0 + 64, ch, mt * 128:(mt + 1) * 128], qm[r0:r0 + 64, ch, gs],
                                                                                 start=True, stop=True), reads=[r_kmT, r_qm], writes=[r_ps[pb]])
                pi = cnt["pt"] % 3
                cnt["pt"] += 1
                P.add("act", lambda h, pb=pb, pi=pi: h.activation(out=pt[pi][:], in_=ps[pb][:], func=AF.Exp), reads=[r_ps[pb]], writes=[r_pt[pi]])
                P.add("pe", lambda h, po=po, pi=pi, mt=mt, hm=hm: h.matmul(ps[po][0:65, :], vma[:, mt, hm, :], pt[pi][:], start=(mt == 0), stop=(mt == 1)),
                      reads=[r_vma, r_pt[pi]], writes=[r_ps[po]])
            P.add("dve", lambda h, po=po: h.reciprocal(out=rd[64:65, :], in_=ps[po][64:65, :]), reads=[r_ps[po], r_rd], writes=[r_rd])
            pbc = nps()
            P.add("pe", lambda h, pbc=pbc: h.matmul(ps[pbc][0:64, :], sel[:, :], rd[:, :], start=True, stop=True), reads=[r_sel, r_rd], writes=[r_ps[pbc]])
            sb = k % 2
            mb = k % 2
            k += 1
            P.add("act", lambda h, po=po, sb=sb: h.activation(out=osb[sb][:], in_=ps[po][0:64, :], func=AF.Copy), reads=[r_ps[po]], writes=[r_osb[sb]])
            P.add("dve", lambda h, sb=sb, mb=mb, pbc=pbc: h.tensor_tensor(out=mo[mb][:], in0=osb[sb][:], in1=ps[pbc][0:64, :], op=ALU.mult),
                  reads=[r_osb[sb], r_ps[pbc]], writes=[r_mo[mb]])
            P.dma("pool", yT[r0:r0 + 64, 6 + ch, gs], mo[mb][:], reads=[r_mo[mb]], writes=[r_y[6 + ch][G]])

    if kind in (0, 1):
        for c in range(6):
            for half in range(2):
                hh = 2 * c + half
                g, u = hh % 4, hh // 4
                P.dma("sp", yT[half * 64:(half + 1) * 64, c, :], recv_o[g, u], writes=[r_y[c][G] for G in range(NG)])
    else:
        lp = A.sb([128, 4, 64], F32)
        r_lp = Res()
        for i, nm in enumerate(("lq1", "lk1", "lq2", "lk2")):
            P.dma("sp", lp[:, i, :], diffp[nm][0].partition_broadcast(128), writes=[r_lp])
        sgc = A.sb([128, 1], F32)
        r_sg = Res()
        P.dma("sp", sgc[:], diffp["sg"], writes=[r_sg])
        lpr = A.sb([128, 2, 64], F32)
        lsum = A.sb([128, 2], F32)
        nlam = A.sb([128, 1], F32)
        r_lam = Res()
        P.add("dve", lambda h: h.tensor_tensor(out=lpr[:, 0, :], in0=lp[:, 0, :], in1=lp[:, 1, :], op=ALU.mult), reads=[r_lp], writes=[r_lam])
        P.add("dve", lambda h: h.tensor_tensor(out=lpr[:, 1, :], in0=lp[:, 2, :], in1=lp[:, 3, :], op=ALU.mult), reads=[r_lp, r_lam], writes=[r_lam])
        P.add("dve", lambda h: h.tensor_reduce(out=lsum[:, :], in_=lpr[:, :, :], axis=AX.X, op=ALU.add), reads=[r_lam], writes=[r_lam])
        P.add("act", lambda h: h.activation(out=lsum[:, :], in_=lsum[:, :], func=AF.Exp), reads=[r_lam], writes=[r_lam])
        P.add("dve", lambda h: h.tensor_tensor(out=nlam[:, :], in0=lsum[:, 1:2], in1=lsum[:, 0:1], op=ALU.subtract), reads=[r_lam], writes=[r_lam])
        P.add("dve", lambda h: h.tensor_scalar(out=nlam[:, :], in0=nlam[:, :], scalar1=-lam_init, scalar2=None, op0=ALU.add), reads=[r_lam], writes=[r_lam])
        P.add("dve", lambda h: h.tensor_scalar(out=sgc[:, :], in0=sgc[:, :], scalar1=(1.0 - lam_init), scalar2=None, op0=ALU.mult), reads=[r_sg], writes=[r_sg])
        m12 = [A.sb([128, 2, 512], BF16) for _ in range(2)]
        r_m12 = [Res() for _ in range(2)]
        od = [A.sb([128, 512], F32) for _ in range(2)]
        r_od = [Res() for _ in range(2)]
        sq2 = [A.sb([128, 512], BF16) for _ in range(2)]
        r_sq2 = [Res() for _ in range(2)]
        rs2 = A.sb([128, 512], F32)
        r_rs2 = Res()
        k = 0
        for G in range(NG):
            gs = slice(G * 512, (G + 1) * 512)
            for c in range(6):
                b = k % 2
                k += 1
                for m_ in range(2):
                    mm = 2 * c + m_
                    g, u = mm // 3, mm % 3
                    P.dma("sp", m12[b][:, m_, :], recv_o[g, u, :, gs], writes=[r_m12[b]])
                P.add("dve", lambda h, b=b: h.scalar_tensor_tensor(out=od[b][:], in0=m12[b][:, 1, :], scalar=nlam[:, 0:1], in1=m12[b][:, 0, :],
                                                                     op0=ALU.mult, op1=ALU.add), reads=[r_m12[b], r_lam], writes=[r_od[b]])
                P.add("act", lambda h, b=b: h.activation(out=sq2[b][:], in_=od[b][:], func=AF.Square), reads=[r_od[b]], writes=[r_sq2[b]])
                pb = nps()
                P.add("pe", lambda h, pb=pb, b=b: h.matmul(ps[pb][:], ones_bf[:], sq2[b][:], start=True, stop=True), reads=[r_sq2[b], r_ones], writes=[r_ps[pb]])
                P.add("act", lambda h, pb=pb: h.activation(out=rs2[:], in_=ps[pb][:], func=AF.Sqrt, scale=1.0 / 128.0, bias=SUBLN_EPS), reads=[r_ps[pb]], writes=[r_rs2])
                P.add("dve", lambda h: h.reciprocal(out=rs2[:], in_=rs2[:]), reads=[r_rs2], writes=[r_rs2])
                P.add("dve", lambda h, b=b, c=c, gs=gs: h.scalar_tensor_tensor(out=yT[:, c, gs], in0=od[b][:], scalar=sgc[:, 0:1], in1=rs2[:],
                                                                                 op0=ALU.mult, op1=ALU.mult), reads=[r_od[b], r_sg, r_rs2], writes=[r_y[c][G]])

    gt = [A.sb([128, 512], BF16) for _ in range(3)]
    r_gt = [Res() for _ in range(3)]
    gview = gateT_d.rearrange("(c p) t -> p c t", p=128)
    kk = 0
    for c in range(8):
        for G in range(NG):
            b = kk % 3
            kk += 1
            gs = slice(G * 512, (G + 1) * 512)
            P.dma("sp", gt[b][:], gview[:, c, gs], writes=[r_gt[b]])
            P.add("act", lambda h, b=b: h.activation(out=gt[b][:], in_=gt[b][:], func=AF.Silu), reads=[r_gt[b]], writes=[r_gt[b]])
            P.add("dve", lambda h, b=b, c=c, gs=gs: h.tensor_tensor(out=yT[:, c, gs], in0=yT[:, c, gs], in1=gt[b][:], op=ALU.mult),
                  reads=[r_gt[b], r_y[c][G]], writes=[r_y[c][G]])

    wo = A.sb([128, 8, D], BF16)
    r_wo = [Res() for _ in range(8)]
    wo_v = wout_d.rearrange("(c p) n -> p c n", p=128)
    for j in range(8):
        b = cnt["st"] % 2
        cnt["st"] += 1
        P.dma("sp", wst[b][:], wo_v[:, :, j * WS:(j + 1) * WS], writes=[r_wst[b]])
        for c in range(8):
            if c % 2 == 0:
                P.add("dve", lambda h, b=b, c=c, j=j: h.tensor_copy(out=wo[:, c, j * WS:(j + 1) * WS], in_=wst[b][:, c, :]), reads=[r_wst[b]], writes=[r_wo[j]])
            else:
                P.add("act", lambda h, b=b, c=c, j=j: h.activation(out=wo[:, c, j * WS:(j + 1) * WS], in_=wst[b][:, c, :], func=AF.Copy), reads=[r_wst[b]], writes=[r_wo[j]])
    for G in range(NG):
        gs = slice(G * 512, (G + 1) * 512)
        for co in range(8):
            pb = nps()
            for c in range(8):
                P.add("pe", lambda h, pb=pb, c=c, co=co, gs=gs: h.matmul(ps[pb][:], wo[:, c, co * 128:(co + 1) * 128], yT[:, c, gs], start=(c == 0), stop=(c == 7)),
                      reads=[r_wo[co], r_y[c][G]], writes=[r_ps[pb]])
            P.add("dve", lambda h, pb=pb, co=co, gs=gs: h.tensor_tensor(out=xT_sb[:, co, gs], in0=xT_sb[:, co, gs], in1=ps[pb][:], op=ALU.add),
                  reads=[r_ps[pb], xT_res[G]], writes=[xT_res[G]])
    if xT_out is not None:
        xo = xT_out.rearrange("(c p) t -> p c t", p=128)
        for G in range(NG):
            gs = slice(G * 512, (G + 1) * 512)
            P.dma("sp", xo[:, :, gs], xT_sb[:, :, gs], reads=[xT_res[G]])
    if final is not None:
        fg = A.sb([128, 8], F32)
        r_fg = Res()
        P.dma("sp", fg[:], final["gcol"], writes=[r_fg])
        ob = [A.sb([128, 512], F32) for _ in range(2)]
        r_ob = [Res() for _ in range(2)]
        fo = final["out"].rearrange("(c p) t -> p c t", p=128)
        k = 0
        for G in range(NG):
            gs = slice(G * 512, (G + 1) * 512)
            pb = nps()
            for c in range(8):
                b = c % 2
                P.add("act", lambda h, b=b, c=c, gs=gs: h.activation(out=sqm[b][:], in_=xT_sb[:, c, gs], func=AF.Square), reads=[xT_res[G]], writes=[r_sqm[b]])
                P.add("pe", lambda h, b=b, c=c, pb=pb: h.matmul(ps[pb][:], ones_bf[:], sqm[b][:], start=(c == 0), stop=(c == 7)), reads=[r_sqm[b], r_ones], writes=[r_ps[pb]])
            P.add("act", lambda h, pb=pb: h.activation(out=rstd[:], in_=ps[pb][:], func=AF.Sqrt, scale=1.0 / D, bias=RMS_EPS), reads=[r_ps[pb]], writes=[r_rstd])
            P.add("dve", lambda h: h.reciprocal(out=rstd[:], in_=rstd[:]), reads=[r_rstd], writes=[r_rstd])
            for c in range(8):
                b = k % 2
                k += 1
                P.add("dve", lambda h, b=b, c=c, gs=gs: h.scalar_tensor_tensor(out=ob[b][:], in0=xT_sb[:, c, gs], scalar=fg[:, c:c + 1], in1=rstd[:],
                                                                                 op0=ALU.mult, op1=ALU.mult), reads=[xT_res[G], r_fg, r_rstd], writes=[r_ob[b]])
                P.dma("sp", fo[:, c, gs], ob[b][:], reads=[r_ob[b]])


def build_phase_c_prog(kind, layer, last):
    nc = bass.Bass("TRN2", target_bir_lowering=False)
    vd = 128 if kind == 2 else 64
    xT_d = nc.dram_tensor("xT", [D, TL], F32, kind="ExternalInput").ap()
    recv_o = nc.dram_tensor("recv_o", [4, 3, vd, TL], BF16, kind="ExternalInput").ap()
    gateT = nc.dram_tensor("gateT", [D, TL], BF16, kind="ExternalInput").ap()
    qmT = nc.dram_tensor("qmT", [256, TL], BF16, kind="ExternalInput").ap()
    memT = nc.dram_tensor("memT", [D, 256], F32, kind="ExternalInput").ap()
    mgcol = nc.dram_tensor("mgcol", [128, 8], F32, kind="ExternalInput").ap()
    wkv = nc.dram_tensor("wkv", [D, 512], F32, kind="ExternalInput").ap()
    wout = nc.dram_tensor("wout", [D, D], F32, kind="ExternalInput").ap()
    sel = nc.dram_tensor("sel", [128, 64], F32, kind="ExternalInput").ap()
    diffp = None
    if kind == 2:
        diffp = {nm: nc.dram_tensor(nm, [1, 64], F32, kind="ExternalInput").ap() for nm in ("lq1", "lk1", "lq2", "lk2")}
        diffp["sg"] = nc.dram_tensor("sg", [128, 1], F32, kind="ExternalInput").ap()
    final = None
    xT_out = None
    if last:
        final = dict(gcol=nc.dram_tensor("fgcol", [128, 8], F32, kind="ExternalInput").ap(),
                     out=nc.dram_tensor("outT", [D, TL], F32, kind="ExternalOutput").ap())
    else:
        xT_out = nc.dram_tensor("xT_out", [D, TL], F32, kind="ExternalOutput").ap()
    P = Prog(nc)
    A = SB(nc)
    xT_sb = A.sb([128, 8, TL], F32)
    xres = [Res() for _ in range(TL // 512)]
    phase_c(nc, P, A, kind, layer, xres, xT_sb, recv_o, gateT, qmT, memT, mgcol, wkv, wout, sel, diffp=diffp, final=final,
            xT_out=xT_out, load_x_from=xT_d)
    P.emit()
    return nc, P


_PROGS = {}
DEBUG = {}


def _prog(key, builder):
    if key not in _PROGS:
        _PROGS[key] = builder()[0]
    return _PROGS[key]


def _col8(v):
    return np.ascontiguousarray(np.asarray(v, np.float32).reshape(8, 128).T)


def kernel_unfused(x, mem, norm_g, w_in, w_out, mem_norm_g, w_mem_kv, diff_lambda_q1, diff_lambda_k1,
                   diff_lambda_q2, diff_lambda_k2, diff_subln_g, final_norm_g):
    x = np.asarray(x, np.float32)
    mem = np.asarray(mem, np.float32)
    cores = list(range(NCORE))
    xT = [np.ascontiguousarray(x[c // 4, (c % 4) * TL:(c % 4 + 1) * TL, :].T) for c in cores]
    memT = [np.ascontiguousarray(mem[b].T) for b in range(B)]
    sel = np.zeros((128, 64), np.float32)
    sel[64, :] = 1.0
    out = None
    for layer in range(DEPTH):
        kind = layer % 3
        vd = 128 if kind == 2 else 64
        last = layer == DEPTH - 1
        ncA = _prog(("A", kind), lambda: build_phase_a_prog(kind))
        wl = np.ascontiguousarray(np.asarray(w_in[layer], np.float32))
        gcol = _col8(norm_g[layer])
        resA = run_bass_kernel_spmd(ncA, [{"xT": xT[c], "w_in": wl, "gcol": gcol} for c in cores], core_ids=cores).results
        in_b = []
        for c in cores:
            b, g = c // 4, c % 4
            rq = np.stack([resA[b * 4 + r]["send_qk"][g] for r in range(4)], axis=0)
            rv = np.stack([resA[b * 4 + r]["send_v"][g] for r in range(4)], axis=0)
            m = {"recv_qk": np.ascontiguousarray(rq), "recv_v": np.ascontiguousarray(rv)}
            m.update(phase_b_consts(kind, g))
            in_b.append(m)
        ncB = _prog(("B", kind), lambda: build_phase_b_prog(kind))
        resB = run_bass_kernel_spmd(ncB, in_b, core_ids=cores).results
        in_c = []
        for c in cores:
            b, r = c // 4, c % 4
            ro = np.stack([resB[b * 4 + g]["oT"][:, :, r * TL:(r + 1) * TL] for g in range(4)], axis=0)
            m = {"xT": xT[c], "recv_o": np.ascontiguousarray(ro), "gateT": resA[c]["gateT"], "qmT": resA[c]["qmT"],
                 "memT": memT[b], "mgcol": _col8(mem_norm_g[layer]),
                 "wkv": np.ascontiguousarray(np.asarray(w_mem_kv[layer], np.float32)),
                 "wout": np.ascontiguousarray(np.asarray(w_out[layer], np.float32)), "sel": sel}
            if kind == 2:
                ci = layer // 3
                m["lq1"] = np.asarray(diff_lambda_q1[ci], np.float32).reshape(1, 64)
                m["lk1"] = np.asarray(diff_lambda_k1[ci], np.float32).reshape(1, 64)
                m["lq2"] = np.asarray(diff_lambda_q2[ci], np.float32).reshape(1, 64)
                m["lk2"] = np.asarray(diff_lambda_k2[ci], np.float32).reshape(1, 64)
                m["sg"] = np.asarray(diff_subln_g[ci], np.float32).reshape(128, 1)
            if last:
                m["fgcol"] = _col8(final_norm_g)
            in_c.append(m)
        ncC = _prog(("C", kind, layer, last), lambda: build_phase_c_prog(kind, layer, last))
        resC = run_bass_kernel_spmd(ncC, in_c, core_ids=cores).results
        if last:
            out = np.empty((B, S, D), np.float32)
            for c in cores:
                out[c // 4, (c % 4) * TL:(c % 4 + 1) * TL, :] = resC[c]["outT"].T
        else:
            xT = [resC[c]["xT_out"] for c in cores]
            if "dump" in DEBUG:
                xs = np.empty((B, S, D), np.float32)
                for c in cores:
                    xs[c // 4, (c % 4) * TL:(c % 4 + 1) * TL, :] = xT[c].T
                DEBUG["dump"].append(xs)
    return out


KINDS = [l % 3 for l in range(DEPTH)]


def build_fused():
    nc = bass.Bass("TRN2", target_bir_lowering=False)
    _PID_CACHE.clear()

    def din(name, shape, dt=F32):
        return nc.dram_tensor(name, shape, dt, kind="ExternalInput").ap()

    def dint(name, shape, dt):
        return nc.dram_tensor(name, shape, dt, kind="Internal").ap()

    xT_in = din("xT_in", [4, D, TL])
    w_in = din("w_in", [DEPTH, D, INW])
    w_out = din("w_out", [DEPTH, D, D])
    wkv = din("wkv", [DEPTH, D, 512])
    gcols = din("gcols", [DEPTH, 128, 8])
    mgcols = din("mgcols", [DEPTH, 128, 8])
    fgcol = din("fgcol", [128, 8])
    memT = din("memT", [D, 256])
    sel = din("sel", [128, 64])
    diffp = {nm: din(nm, [1, 64]) for nm in ("lq1", "lk1", "lq2", "lk2")}
    diffp["sg"] = din("sg", [128, 1])
    ident = din("ident", [128, 128], BF16)
    kones = din("kones", [35, S], BF16)
    masks = {0: din("masks0", [128, 33, 512], BF16), 1: din("masks1", [128, 4, 512], BF16)}
    masks[2] = masks[1]
    qrow = {k: din(f"qrow{k}", [4, 3, 3, S], BF16) for k in (0, 1, 2)}
    biast = {k: din(f"biast{k}", [4, 128, 3, 64]) for k in (0, 1, 2)}
    outT = nc.dram_tensor("outT", [D, TL], F32, kind="ExternalOutput").ap()

    xbuf = dint("xbuf", [2, 4, D, TL], F32)
    qkbuf = dint("qkbuf", [4, 4, 2, 3, 64, TL], BF16)
    vbuf = {64: dint("vbuf64", [4, 4, 3, TL, 64], BF16), 128: dint("vbuf128", [4, 4, 3, TL, 128], BF16)}
    gbuf = dint("gbuf", [4, D, TL], BF16)
    qmbuf = dint("qmbuf", [4, 256, TL], BF16)
    obuf = {64: dint("obuf64", [4, 4, 3, 64, TL], BF16), 128: dint("obuf128", [4, 4, 3, 128, TL], BF16)}

    stage = dict(x=dint("st_x", [D, TL], F32), qk=dint("st_qk", [4, 2, 3, 64, TL], BF16), v=dint("st_v", [4, 3, TL, 64], BF16),
                 g=dint("st_g", [D, TL], BF16), qm=dint("st_qm", [256, TL], BF16), o=dint("st_o", [4, 3, 64, TL], BF16))
    P = Prog(nc)
    A = SB(nc, arena_words=48 * 1024 - 64)
    for layer in range(DEPTH):
        kind = KINDS[layer]
        vd = 128 if kind == 2 else 64
        last = layer == DEPTH - 1
        if last:
            for off in (3, 0):
                A.reset()
                P.dma("sp", stage["x"], core_slice(xbuf[layer % 2], off))
                P.barrier()
                xT_sb = A.sb([128, 8, TL], F32)
                xres = [Res() for _ in range(TL // 512)]
                phase_a(nc, P, A, kind, xres, xT_sb, w_in[layer], gcols[layer], stage["qk"], stage["v"], stage["g"], stage["qm"],
                        load_x_from=stage["x"])
                P.barrier()
                for nm, buf in (("qk", qkbuf), ("v", vbuf[vd]), ("g", gbuf), ("qm", qmbuf)):
                    P.dma("sp", core_slice(buf, off), stage[nm])
                P.barrier()
        else:
            for s_ in range(4):
                A.reset()
                xT_sb = A.sb([128, 8, TL], F32)
                xres = [Res() for _ in range(TL // 512)]
                src = xT_in[s_] if layer == 0 else xbuf[layer % 2, s_]
                phase_a(nc, P, A, kind, xres, xT_sb, w_in[layer], gcols[layer], qkbuf[s_], vbuf[vd][s_], gbuf[s_], qmbuf[s_], load_x_from=src)
                P.barrier()
        for g in range(4):
            A.reset()
            cd = dict(ident=ident, sel=sel, kones=kones, masks=masks[kind], qrow=qrow[kind][g], biast=biast[kind][g])
            phase_b(nc, P, A, kind, qkbuf[:, g], vbuf[vd][:, g], cd, obuf[vd][:, g])
            P.barrier()
        if last:
            A.reset()
            P.dma("sp", stage["x"], core_slice(xbuf[layer % 2], 0))
            P.dma("sp", stage["o"], core_slice(obuf[vd], 0))
            P.dma("sp", stage["g"], core_slice(gbuf, 0))
            P.dma("sp", stage["qm"], core_slice(qmbuf, 0))
            P.barrier()
            xT_sb = A.sb([128, 8, TL], F32)
            xres = [Res() for _ in range(TL // 512)]
            phase_c(nc, P, A, kind, layer, xres, xT_sb, stage["o"], stage["g"], stage["qm"], memT, mgcols[layer],
                    wkv[layer], w_out[layer], sel, diffp=None, final=dict(gcol=fgcol, out=outT), xT_out=None, load_x_from=stage["x"])
            P.barrier()
        else:
            for s_ in range(4):
                A.reset()
                xT_sb = A.sb([128, 8, TL], F32)
                xres = [Res() for _ in range(TL // 512)]
                src = xT_in[s_] if layer == 0 else xbuf[layer % 2, s_]
                phase_c(nc, P, A, kind, layer, xres, xT_sb, obuf[vd][s_], gbuf[s_], qmbuf[s_], memT, mgcols[layer],
                        wkv[layer], w_out[layer], sel, diffp=(diffp if kind == 2 else None), final=None,
                        xT_out=xbuf[(layer + 1) % 2, s_], load_x_from=src)
                P.barrier()
    P.emit()
    return nc, P


_FUSED = {}


def kernel(x, mem, norm_g, w_in, w_out, mem_norm_g, w_mem_kv, diff_lambda_q1, diff_lambda_k1,
           diff_lambda_q2, diff_lambda_k2, diff_subln_g, final_norm_g):
    x = np.asarray(x, np.float32)
    mem = np.asarray(mem, np.float32)
    cores = list(range(NCORE))
    if "nc" not in _FUSED:
        _FUSED["nc"] = build_fused()[0]
    nc = _FUSED["nc"]
    f32 = lambda a: np.ascontiguousarray(np.asarray(a, np.float32))
    sel = np.zeros((128, 64), np.float32)
    sel[64, :] = 1.0
    shared = {
        "w_in": f32(w_in), "w_out": f32(w_out), "wkv": f32(w_mem_kv),
        "gcols": np.stack([_col8(norm_g[l]) for l in range(DEPTH)]),
        "mgcols": np.stack([_col8(mem_norm_g[l]) for l in range(DEPTH)]),
        "fgcol": _col8(final_norm_g), "sel": sel,
        "lq1": f32(diff_lambda_q1[0]).reshape(1, 64), "lk1": f32(diff_lambda_k1[0]).reshape(1, 64),
        "lq2": f32(diff_lambda_q2[0]).reshape(1, 64), "lk2": f32(diff_lambda_k2[0]).reshape(1, 64),
        "sg": f32(diff_subln_g[0]).reshape(128, 1),
    }
    for k in (0, 1, 2):
        cs = [phase_b_consts(k, g) for g in range(4)]
        shared[f"qrow{k}"] = np.stack([c["qrow"] for c in cs])
        shared[f"biast{k}"] = np.stack([c["biast"] for c in cs])
        if k < 2:
            shared[f"masks{k}"] = cs[0]["masks"]
        shared["ident"] = cs[0]["ident"]
        shared["kones"] = cs[0]["kones"]
    in_maps = []
    for c in cores:
        b = c // 4
        m = dict(shared)
        m["xT_in"] = np.ascontiguousarray(x[b].reshape(4, TL, D).transpose(0, 2, 1))
        m["memT"] = np.ascontiguousarray(mem[b].T)
        in_maps.append(m)
    res = run_bass_kernel_spmd(nc, in_maps, core_ids=cores).results
    out = np.empty((B, S, D), np.float32)
    for c in cores:
        b, r = c // 4, c % 4
        out[b, r * TL:(r + 1) * TL, :] = res[c]["outT"].T
    return out
```
